# Optimizing a Trainium2 kernel written in Bass

```python
import numpy as np
import jax
import jax.numpy as jnp
from jax import lax

D_MODEL = 1024
BATCH = 2
SEQ = 8192
DEPTH = 4

N_META = 16
BLOCK_Q = 128
D_MIX = D_MODEL
N_GROUPS = 4
GROUP_W = D_MIX // N_GROUPS
HEAD_DIM = 64
FOX_HEADS = GROUP_W // HEAD_DIM
CONV_WIDTH = 31
LRU_BLOCKS = 4
LRU_CONV_WIDTH = 4
LRU_C = 8.0
DSA_HEADS = GROUP_W // HEAD_DIM
DSA_LATENT = 128
IDX_HEADS = 8
IDX_DIM = 32
TOPK_MAX = 256
D_FF = 2560
RMS_EPS = 1e-6
LN_EPS = 1e-5

SPLIT_SIZES = (
    GROUP_W, GROUP_W, GROUP_W, FOX_HEADS,
    2 * GROUP_W,
    GROUP_W, GROUP_W,
    DSA_HEADS * HEAD_DIM, DSA_LATENT,
    IDX_HEADS * IDX_DIM, IDX_DIM, IDX_HEADS,
)
D_IN = sum(SPLIT_SIZES)

kernel_name = "hymba_fox_conformer_rglru_dsa_trunk"


def rms_norm(x, g):
    xf = x.astype(jnp.float32)
    y = xf * lax.rsqrt(jnp.mean(xf * xf, axis=-1, keepdims=True) + RMS_EPS)
    return (y * g.astype(jnp.float32)).astype(x.dtype)


def layer_norm(x, g, b):
    xf = x.astype(jnp.float32)
    mu = jnp.mean(xf, axis=-1, keepdims=True)
    var = jnp.mean(jnp.square(xf - mu), axis=-1, keepdims=True)
    y = (xf - mu) * lax.rsqrt(var + LN_EPS) * g.astype(jnp.float32) + b.astype(jnp.float32)
    return y.astype(x.dtype)


def swiglu(x, w_in, w_out):
    gate, up = jnp.split(x @ w_in, 2, axis=-1)
    return (jax.nn.silu(gate) * up) @ w_out


def causal_depthwise_conv(x, w, b):
    width = w.shape[0]
    y = lax.conv_general_dilated(
        x, w[:, None, :].astype(x.dtype), window_strides=(1,),
        padding=[(width - 1, 0)], dimension_numbers=("NWC", "WIO", "NWC"),
        feature_group_count=x.shape[-1])
    return y + b.astype(x.dtype)


def block_sweep(fn, per_query):
    b, t = per_query[0].shape[:2]
    n_blk = (t - N_META) // BLOCK_Q
    out_meta = fn(tuple(a[:, :N_META] for a in per_query), jnp.arange(N_META))
    real = tuple(a[:, N_META:].reshape((b, n_blk, BLOCK_Q) + a.shape[2:]).swapaxes(0, 1)
                 for a in per_query)
    pos = (N_META + jnp.arange(n_blk * BLOCK_Q)).reshape(n_blk, BLOCK_Q)
    out_real = lax.map(lambda qp: fn(qp[0], qp[1]), (real, pos))
    out_real = out_real.swapaxes(0, 1).reshape((b, n_blk * BLOCK_Q) + out_real.shape[3:])
    return jnp.concatenate([out_meta, out_real], axis=1)


def forgetting_attention(q, k, v, f_logit, b_f):
    log_f = jax.nn.log_sigmoid(f_logit.astype(jnp.float32) + b_f.astype(jnp.float32))
    cum = jnp.cumsum(log_f, axis=1)
    cum_k = cum.transpose(0, 2, 1)
    key_pos = jnp.arange(q.shape[1])
    scale = HEAD_DIM ** -0.5

    def attend(qa, q_pos):
        q_blk, cum_q = qa
        s = jnp.einsum("bqhd,bkhd->bhqk", q_blk, k).astype(jnp.float32) * scale
        s = s + cum_q.transpose(0, 2, 1)[..., None] - cum_k[:, :, None, :]
        s = jnp.where(key_pos[None, None, None, :] <= q_pos[None, None, :, None], s, -jnp.inf)
        p = jax.nn.softmax(s, axis=-1).astype(v.dtype)
        return jnp.einsum("bhqk,bkhd->bqhd", p, v)

    return block_sweep(attend, (q, cum))


def conformer_conv(u, dw_w, dw_b, ln_g, ln_b):
    a, g = jnp.split(u, 2, axis=-1)
    h = a * jax.nn.sigmoid(g)
    h = causal_depthwise_conv(h, dw_w, dw_b)
    h = layer_norm(h, ln_g, ln_b)
    return jax.nn.silu(h)


def rg_lru_branch(xb, gb, conv_w, conv_b, w_a, b_a, w_i, b_i, lam):
    xc = causal_depthwise_conv(xb, conv_w, conv_b)
    bsz, t, w = xc.shape
    xh = xc.reshape(bsz, t, LRU_BLOCKS, w // LRU_BLOCKS)
    r = jax.nn.sigmoid(jnp.einsum("btnc,ncd->btnd", xh, w_a).reshape(bsz, t, w) + b_a)
    i = jax.nn.sigmoid(jnp.einsum("btnc,ncd->btnd", xh, w_i).reshape(bsz, t, w) + b_i)
    log_a = LRU_C * r.astype(jnp.float32) * jax.nn.log_sigmoid(lam.astype(jnp.float32))
    a = jnp.exp(log_a)
    u = jnp.sqrt(-jnp.expm1(2.0 * log_a)) * (i.astype(jnp.float32) * xc.astype(jnp.float32))

    def combine(left, right):
        a1, b1 = left
        a2, b2 = right
        return a1 * a2, a2 * b1 + b2

    _, h = lax.associative_scan(combine, (a, u), axis=1)
    return h.astype(xb.dtype) * jax.nn.gelu(gb)


def dsa_attention(q, c_kv, q_idx, k_idx, w_idx, kv_g, w_uk, w_uv, k_ln_g, k_ln_b, topk):
    c = rms_norm(c_kv, kv_g)
    k_i = layer_norm(k_idx, k_ln_g, k_ln_b)
    q_lat = jnp.einsum("bthd,hcd->bthc", q, w_uk)
    w_h = w_idx.astype(jnp.float32) * (IDX_HEADS ** -0.5 * IDX_DIM ** -0.5)
    bsz, t = c.shape[:2]
    key_pos = jnp.arange(t)
    bidx = jnp.arange(bsz)[:, None, None]
    scale = HEAD_DIM ** -0.5

    def attend(qa, q_pos):
        ql, qi, wh = qa
        sc = jax.nn.relu(jnp.einsum("bqhd,bkd->bqhk", qi, k_i).astype(jnp.float32))
        sc = jnp.einsum("bqhk,bqh->bqk", sc, wh)
        sc = jnp.where(key_pos[None, None, :] <= q_pos[None, :, None], sc, -jnp.inf)
        _, sel = lax.top_k(sc, topk)
        c_sel = c[bidx, sel]
        s = jnp.einsum("bqhc,bqkc->bhqk", ql, c_sel).astype(jnp.float32) * scale
        s = jnp.where((sel <= q_pos[None, :, None])[:, None], s, -jnp.inf)
        p = jax.nn.softmax(s, axis=-1).astype(c.dtype)
        return jnp.einsum("bhqk,bqkc->bqhc", p, c_sel)

    o_lat = block_sweep(attend, (q_lat, q_idx, w_h))
    return jnp.einsum("bthc,hcd->bthd", o_lat, w_uv)


def setup_inputs(seed: int = 0) -> dict:
    key = jax.random.key(seed)
    ks = jax.random.split(key, 32)
    f32 = jnp.float32
    nrm = lambda k, shape, s: jax.random.normal(k, shape, f32) * s
    u = jax.random.uniform(ks[20], (DEPTH, GROUP_W), f32, 0.9, 0.999)
    base = u ** (1.0 / LRU_C)
    lam = jnp.log(base) - jnp.log1p(-base)
    return {
        "x": nrm(ks[0], (BATCH, SEQ, D_MODEL), 1.0),
        "meta_tokens": nrm(ks[1], (N_META, D_MODEL), 1.0),
        "norm_g": 1.0 + nrm(ks[2], (DEPTH, 6, D_MODEL), 0.05),
        "ffn_w_in": nrm(ks[3], (DEPTH, 2, D_MODEL, 2 * D_FF), D_MODEL ** -0.5),
        "ffn_w_out": nrm(ks[4], (DEPTH, 2, D_FF, D_MODEL), D_FF ** -0.5),
        "w_in": nrm(ks[5], (DEPTH, D_MODEL, D_IN), D_MODEL ** -0.5),
        "w_out": nrm(ks[6], (DEPTH, D_MIX, D_MODEL), D_MIX ** -0.5),
        "fox_b_f": 4.0 + nrm(ks[7], (DEPTH, FOX_HEADS), 0.5),
        "conv_dw_w": nrm(ks[8], (DEPTH, CONV_WIDTH, GROUP_W), CONV_WIDTH ** -0.5),
        "conv_dw_b": nrm(ks[9], (DEPTH, GROUP_W), 0.02),
        "conv_ln_g": 1.0 + nrm(ks[10], (DEPTH, GROUP_W), 0.05),
        "conv_ln_b": nrm(ks[11], (DEPTH, GROUP_W), 0.02),
        "lru_conv_w": nrm(ks[12], (DEPTH, LRU_CONV_WIDTH, GROUP_W), LRU_CONV_WIDTH ** -0.5),
        "lru_conv_b": nrm(ks[13], (DEPTH, GROUP_W), 0.02),
        "lru_w_a": nrm(ks[14], (DEPTH, LRU_BLOCKS, GROUP_W // LRU_BLOCKS, GROUP_W // LRU_BLOCKS), (GROUP_W // LRU_BLOCKS) ** -0.5),
        "lru_b_a": nrm(ks[15], (DEPTH, GROUP_W), 0.02),
        "lru_w_i": nrm(ks[16], (DEPTH, LRU_BLOCKS, GROUP_W // LRU_BLOCKS, GROUP_W // LRU_BLOCKS), (GROUP_W // LRU_BLOCKS) ** -0.5),
        "lru_b_i": nrm(ks[17], (DEPTH, GROUP_W), 0.02),
        "lru_lambda": lam,
        "dsa_kv_norm_g": 1.0 + nrm(ks[18], (DEPTH, DSA_LATENT), 0.05),
        "dsa_w_uk": nrm(ks[19], (DEPTH, DSA_HEADS, DSA_LATENT, HEAD_DIM), DSA_LATENT ** -0.5),
        "dsa_w_uv": nrm(ks[21], (DEPTH, DSA_HEADS, DSA_LATENT, HEAD_DIM), DSA_LATENT ** -0.5),
        "idx_k_ln_g": 1.0 + nrm(ks[22], (DEPTH, IDX_DIM), 0.05),
        "idx_k_ln_b": nrm(ks[23], (DEPTH, IDX_DIM), 0.02),
    }


def reference(x, meta_tokens, norm_g, ffn_w_in, ffn_w_out, w_in, w_out, fox_b_f,
              conv_dw_w, conv_dw_b, conv_ln_g, conv_ln_b,
              lru_conv_w, lru_conv_b, lru_w_a, lru_b_a, lru_w_i, lru_b_i, lru_lambda,
              dsa_kv_norm_g, dsa_w_uk, dsa_w_uv, idx_k_ln_g, idx_k_ln_b):
    bsz, seq, d = x.shape
    topk = min(TOPK_MAX, seq // 4)
    split_at = np.cumsum(SPLIT_SIZES)[:-1].tolist()
    meta = jnp.broadcast_to(meta_tokens[None].astype(x.dtype), (bsz, N_META, d))
    h = jnp.concatenate([meta, x], axis=1)
    t = h.shape[1]
    heads = lambda a, n: a.reshape(bsz, t, n, -1)

    for l in range(DEPTH):
        g = norm_g[l]
        h = h + 0.5 * rms_norm(swiglu(rms_norm(h, g[0]), ffn_w_in[l, 0], ffn_w_out[l, 0]), g[1])

        z = rms_norm(h, g[2]) @ w_in[l]
        fq, fk, fv, ff, cu, lx, lg, dq, dkv, iq, ik, iw = jnp.split(z, split_at, axis=-1)
        y_fox = forgetting_attention(heads(fq, FOX_HEADS), heads(fk, FOX_HEADS),
                                     heads(fv, FOX_HEADS), ff, fox_b_f[l]).reshape(bsz, t, GROUP_W)
        y_conv = conformer_conv(cu, conv_dw_w[l], conv_dw_b[l], conv_ln_g[l], conv_ln_b[l])
        y_lru = rg_lru_branch(lx, lg, lru_conv_w[l], lru_conv_b[l], lru_w_a[l], lru_b_a[l],
                              lru_w_i[l], lru_b_i[l], lru_lambda[l])
        y_dsa = dsa_attention(heads(dq, DSA_HEADS), dkv, heads(iq, IDX_HEADS), ik, iw,
                              dsa_kv_norm_g[l], dsa_w_uk[l], dsa_w_uv[l],
                              idx_k_ln_g[l], idx_k_ln_b[l], topk).reshape(bsz, t, GROUP_W)
        mix = jnp.concatenate([y_fox, y_conv, y_lru, y_dsa], axis=-1) @ w_out[l]
        h = h + rms_norm(mix, g[3])

        h = h + 0.5 * rms_norm(swiglu(rms_norm(h, g[4]), ffn_w_in[l, 1], ffn_w_out[l, 1]), g[5])

    return h[:, N_META:]
```

```python
import numpy as np
from contextlib import ExitStack
import concourse.bass as bass
import concourse.mybir as mybir
from concourse.bass_utils import run_bass_kernel_spmd

F32 = mybir.dt.float32
BF16 = mybir.dt.bfloat16
I32 = mybir.dt.int32
AF = mybir.ActivationFunctionType
ALU = mybir.AluOpType
AX = mybir.AxisListType

D = 1024
DFF = 2560
KC = D // 128
JC = DFF // 128
DIN = 2476
NCORES = 8
NMETA = 16
SEQ = 8192
T_FULL = SEQ + NMETA
CH = 114
GRP = 4 * CH


class Dep:
    __slots__ = ("w", "r")

    def __init__(self):
        self.w = None
        self.r = {}


class Sched:
    def __init__(self, nc, es, n_dma_sems=24):
        self.nc = nc
        self.es = es
        self.eng = {"pe": nc.tensor, "act": nc.scalar, "dve": nc.vector,
                    "pool": nc.gpsimd, "sp": nc.sync}
        self.sems = {}
        for k in self.eng:
            self.sems[k] = es.enter_context(nc.semaphore("c_" + k))
        self.cnt = {k: 0 for k in self.eng}
        self.seen = {k: {} for k in self.eng}
        self.dpool = []
        for i in range(n_dma_sems):
            key = "d%d" % i
            self.sems[key] = es.enter_context(nc.semaphore(key))
            self.dpool.append([key, 0])
        self.dnext = 0
        self.nwaits = 0
        self.nops = 0
        self._uid = 0

    def sb(self, name, shape, dtype, es=None):
        self._uid += 1
        return (es or self.es).enter_context(
            self.nc.sbuf_tensor("sb%d_%s" % (self._uid, name), list(shape), dtype))

    def ps(self, name, shape, dtype=F32, es=None):
        self._uid += 1
        return (es or self.es).enter_context(
            self.nc.psum_tensor("ps%d_%s" % (self._uid, name), list(shape), dtype))

    def _wait(self, eng, ev):
        if ev is None:
            return
        key, val = ev
        if key == eng and eng in ("pe", "sp"):
            return
        if self.seen[eng].get(key, 0) >= val:
            return
        self.eng[eng].wait_ge(self.sems[key], val)
        self.seen[eng][key] = val
        self.nwaits += 1

    def _deps(self, eng, r, w):
        for d in r:
            self._wait(eng, d.w)
        for d in w:
            self._wait(eng, d.w)
            for e, ev in d.r.items():
                if e != eng:
                    self._wait(eng, ev)

    def op(self, eng, fn, r=(), w=()):
        self._deps(eng, r, w)
        ins = fn(self.eng[eng])
        self.cnt[eng] += 1
        ins.then_inc(self.sems[eng], 1)
        ev = (eng, self.cnt[eng])
        for d in r:
            d.r[eng] = ev
        for d in w:
            d.w = ev
            d.r = {}
        self.nops += 1
        return ev

    def dma(self, out, in_, r=(), w=(), q="sp", **kw):
        self._deps(q, r, w)
        slot = self.dpool[self.dnext]
        self.dnext = (self.dnext + 1) % len(self.dpool)
        key, val = slot
        if val > 0:
            self._wait(q, (key, val))
        ins = self.eng[q].dma_start(out=out, in_=in_, **kw)
        ins.then_inc(self.sems[key], 16)
        slot[1] = val + 16
        ev = (key, val + 16)
        for d in r:
            d.r[key] = ev
        for d in w:
            d.w = ev
            d.r = {}
        self.nops += 1
        return ev

    def barrier(self):
        for e in ("pe", "act", "dve", "pool", "sp"):
            for key, val in self.dpool:
                if val > 0:
                    self._wait(e, (key, val))
            for k in ("pe", "act", "dve", "pool"):
                if k != e and self.cnt[k] > 0:
                    self._wait(e, (k, self.cnt[k]))
            if e not in ("pe", "sp") and self.cnt[e] > 0:
                self._wait(e, (e, self.cnt[e]))

    def finish(self):
        for key, val in self.dpool:
            if val > 0:
                self._wait("sp", (key, val))
        for k in ("pe", "act", "dve", "pool"):
            if self.cnt[k] > 0:
                self._wait("sp", (k, self.cnt[k]))


class TStage:
    def __init__(self, S, NT):
        self.S = S
        self.NT = NT
        self.ones = S.sb("ones", [128, 128], F32)
        self.ones_d = Dep()
        S.op("dve", lambda E: E.memset(self.ones[:], 1.0), w=[self.ones_d])
        self.eps = S.sb("eps", [128, 1], F32)
        self.eps_d = Dep()
        S.op("dve", lambda E: E.memset(self.eps[:], 1e-6), w=[self.eps_d])
        self.win = S.sb("win", [128, KC, 2 * DFF], BF16)
        self.win_d = Dep()
        self.wout = S.sb("wout", [128, JC, D], BF16)
        self.wout_d = Dep()
        self.stages = [(S.sb("stg%d" % i, [128, 1280], F32), Dep()) for i in range(3)]
        self.gt = S.sb("gt", [128, 6 * KC], F32)
        self.gt_d = Dep()
        self.x32 = [(S.sb("x32_%d" % i, [128, KC, NT], F32), Dep()) for i in range(2)]
        self.sq = (S.sb("sq", [128, KC, NT], F32), Dep())
        self.xn = (S.sb("xn", [128, KC, NT], BF16), Dep())
        self.act = (S.sb("actb", [128, JC, NT], BF16), Dep())
        self.sg = [(S.sb("sg%d" % i, [128, NT], F32), Dep()) for i in range(2)]
        self.y32 = (S.sb("y32", [128, KC, NT], F32), Dep())
        self.rstd = (S.sb("rstd", [128, NT], F32), Dep())
        self.tmp = [(S.sb("tmp%d" % i, [128, NT], F32), Dep()) for i in range(2)]
        self.psum = [(S.ps("ps%d" % i, [128, 512]), Dep()) for i in range(8)]
        self.pi = 0
        self.lc = 0
        self.ddeps = {}

    def ddep(self, key, t0):
        k = (key, t0)
        if k not in self.ddeps:
            self.ddeps[k] = Dep()
        return self.ddeps[k]

    def nextps(self):
        p = self.psum[self.pi % 8]
        self.pi += 1
        return p

    def load_cast(self, dst, dst_dep, src_dram, F, scale=None, scale_dep=None):
        S = self.S
        i = self.lc
        self.lc += 1
        st, sd = self.stages[i % len(self.stages)]
        S.dma(st[:, 0:F], src_dram, w=[sd])
        e = ("act", "dve", "pool")[i % 3]
        r = [sd] + ([scale_dep] if scale_dep is not None else [])
        if scale is None:
            if e == "act":
                S.op(e, lambda E: E.copy(out=dst, in_=st[:, 0:F]), r=r, w=[dst_dep])
            else:
                S.op(e, lambda E: E.tensor_copy(out=dst, in_=st[:, 0:F]), r=r, w=[dst_dep])
        else:
            if e == "act":
                S.op(e, lambda E: E.activation(out=dst, in_=st[:, 0:F], func=AF.Copy, scale=scale), r=r, w=[dst_dep])
            else:
                S.op(e, lambda E: E.tensor_scalar(out=dst, in0=st[:, 0:F], scalar1=scale, scalar2=None, op0=ALU.mult),
                     r=r, w=[dst_dep])

    def rms_rstd(self, src, src_d, n, out_scale=1.0):
        S = self.S
        sq, sq_d = self.sq
        S.op("act", lambda E: E.activation(out=sq[:, :, 0:n], in_=src[:, :, 0:n], func=AF.Square), r=[src_d], w=[sq_d])
        ps, ps_d = self.nextps()
        for c in range(KC):
            S.op("pe", lambda E: E.matmul(ps[:, 0:n], self.ones[:], sq[:, c, 0:n], start=(c == 0), stop=(c == KC - 1)),
                 r=[self.ones_d, sq_d], w=[ps_d])
        rs, rs_d = self.rstd
        S.op("act", lambda E: E.activation(out=rs[:, 0:n], in_=ps[:, 0:n], func=AF.Sqrt, scale=1.0 / D, bias=self.eps[:, 0:1]),
             r=[ps_d, self.eps_d], w=[rs_d])
        S.op("dve", lambda E: E.reciprocal(out=rs[:, 0:n], in_=rs[:, 0:n]), r=[rs_d], w=[rs_d])
        if out_scale != 1.0:
            S.op("dve", lambda E: E.tensor_scalar(out=rs[:, 0:n], in0=rs[:, 0:n], scalar1=float(out_scale), scalar2=None, op0=ALU.mult),
                 r=[rs_d], w=[rs_d])
        return rs, rs_d

    def load_gains(self, g_dram):
        self.S.dma(self.gt[:], g_dram, w=[self.gt_d])

    def gcol(self, gi, c):
        return self.gt[:, gi * KC + c:gi * KC + c + 1]

    def load_ffn_weights(self, win_dram, wout_dram, gi):
        for c in range(KC):
            for hh in range(4):
                self.load_cast(self.win[:, c, hh * 1280:(hh + 1) * 1280], self.win_d,
                               win_dram[:, c, hh * 1280:(hh + 1) * 1280], 1280,
                               scale=self.gcol(gi, c), scale_dep=self.gt_d)
        for j in range(JC):
            self.load_cast(self.wout[:, j, :], self.wout_d, wout_dram[:, j, :], D)

    def load_sq_weights(self, w_dram, ncols, gi=None):
        for c in range(KC):
            for f0 in range(0, ncols, 1280):
                f = min(1280, ncols - f0)
                self.load_cast(self.win[:, c, f0:f0 + f], self.win_d, w_dram[:, c, f0:f0 + f], f,
                               scale=(self.gcol(gi, c) if gi is not None else None),
                               scale_dep=(self.gt_d if gi is not None else None))

    def normalize(self, x32, x_d, n):
        S = self.S
        rs, rs_d = self.rms_rstd(x32, x_d, n)
        xn, xn_d = self.xn
        for c in range(KC):
            e = "dve" if c % 2 == 0 else "pool"
            S.op(e, lambda E: E.tensor_tensor(out=xn[:, c, 0:n], in0=x32[:, c, 0:n], in1=rs[:, 0:n], op=ALU.mult),
                 r=[x_d, rs_d], w=[xn_d])
        return xn, xn_d

    def add_normed(self, x32, x_d, n, gpost, half):
        S = self.S
        y32, y_d = self.y32
        rs2, rs2_d = self.rms_rstd(y32, y_d, n, out_scale=half)
        for c in range(KC):
            tp, tp_d = self.tmp[c % 2]
            e = "dve" if c % 2 == 0 else "pool"
            S.op("dve", lambda E: E.scalar_tensor_tensor(out=tp[:, 0:n], in0=y32[:, c, 0:n], scalar=self.gcol(gpost, c),
                                                         in1=rs2[:, 0:n], op0=ALU.mult, op1=ALU.mult),
                 r=[y_d, rs2_d, self.gt_d], w=[tp_d])
            S.op(e, lambda E: E.tensor_tensor(out=x32[:, c, 0:n], in0=tp[:, 0:n], in1=x32[:, c, 0:n], op=ALU.add),
                 r=[tp_d, x_d], w=[x_d])

    def ffn_compute(self, x32, x_d, n, gpost):
        S = self.S
        xn, xn_d = self.normalize(x32, x_d, n)
        act, act_d = self.act
        for j in range(JC):
            pg, pg_d = self.nextps()
            for c in range(KC):
                S.op("pe", lambda E: E.matmul(pg[:, 0:n], self.win[:, c, j * 128:(j + 1) * 128], xn[:, c, 0:n],
                                              start=(c == 0), stop=(c == KC - 1)),
                     r=[self.win_d, xn_d], w=[pg_d])
            pu, pu_d = self.nextps()
            for c in range(KC):
                S.op("pe", lambda E: E.matmul(pu[:, 0:n], self.win[:, c, DFF + j * 128:DFF + (j + 1) * 128], xn[:, c, 0:n],
                                              start=(c == 0), stop=(c == KC - 1)),
                     r=[self.win_d, xn_d], w=[pu_d])
            sg, sg_d = self.sg[j % 2]
            S.op("act", lambda E: E.activation(out=sg[:, 0:n], in_=pg[:, 0:n], func=AF.Silu), r=[pg_d], w=[sg_d])
            S.op("dve", lambda E: E.tensor_tensor(out=act[:, j, 0:n], in0=sg[:, 0:n], in1=pu[:, 0:n], op=ALU.mult),
                 r=[sg_d, pu_d], w=[act_d])
        y32, y_d = self.y32
        for c in range(KC):
            po, po_d = self.nextps()
            for j in range(JC):
                S.op("pe", lambda E: E.matmul(po[:, 0:n], self.wout[:, j, c * 128:(c + 1) * 128], act[:, j, 0:n],
                                              start=(j == 0), stop=(j == JC - 1)),
                     r=[self.wout_d, act_d], w=[po_d])
            S.op("act", lambda E: E.copy(out=y32[:, c, 0:n], in_=po[:, 0:n]), r=[po_d], w=[y_d])
        self.add_normed(x32, x_d, n, gpost, 0.5)

    def ffn_phase(self, h_in, h_out, ntok, gpost, kin="hin", kout="hout"):
        S = self.S
        it = 0
        for t0 in range(0, ntok, self.NT):
            n = min(self.NT, ntok - t0)
            x32, x_d = self.x32[it % 2]
            S.dma(x32[:, :, 0:n], h_in[:, t0:t0 + n].rearrange("(c p) n -> p c n", p=128), r=[self.ddep(kin, t0)], w=[x_d])
            self.ffn_compute(x32, x_d, n, gpost)
            S.dma(h_out[:, t0:t0 + n].rearrange("(c p) n -> p c n", p=128), x32[:, :, 0:n], r=[x_d], w=[self.ddep(kout, t0)])
            it += 1

    def proj_phase(self, h_in, z_out, ntok, ncols, kin="hout"):
        S = self.S
        it = 0
        for t0 in range(0, ntok, self.NT):
            n = min(self.NT, ntok - t0)
            x32, x_d = self.x32[it % 2]
            S.dma(x32[:, :, 0:n], h_in[:, t0:t0 + n].rearrange("(c p) n -> p c n", p=128), r=[self.ddep(kin, t0)], w=[x_d])
            xn, xn_d = self.normalize(x32, x_d, n)
            k = 0
            for m0 in range(0, ncols, 128):
                m = min(128, ncols - m0)
                pz, pz_d = self.nextps()
                for c in range(KC):
                    S.op("pe", lambda E: E.matmul(pz[0:m, 0:n], self.win[:, c, m0:m0 + m], xn[:, c, 0:n],
                                                  start=(c == 0), stop=(c == KC - 1)),
                         r=[self.win_d, xn_d], w=[pz_d])
                zs, zs_d = self.sg[k % 2]
                if k % 2 == 0:
                    S.op("act", lambda E: E.copy(out=zs[0:m, 0:n], in_=pz[0:m, 0:n]), r=[pz_d], w=[zs_d])
                else:
                    S.op("dve", lambda E: E.tensor_copy(out=zs[0:m, 0:n], in_=pz[0:m, 0:n]), r=[pz_d], w=[zs_d])
                S.dma(z_out[m0:m0 + m, t0:t0 + n], zs[0:m, 0:n], r=[zs_d])
                k += 1
            it += 1

    def mixin_phase(self, h_in, y_in, h_out, ntok, gpost, kout="hmid"):
        S = self.S
        it = 0
        for t0 in range(0, ntok, self.NT):
            n = min(self.NT, ntok - t0)
            x32, x_d = self.x32[it % 2]
            S.dma(x32[:, :, 0:n], h_in[:, t0:t0 + n].rearrange("(c p) n -> p c n", p=128), w=[x_d])
            sq, sq_d = self.sq
            S.dma(sq[:, :, 0:n], y_in[:, t0:t0 + n].rearrange("(c p) n -> p c n", p=128), w=[sq_d])
            xn, xn_d = self.xn
            S.op("dve", lambda E: E.tensor_copy(out=xn[:, :, 0:n], in_=sq[:, :, 0:n]), r=[sq_d], w=[xn_d])
            y32, y_d = self.y32
            for c in range(KC):
                po, po_d = self.nextps()
                for k in range(KC):
                    S.op("pe", lambda E: E.matmul(po[:, 0:n], self.win[:, k, c * 128:(c + 1) * 128], xn[:, k, 0:n],
                                                  start=(k == 0), stop=(k == KC - 1)),
                         r=[self.win_d, xn_d], w=[po_d])
                S.op("act", lambda E: E.copy(out=y32[:, c, 0:n], in_=po[:, 0:n]), r=[po_d], w=[y_d])
            self.add_normed(x32, x_d, n, gpost, 1.0)
            S.dma(h_out[:, t0:t0 + n].rearrange("(c p) n -> p c n", p=128), x32[:, :, 0:n], r=[x_d], w=[self.ddep(kout, t0)])
            it += 1


def build_T1(ntok, NT=342):
    nc = bass.Bass("TRN2", target_bir_lowering=False)
    h_in = nc.dram_tensor("h_in", [D, ntok], F32, kind="ExternalInput").ap()
    g_in = nc.dram_tensor("g_in", [128, 6 * KC], F32, kind="ExternalInput").ap()
    fwin = nc.dram_tensor("fwin", [128, KC, 2 * DFF], F32, kind="ExternalInput").ap()
    fwout = nc.dram_tensor("fwout", [128, JC, D], F32, kind="ExternalInput").ap()
    pw = nc.dram_tensor("pw", [128, KC, DIN], F32, kind="ExternalInput").ap()
    h_out = nc.dram_tensor("h_out", [D, ntok], F32, kind="ExternalOutput").ap()
    z_out = nc.dram_tensor("z_out", [DIN, ntok], F32, kind="ExternalOutput").ap()
    with ExitStack() as es:
        S = Sched(nc, es)
        T = TStage(S, NT)
        T.load_gains(g_in)
        T.load_ffn_weights(fwin, fwout, 0)
        T.ffn_phase(h_in, h_out, ntok, 1)
        T.load_sq_weights(pw, DIN, gi=2)
        T.proj_phase(h_out, z_out, ntok, DIN)
        S.finish()
    return nc


def build_T2(ntok, NT=342):
    nc = bass.Bass("TRN2", target_bir_lowering=False)
    h_in = nc.dram_tensor("h_in", [D, ntok], F32, kind="ExternalInput").ap()
    y_in = nc.dram_tensor("y_in", [D, ntok], F32, kind="ExternalInput").ap()
    g_in = nc.dram_tensor("g_in", [128, 6 * KC], F32, kind="ExternalInput").ap()
    ow = nc.dram_tensor("ow", [128, KC, D], F32, kind="ExternalInput").ap()
    fwin = nc.dram_tensor("fwin", [128, KC, 2 * DFF], F32, kind="ExternalInput").ap()
    fwout = nc.dram_tensor("fwout", [128, JC, D], F32, kind="ExternalInput").ap()
    h_mid = nc.dram_tensor("h_mid", [D, ntok], F32, kind="Internal").ap()
    h_out = nc.dram_tensor("h_out", [D, ntok], F32, kind="ExternalOutput").ap()
    with ExitStack() as es:
        S = Sched(nc, es)
        T = TStage(S, NT)
        T.load_gains(g_in)
        T.load_sq_weights(ow, D)
        T.mixin_phase(h_in, y_in, h_mid, ntok, 3)
        T.load_ffn_weights(fwin, fwout, 4)
        T.ffn_phase(h_mid, h_out, ntok, 5, kin="hmid", kout="hout")
        S.finish()
    return nc


def lay_kc(w):
    k = w.shape[0] // 128
    return np.ascontiguousarray(w.reshape(k, 128, w.shape[1]).transpose(1, 0, 2))


def lay_gains(g):
    return np.ascontiguousarray(g.reshape(6, KC, 128).transpose(2, 0, 1).reshape(128, 6 * KC))


def build_fox(T):
    NCH = T // CH
    NQ = T // GRP
    HD = 64
    nc = bass.Bass("TRN2", target_bir_lowering=False)
    q_in = nc.dram_tensor("q_in", [HD, T], F32, kind="ExternalInput").ap()
    k_in = nc.dram_tensor("k_in", [HD, T], F32, kind="ExternalInput").ap()
    v_in = nc.dram_tensor("v_in", [CH, NCH, HD], F32, kind="ExternalInput").ap()
    ff_in = nc.dram_tensor("ff_in", [1, T], F32, kind="ExternalInput").ap()
    bf_in = nc.dram_tensor("bf_in", [1, 1], F32, kind="ExternalInput").ap()
    y_out = nc.dram_tensor("y_out", [HD, T], F32, kind="ExternalOutput").ap()
    with ExitStack() as es:
        S = Sched(nc, es)
        R1 = S.sb("R1", [65, T], F32); R1_d = Dep()
        R2 = S.sb("R2", [65, T], F32); R2_d = Dep()
        qa = S.sb("qa", [65, T], BF16); qa_d = Dep(); qar_d = Dep()
        ka = S.sb("ka", [65, T], BF16); ka_d = Dep(); kar_d = Dep()
        va = S.sb("va", [CH, NCH, HD + 1], BF16); va_d = Dep()
        vst = S.sb("vst", [CH, NCH, HD], F32); vst_d = Dep()
        PIECE = 2052 if T % 2052 == 0 else GRP
        stg = [(S.sb("stg%d" % i, [HD, PIECE], F32), Dep()) for i in range(2)]
        sm = S.sb("sm", [65, 8], F32); sm_d = Dep()
        ones = S.sb("ones", [65, 128], F32); ones_d = Dep()
        cc = S.sb("cc", [CH, NCH + NQ], F32); cc_d = Dep()
        biasT = S.sb("biasT", [CH, NQ, NCH], F32); biasT_d = Dep()
        mf = S.sb("mf", [CH, 4, GRP], F32); mf_d = Dep()
        mask = S.sb("mask", [CH, 4, GRP], BF16); mask_d = Dep()
        pt = [(S.sb("pt%d" % i, [CH, GRP], BF16), Dep()) for i in range(4)]
        osb = [(S.sb("osb%d" % i, [65, GRP], F32), Dep()) for i in range(2)]
        ysb = [(S.sb("ysb%d" % i, [HD, GRP], F32), Dep()) for i in range(2)]
        ps_s = [(S.ps("pss%d" % i, [128, 512]), Dep()) for i in range(3)]
        ps_o = [(S.ps("pso%d" % i, [128, 512]), Dep()) for i in range(2)]
        ps_m = [(S.ps("psm%d" % i, [128, 512]), Dep()) for i in range(2)]

        S.op("dve", lambda E: E.memset(ones[:], 1.0), w=[ones_d])
        S.op("pool", lambda E: E.iota(mf[:], [[-CH, 4], [1, GRP]], base=0, channel_multiplier=-1,
                                      allow_small_or_imprecise_dtypes=True), w=[mf_d])
        S.op("dve", lambda E: E.tensor_scalar(out=mask[:], in0=mf[:], scalar1=0.0, scalar2=None, op0=ALU.is_ge),
             r=[mf_d], w=[mask_d])
        S.dma(R1[64:65, :], ff_in, w=[R1_d])
        S.dma(sm[64:65, 0:1], bf_in, w=[sm_d])
        S.op("dve", lambda E: E.tensor_scalar(out=sm[64:65, 1:2], in0=sm[64:65, 0:1], scalar1=-1.0, scalar2=None, op0=ALU.mult),
             r=[sm_d], w=[sm_d])
        S.op("dve", lambda E: E.memset(sm[64:65, 2:3], 1.0), r=[], w=[sm_d])
        S.op("act", lambda E: E.activation(out=R1[64:65, :], in_=R1[64:65, :], func=AF.Exp, scale=-1.0, bias=sm[64:65, 1:2]),
             r=[R1_d, sm_d], w=[R1_d])
        S.op("act", lambda E: E.activation(out=R1[64:65, :], in_=R1[64:65, :], func=AF.Ln, scale=1.0, bias=sm[64:65, 2:3]),
             r=[R1_d, sm_d], w=[R1_d])
        S.op("dve", lambda E: E.tensor_scalar(out=R1[64:65, :], in0=R1[64:65, :], scalar1=-1.0, scalar2=None, op0=ALU.mult),
             r=[R1_d], w=[R1_d])
        S.op("dve", lambda E: E.tensor_tensor_scan(out=R2[64:65, :], data0=R1[64:65, :], data1=R1[64:65, :], initial=0.0,
                                                   op0=ALU.add, op1=ALU.min),
             r=[R1_d], w=[R2_d])
        i = 0
        for src, dst, dd, sc in ((q_in, qa, qa_d, HD ** -0.5), (k_in, ka, ka_d, 1.0)):
            for p0 in range(0, T, PIECE):
                st, sd = stg[i % 2]
                S.dma(st[:, :], src[:, p0:p0 + PIECE], w=[sd])
                if i % 2 == 0:
                    S.op("act", lambda E: E.mul(out=dst[0:HD, p0:p0 + PIECE], in_=st[:, :], mul=float(sc)), r=[sd], w=[dd])
                else:
                    S.op("dve", lambda E: E.tensor_scalar(out=dst[0:HD, p0:p0 + PIECE], in0=st[:, :], scalar1=float(sc),
                                                          scalar2=None, op0=ALU.mult), r=[sd], w=[dd])
                i += 1
        S.op("pool", lambda E: E.memset(ka[64:65, :], 1.0), w=[kar_d])
        for Q in range(NQ):
            sl = slice(Q * GRP, (Q + 1) * GRP)
            S.op("dve", lambda E: E.tensor_scalar(out=qa[64:65, sl], in0=R2[64:65, sl], scalar1=R2[64:65, Q * GRP:Q * GRP + 1],
                                                  scalar2=None, op0=ALU.subtract), r=[R2_d], w=[qar_d])
        pm, pm_d = ps_m[0]
        for c in range(NCH):
            S.op("pe", lambda E: E.matmul(pm[0:CH, c:c + 1], R2[64:65, c * CH:(c + 1) * CH], ones[64:65, 0:1], start=True, stop=True),
                 r=[R2_d, ones_d], w=[pm_d])
        q0s = bass.AP(R2.tensor if hasattr(R2, "tensor") else R2, 64 * 0, [[1, 1]]) if False else None
        for Q in range(NQ):
            S.op("pe", lambda E: E.matmul(pm[0:CH, NCH + Q:NCH + Q + 1], ones[64:65, 0:CH], R2[64:65, Q * GRP:Q * GRP + 1],
                                          start=True, stop=True), r=[R2_d, ones_d], w=[pm_d])
        S.op("act", lambda E: E.copy(out=cc[:, :], in_=pm[0:CH, 0:NCH + NQ]), r=[pm_d], w=[cc_d])
        for Q in range(NQ):
            S.op("dve", lambda E: E.tensor_scalar(out=biasT[:, Q, :], in0=cc[:, 0:NCH], scalar1=cc[:, NCH + Q:NCH + Q + 1],
                                                  scalar2=-1.0, op0=ALU.subtract, op1=ALU.mult), r=[cc_d], w=[biasT_d])
        S.dma(vst[:], v_in, w=[vst_d])
        S.op("pool", lambda E: E.tensor_copy(out=va[:, :, 0:HD], in_=vst[:]), r=[vst_d], w=[va_d])
        S.op("pool", lambda E: E.memset(va[:, :, HD:HD + 1], 1.0), w=[va_d])

        si = 0
        for Q in range(NQ):
            qsl = slice(Q * GRP, (Q + 1) * GRP)
            po, po_d = ps_o[Q % 2]
            nck = 4 * Q + 4
            pend = None

            def qk(c):
                nonlocal si
                pS, pS_d = ps_s[si % 3]
                ptile, pt_d = pt[si % 4]
                si += 1
                S.op("pe", lambda E: E.matmul(pS[0:CH, 0:GRP], ka[0:65, c * CH:(c + 1) * CH], qa[0:65, qsl], start=True, stop=True),
                     r=[ka_d, kar_d, qa_d, qar_d], w=[pS_d])
                S.op("act", lambda E: E.activation(out=ptile[:, :], in_=pS[0:CH, 0:GRP], func=AF.Exp, scale=1.0,
                                                   bias=biasT[:, Q, c:c + 1]), r=[pS_d, biasT_d], w=[pt_d])
                d = c - 4 * Q
                if d >= 0:
                    S.op("dve", lambda E: E.tensor_tensor(out=ptile[:, :], in0=ptile[:, :], in1=mask[:, d, :], op=ALU.mult),
                         r=[pt_d, mask_d], w=[pt_d])
                return ptile, pt_d

            def pv(c, ptile, pt_d):
                S.op("pe", lambda E: E.matmul(po[0:HD + 1, 0:GRP], va[:, c, :], ptile[:, :], start=(c == 0), stop=(c == nck - 1)),
                     r=[va_d, pt_d], w=[po_d])

            for c in range(nck):
                cur = qk(c)
                if pend is not None:
                    pv(c - 1, *pend)
                pend = cur
            pv(nck - 1, *pend)
            ob, ob_d = osb[Q % 2]
            S.op("act", lambda E: E.copy(out=ob[:, :], in_=po[0:HD + 1, 0:GRP]), r=[po_d], w=[ob_d])
            S.op("dve", lambda E: E.reciprocal(out=ob[64:65, :], in_=ob[64:65, :]), r=[ob_d], w=[ob_d])
            pb, pb_d = ps_m[1]
            S.op("pe", lambda E: E.matmul(pb[0:HD, 0:GRP], ones[64:65, 0:HD], ob[64:65, :], start=True, stop=True),
                 r=[ones_d, ob_d], w=[pb_d])
            yb, yb_d = ysb[Q % 2]
            S.op("dve", lambda E: E.tensor_tensor(out=yb[:, :], in0=ob[0:HD, :], in1=pb[0:HD, 0:GRP], op=ALU.mult),
                 r=[ob_d, pb_d], w=[yb_d])
            S.dma(y_out[:, qsl], yb[:, :], r=[yb_d])
        S.finish()
        print("fox ops", S.nops, "waits", S.nwaits)
    return nc


CW = 31
HAL = CW - 1
GW = 256


def build_cl(T, NTK):
    SEG = 2052 if T % 2052 == 0 else 456
    SUB = 342 if SEG % 342 == 0 else 228
    NT = 342 if NTK % 342 == 0 else 228
    nc = bass.Bass("TRN2", target_bir_lowering=False)
    cu_in = nc.dram_tensor("cu_in", [2 * GW, HAL + NTK], F32, kind="ExternalInput").ap()
    cw_in = nc.dram_tensor("cw_in", [128, 2, CW], F32, kind="ExternalInput").ap()
    cv_in = nc.dram_tensor("cv_in", [128, 2, 3], F32, kind="ExternalInput").ap()
    lx_in = nc.dram_tensor("lx_in", [64, 3 + T], F32, kind="ExternalInput").ap()
    lg_in = nc.dram_tensor("lg_in", [64, T], F32, kind="ExternalInput").ap()
    lcw_in = nc.dram_tensor("lcw_in", [64, 4], F32, kind="ExternalInput").ap()
    lv_in = nc.dram_tensor("lv_in", [64, 4], F32, kind="ExternalInput").ap()
    lwa_in = nc.dram_tensor("lwa_in", [64, 64], F32, kind="ExternalInput").ap()
    lwi_in = nc.dram_tensor("lwi_in", [64, 64], F32, kind="ExternalInput").ap()
    yc_out = nc.dram_tensor("yc_out", [GW, NTK], F32, kind="ExternalOutput").ap()
    yl_out = nc.dram_tensor("yl_out", [64, T], F32, kind="ExternalOutput").ap()
    with ExitStack() as es:
        S = Sched(nc, es)
        psum = [(S.ps("ps%d" % i, [128, 512]), Dep()) for i in range(6)]
        pi = [0]

        def nextps():
            p = psum[pi[0] % len(psum)]
            pi[0] += 1
            return p

        cst = S.sb("cst", [128, 4], F32); cst_d = Dep()
        S.op("dve", lambda E: E.memset(cst[:, 0:1], 1e-5), w=[cst_d])
        S.op("dve", lambda E: E.memset(cst[:, 1:2], 1.0), w=[cst_d])
        o256 = S.sb("o256", [128, 128], F32); o256_d = Dep()
        S.op("dve", lambda E: E.memset(o256[:], 1.0 / GW), w=[o256_d])

        with ExitStack() as es2:
            A = S.sb("A", [128, 2, HAL + NTK], F32, es2); A_d = [Dep(), Dep()]
            G = S.sb("G", [128, 2, HAL + NTK], F32, es2); G_d = [Dep(), Dep()]
            acc = S.sb("acc", [128, 2, NTK], F32, es2); acc_d = [Dep(), Dep()]
            cw = S.sb("cw", [128, 2, CW], F32, es2); cw_d = Dep()
            cv = S.sb("cv", [128, 2, 3], F32, es2); cv_d = Dep()
            yc = [(S.sb("yc%d" % i, [128, NT], F32, es2), Dep()) for i in range(2)]
            sq = [(S.sb("sqc%d" % i, [128, NT], F32, es2), Dep()) for i in range(2)]
            rs = (S.sb("rsc", [128, NT], F32, es2), Dep())
            yo = [(S.sb("yo%d" % i, [128, NT], F32, es2), Dep()) for i in range(2)]
            S.dma(cw[:], cw_in, w=[cw_d])
            S.dma(cv[:], cv_in, w=[cv_d])
            for cc in range(2):
                S.dma(A[:, cc, :], cu_in[cc * 128:(cc + 1) * 128, :], w=[A_d[cc]])
                S.dma(G[:, cc, :], cu_in[GW + cc * 128:GW + (cc + 1) * 128, :], w=[G_d[cc]])
            for cc in range(2):
                e = "dve" if cc == 0 else "pool"
                S.op("act", lambda E: E.activation(out=G[:, cc, :], in_=G[:, cc, :], func=AF.Sigmoid), r=[G_d[cc]], w=[G_d[cc]])
                S.op(e, lambda E: E.tensor_tensor(out=A[:, cc, :], in0=A[:, cc, :], in1=G[:, cc, :], op=ALU.mult),
                     r=[A_d[cc], G_d[cc]], w=[A_d[cc]])
            for k in range(CW):
                for cc in range(2):
                    e = "dve"
                    if k == 0:
                        S.op(e, lambda E: E.tensor_scalar(out=acc[:, cc, :], in0=A[:, cc, 0:NTK], scalar1=cw[:, cc, 0:1],
                                                          scalar2=cv[:, cc, 0:1], op0=ALU.mult, op1=ALU.add),
                             r=[A_d[cc], cw_d, cv_d], w=[acc_d[cc]])
                    else:
                        S.op(e, lambda E: E.scalar_tensor_tensor(out=acc[:, cc, :], in0=A[:, cc, k:k + NTK], scalar=cw[:, cc, k:k + 1],
                                                                 in1=acc[:, cc, :], op0=ALU.mult, op1=ALU.add),
                             r=[A_d[cc], cw_d, acc_d[cc]], w=[acc_d[cc]])
            it = 0
            for t0 in range(0, NTK, NT):
                n = min(NT, NTK - t0)
                pm, pm_d = nextps()
                for cc in range(2):
                    S.op("pe", lambda E: E.matmul(pm[:, 0:n], o256[:], acc[:, cc, t0:t0 + n], start=(cc == 0), stop=(cc == 1)),
                         r=[o256_d, acc_d[cc]], w=[pm_d])
                pv, pv_d = nextps()
                for cc in range(2):
                    y_, y_d = yc[cc]
                    s_, s_d = sq[cc]
                    S.op("dve", lambda E: E.tensor_tensor(out=y_[:, 0:n], in0=acc[:, cc, t0:t0 + n], in1=pm[:, 0:n], op=ALU.subtract),
                         r=[acc_d[cc], pm_d], w=[y_d])
                    S.op("act", lambda E: E.activation(out=s_[:, 0:n], in_=y_[:, 0:n], func=AF.Square), r=[y_d], w=[s_d])
                    S.op("pe", lambda E: E.matmul(pv[:, 0:n], o256[:], s_[:, 0:n], start=(cc == 0), stop=(cc == 1)),
                         r=[o256_d, s_d], w=[pv_d])
                r_, r_d = rs
                S.op("act", lambda E: E.activation(out=r_[:, 0:n], in_=pv[:, 0:n], func=AF.Sqrt, scale=1.0, bias=cst[:, 0:1]),
                     r=[pv_d, cst_d], w=[r_d])
                S.op("dve", lambda E: E.reciprocal(out=r_[:, 0:n], in_=r_[:, 0:n]), r=[r_d], w=[r_d])
                for cc in range(2):
                    y_, y_d = yc[cc]
                    o_, o_d = yo[cc]
                    e = "dve" if cc == 0 else "pool"
                    S.op(e, lambda E: E.tensor_tensor(out=y_[:, 0:n], in0=y_[:, 0:n], in1=r_[:, 0:n], op=ALU.mult),
                         r=[y_d, r_d], w=[y_d])
                    S.op("act", lambda E: E.activation(out=o_[:, 0:n], in_=y_[:, 0:n], func=AF.Silu, scale=cv[:, cc, 1:2],
                                                       bias=cv[:, cc, 2:3]), r=[y_d, cv_d], w=[o_d])
                    S.dma(yc_out[cc * 128:(cc + 1) * 128, t0:t0 + n], o_[:, 0:n], r=[o_d])
                it += 1

        S.barrier()
        with ExitStack() as es3:
            X = S.sb("X", [64, 3 + SEG], F32, es3); X_d = Dep()
            Gt = S.sb("Gt", [64, SEG], F32, es3); Gt_d = Dep()
            xc = S.sb("xc", [64, SEG], F32, es3); xc_d = Dep()
            rt = S.sb("rt", [64, SEG], F32, es3); rt_d = Dep()
            itl = S.sb("itl", [64, SEG], F32, es3); it_d = Dep()
            at = S.sb("at", [64, SEG], F32, es3); at_d = Dep()
            ut = S.sb("ut", [64, SEG], F32, es3); ut_d = Dep()
            hs = S.sb("hs", [64, SEG], F32, es3); hs_d = Dep()
            g1 = S.sb("g1", [64, SEG], F32, es3); g1_d = Dep()
            lcw = S.sb("lcw", [64, 4], F32, es3); lcw_d = Dep()
            lv = S.sb("lv", [64, 8], F32, es3); lv_d = Dep()
            lwa = S.sb("lwa", [64, 64], F32, es3); lwa_d = Dep()
            lwi = S.sb("lwi", [64, 64], F32, es3); lwi_d = Dep()
            carry = S.sb("carry", [64, 1], F32, es3); carry_d = Dep()
            S.dma(lcw[:], lcw_in, w=[lcw_d])
            S.dma(lv[:, 0:4], lv_in, w=[lv_d])
            S.dma(lwa[:], lwa_in, w=[lwa_d])
            S.dma(lwi[:], lwi_in, w=[lwi_d])
            S.op("act", lambda E: E.activation(out=lv[:, 4:5], in_=lv[:, 3:4], func=AF.Exp, scale=-1.0), r=[lv_d], w=[lv_d])
            S.op("act", lambda E: E.activation(out=lv[:, 4:5], in_=lv[:, 4:5], func=AF.Ln, scale=1.0, bias=cst[0:64, 1:2]),
                 r=[lv_d, cst_d], w=[lv_d])
            S.op("dve", lambda E: E.tensor_scalar(out=lv[:, 5:6], in0=lv[:, 4:5], scalar1=-16.0, scalar2=None, op0=ALU.mult),
                 r=[lv_d], w=[lv_d])
            S.op("dve", lambda E: E.tensor_scalar(out=lv[:, 4:5], in0=lv[:, 4:5], scalar1=-8.0, scalar2=None, op0=ALU.mult),
                 r=[lv_d], w=[lv_d])
            S.op("dve", lambda E: E.memset(carry[:], 0.0), w=[carry_d])
            for t0 in range(0, T, SEG):
                S.dma(X[:, :], lx_in[:, t0:t0 + 3 + SEG], w=[X_d])
                S.dma(Gt[:, :], lg_in[:, t0:t0 + SEG], w=[Gt_d])
                S.op("dve", lambda E: E.tensor_scalar(out=xc[:, :], in0=X[:, 0:SEG], scalar1=lcw[:, 0:1], scalar2=lv[:, 0:1],
                                                      op0=ALU.mult, op1=ALU.add), r=[X_d, lcw_d, lv_d], w=[xc_d])
                for k in range(1, 4):
                    S.op("dve", lambda E: E.scalar_tensor_tensor(out=xc[:, :], in0=X[:, k:k + SEG], scalar=lcw[:, k:k + 1], in1=xc[:, :],
                                                                 op0=ALU.mult, op1=ALU.add), r=[X_d, lcw_d, xc_d], w=[xc_d])
                for u0 in range(0, SEG, SUB):
                    pa, pa_d = nextps()
                    S.op("pe", lambda E: E.matmul(pa[0:64, 0:SUB], lwa[:, :], xc[:, u0:u0 + SUB], start=True, stop=True),
                         r=[lwa_d, xc_d], w=[pa_d])
                    S.op("act", lambda E: E.activation(out=rt[:, u0:u0 + SUB], in_=pa[0:64, 0:SUB], func=AF.Sigmoid, scale=1.0,
                                                       bias=lv[:, 1:2]), r=[pa_d, lv_d], w=[rt_d])
                    pb, pb_d = nextps()
                    S.op("pe", lambda E: E.matmul(pb[0:64, 0:SUB], lwi[:, :], xc[:, u0:u0 + SUB], start=True, stop=True),
                         r=[lwi_d, xc_d], w=[pb_d])
                    S.op("act", lambda E: E.activation(out=itl[:, u0:u0 + SUB], in_=pb[0:64, 0:SUB], func=AF.Sigmoid, scale=1.0,
                                                       bias=lv[:, 2:3]), r=[pb_d, lv_d], w=[it_d])
                S.op("act", lambda E: E.activation(out=at[:, :], in_=rt[:, :], func=AF.Exp, scale=lv[:, 4:5]), r=[rt_d, lv_d], w=[at_d])
                S.op("act", lambda E: E.activation(out=ut[:, :], in_=rt[:, :], func=AF.Exp, scale=lv[:, 5:6]), r=[rt_d, lv_d], w=[ut_d])
                S.op("pool", lambda E: E.tensor_scalar(out=ut[:, :], in0=ut[:, :], scalar1=-1.0, scalar2=1.0, op0=ALU.mult, op1=ALU.add),
                     r=[ut_d], w=[ut_d])
                S.op("act", lambda E: E.activation(out=ut[:, :], in_=ut[:, :], func=AF.Sqrt), r=[ut_d], w=[ut_d])
                S.op("pool", lambda E: E.tensor_tensor(out=itl[:, :], in0=itl[:, :], in1=xc[:, :], op=ALU.mult), r=[it_d, xc_d], w=[it_d])
                S.op("pool", lambda E: E.tensor_tensor(out=ut[:, :], in0=ut[:, :], in1=itl[:, :], op=ALU.mult), r=[ut_d, it_d], w=[ut_d])
                S.op("dve", lambda E: E.tensor_tensor_scan(out=hs[:, :], data0=at[:, :], data1=ut[:, :], initial=carry[:, 0:1],
                                                           op0=ALU.mult, op1=ALU.add), r=[at_d, ut_d, carry_d], w=[hs_d])
                S.op("dve", lambda E: E.tensor_copy(out=carry[:, :], in_=hs[:, SEG - 1:SEG]), r=[hs_d], w=[carry_d])
                S.op("act", lambda E: E.activation(out=g1[:, :], in_=Gt[:, :], func=AF.Square), r=[Gt_d], w=[g1_d])
                S.op("pool", lambda E: E.tensor_scalar(out=g1[:, :], in0=g1[:, :], scalar1=0.044715, scalar2=1.0, op0=ALU.mult, op1=ALU.add),
                     r=[g1_d], w=[g1_d])
                S.op("pool", lambda E: E.tensor_tensor(out=g1[:, :], in0=g1[:, :], in1=Gt[:, :], op=ALU.mult), r=[g1_d, Gt_d], w=[g1_d])
                S.op("act", lambda E: E.activation(out=g1[:, :], in_=g1[:, :], func=AF.Sigmoid, scale=1.5957691216057308), r=[g1_d], w=[g1_d])
                S.op("pool", lambda E: E.tensor_tensor(out=g1[:, :], in0=g1[:, :], in1=Gt[:, :], op=ALU.mult), r=[g1_d, Gt_d], w=[g1_d])
                S.op("dve", lambda E: E.tensor_tensor(out=hs[:, :], in0=hs[:, :], in1=g1[:, :], op=ALU.mult), r=[hs_d, g1_d], w=[hs_d])
                S.dma(yl_out[:, t0:t0 + SEG], hs[:, :], r=[hs_d])
        S.finish()
        print("cl ops", S.nops, "waits", S.nwaits)
    return nc


NEG = -1.0e30
TOPK = 256


def build_dsa(T, NR=22):
    NCH = T // CH
    NB = NCH // 4
    NQT = NB * CH
    LAT = 128
    nc = bass.Bass("TRN2", target_bir_lowering=False)
    dkv_in = nc.dram_tensor("dkv_in", [CH, NCH, LAT], F32, kind="ExternalInput").ap()
    kvg_in = nc.dram_tensor("kvg_in", [CH, LAT], F32, kind="ExternalInput").ap()
    ik_in = nc.dram_tensor("ik_in", [CH, NCH, 32], F32, kind="ExternalInput").ap()
    ikg_in = nc.dram_tensor("ikg_in", [CH, 2, 32], F32, kind="ExternalInput").ap()
    dq_in = nc.dram_tensor("dq_in", [64, 4, NQT], F32, kind="ExternalInput").ap()
    iq_in = nc.dram_tensor("iq_in", [32, 8, NQT], F32, kind="ExternalInput").ap()
    iw_in = nc.dram_tensor("iw_in", [CH, NB, 8], F32, kind="ExternalInput").ap()
    wuk_in = nc.dram_tensor("wuk_in", [64, 4, LAT], F32, kind="ExternalInput").ap()
    wuv_in = nc.dram_tensor("wuv_in", [LAT, 4, 64], F32, kind="ExternalInput").ap()
    joff_in = nc.dram_tensor("joff_in", [CH, 1], F32, kind="ExternalInput").ap()
    y_out = nc.dram_tensor("y_out", [64, 4, NQT], F32, kind="ExternalOutput").ap()
    with ExitStack() as es:
        S = Sched(nc, es)
        psA = [(S.ps("psA%d" % i, [128, 512]), Dep()) for i in range(3)]
        psT = [(S.ps("psT%d" % i, [128, 1024], BF16), Dep()) for i in range(2)]
        pso = (S.ps("pso", [128, 512]), Dep())
        psd = (S.ps("psd", [128, 512]), Dep())
        ai = [0]
        ti = [0]

        def nextA():
            p = psA[ai[0] % 3]
            ai[0] += 1
            return p

        def nextT():
            p = psT[ti[0] % 2]
            ti[0] += 1
            return p

        cst = S.sb("cst", [128, 4], F32); cst_d = Dep()
        S.op("dve", lambda E: E.memset(cst[:, 0:1], 1e-6), w=[cst_d])
        S.op("dve", lambda E: E.memset(cst[:, 1:2], 1e-5), w=[cst_d])
        idf = S.sb("idf", [128, 128], F32); idf_d = Dep()
        ident = S.sb("ident", [128, 128], BF16); ident_d = Dep()
        S.op("pool", lambda E: E.iota(idf[:], [[1, 128]], base=0, channel_multiplier=-1, allow_small_or_imprecise_dtypes=True), w=[idf_d])
        S.op("dve", lambda E: E.tensor_scalar(out=ident[:], in0=idf[:], scalar1=0.0, scalar2=None, op0=ALU.is_equal), r=[idf_d], w=[ident_d])
        identf = S.sb("identf", [128, 128], F32); identf_d = Dep()
        S.op("dve", lambda E: E.tensor_scalar(out=identf[:], in0=idf[:], scalar1=0.0, scalar2=None, op0=ALU.is_equal), r=[idf_d], w=[identf_d])
        onesb = S.sb("onesb", [128, 128], BF16); onesb_d = Dep()
        S.op("dve", lambda E: E.memset(onesb[:], 1.0), w=[onesb_d])
        joff = S.sb("joff", [CH, 1], F32); joff_d = Dep()
        S.dma(joff[:], joff_in, w=[joff_d])
        negm = S.sb("negm", [CH, GRP], F32); negm_d = Dep()
        S.op("pool", lambda E: E.iota(negm[:], [[1, GRP]], base=0, channel_multiplier=-1, allow_small_or_imprecise_dtypes=True), w=[negm_d])
        S.op("dve", lambda E: E.tensor_scalar(out=negm[:], in0=negm[:], scalar1=joff[:, 0:1], scalar2=NEG, op0=ALU.is_gt, op1=ALU.mult),
             r=[negm_d, joff_d], w=[negm_d])
        ctok = S.sb("ctok", [CH, NCH, LAT], BF16); ctok_d = Dep()
        cT = S.sb("cT", [LAT, T], BF16); cT_d = Dep()
        kiT = S.sb("kiT", [32, T], F32); kiT_d = Dep()
        w16 = S.sb("w16", [CH, NB, 8], F32); w16_d = Dep()
        wuk = S.sb("wuk", [64, 4, LAT], F32); wuk_d = Dep()
        wuv = S.sb("wuv", [LAT, 4, 64], BF16); wuv_d = Dep()

        with ExitStack() as es2:
            dkv = S.sb("dkv", [CH, NCH, LAT], F32, es2); dkv_d = Dep()
            sqd = S.sb("sqd", [CH, NCH, LAT], F32, es2); sqd_d = Dep()
            kvg = S.sb("kvg", [CH, LAT], F32, es2); kvg_d = Dep()
            st1 = S.sb("st1", [CH, NCH], F32, es2); st1_d = Dep()
            ik = S.sb("ik", [CH, NCH, 32], F32, es2); ik_d = Dep()
            ik2 = S.sb("ik2", [CH, NCH, 32], F32, es2); ik2_d = Dep()
            ikb = S.sb("ikb", [CH, NCH, 32], F32, es2); ikb_d = Dep()
            ikg = S.sb("ikg", [CH, 2, 32], F32, es2); ikg_d = Dep()
            st2 = S.sb("st2", [CH, NCH], F32, es2); st2_d = Dep()
            wst = S.sb("wst", [128, 4 * LAT], F32, es2); wst_d = Dep()

            def bc_last(ap2, n):
                return ap2.unsqueeze(2).to_broadcast([ap2.shape[0], ap2.shape[1], n])

            def bc_mid(ap2, a):
                return ap2.unsqueeze(1).to_broadcast([ap2.shape[0], a, ap2.shape[1]])

            S.dma(dkv[:], dkv_in, w=[dkv_d])
            S.dma(kvg[:], kvg_in, w=[kvg_d])
            S.op("dve", lambda E: E.tensor_tensor(out=sqd[:], in0=dkv[:], in1=dkv[:], op=ALU.mult), r=[dkv_d], w=[sqd_d])
            S.op("dve", lambda E: E.tensor_reduce(out=st1[:], in_=sqd[:], axis=AX.X, op=ALU.add), r=[sqd_d], w=[st1_d])
            S.op("act", lambda E: E.activation(out=st1[:], in_=st1[:], func=AF.Sqrt, scale=1.0 / LAT, bias=cst[0:CH, 0:1]),
                 r=[st1_d, cst_d], w=[st1_d])
            S.op("dve", lambda E: E.reciprocal(out=st1[:], in_=st1[:]), r=[st1_d], w=[st1_d])
            S.op("dve", lambda E: E.tensor_tensor(out=sqd[:], in0=dkv[:], in1=bc_last(st1[:, :], LAT), op=ALU.mult),
                 r=[dkv_d, st1_d], w=[sqd_d])
            S.op("dve", lambda E: E.tensor_tensor(out=ctok[:], in0=sqd[:], in1=bc_mid(kvg[:, :], NCH), op=ALU.mult),
                 r=[sqd_d, kvg_d], w=[ctok_d])
            for g0 in range(0, NCH, 4):
                pT, pT_d = nextT()
                for k in range(4):
                    S.op("pe", lambda E: E.transpose(out=pT[0:LAT, k * CH:(k + 1) * CH], in_=ctok[:, g0 + k, :], identity=ident[0:CH, 0:CH]),
                         r=[ctok_d, ident_d], w=[pT_d])
                S.op("act", lambda E: E.copy(out=cT[:, g0 * CH:(g0 + 4) * CH], in_=pT[0:LAT, 0:GRP]), r=[pT_d], w=[cT_d])
            S.dma(ik[:], ik_in, w=[ik_d])
            S.dma(ikg[:], ikg_in, w=[ikg_d])
            S.op("dve", lambda E: E.tensor_reduce(out=st2[:], in_=ik[:], axis=AX.X, op=ALU.add), r=[ik_d], w=[st2_d])
            S.op("dve", lambda E: E.tensor_scalar(out=st2[:], in0=st2[:], scalar1=1.0 / 32, scalar2=None, op0=ALU.mult), r=[st2_d], w=[st2_d])
            S.op("dve", lambda E: E.tensor_tensor(out=ik[:], in0=ik[:], in1=bc_last(st2[:, :], 32), op=ALU.subtract),
                 r=[ik_d, st2_d], w=[ik_d])
            S.op("dve", lambda E: E.tensor_tensor(out=ik2[:], in0=ik[:], in1=ik[:], op=ALU.mult), r=[ik_d], w=[ik2_d])
            S.op("dve", lambda E: E.tensor_reduce(out=st2[:], in_=ik2[:], axis=AX.X, op=ALU.add), r=[ik2_d], w=[st2_d])
            S.op("act", lambda E: E.activation(out=st2[:], in_=st2[:], func=AF.Sqrt, scale=1.0 / 32, bias=cst[0:CH, 1:2]),
                 r=[st2_d, cst_d], w=[st2_d])
            S.op("dve", lambda E: E.reciprocal(out=st2[:], in_=st2[:]), r=[st2_d], w=[st2_d])
            S.op("dve", lambda E: E.tensor_tensor(out=ik[:], in0=ik[:], in1=bc_last(st2[:, :], 32), op=ALU.mult), r=[ik_d, st2_d], w=[ik_d])
            S.op("dve", lambda E: E.tensor_tensor(out=ik[:], in0=ik[:], in1=bc_mid(ikg[:, 0, :], NCH), op=ALU.mult), r=[ik_d, ikg_d], w=[ik_d])
            S.op("dve", lambda E: E.tensor_tensor(out=ikb[:], in0=ik[:], in1=bc_mid(ikg[:, 1, :], NCH), op=ALU.add), r=[ik_d, ikg_d], w=[ikb_d])
            for g0 in range(0, NCH, 4):
                pT, pT_d = nextA()
                for k in range(4):
                    S.op("pe", lambda E: E.transpose(out=pT[0:32, k * CH:(k + 1) * CH], in_=ikb[:, g0 + k, :], identity=identf[0:CH, 0:CH]),
                         r=[ikb_d, identf_d], w=[pT_d])
                S.op("act", lambda E: E.copy(out=kiT[:, g0 * CH:(g0 + 4) * CH], in_=pT[0:32, 0:GRP]), r=[pT_d], w=[kiT_d])
            S.dma(w16[:], iw_in, w=[w16_d])
            S.op("dve", lambda E: E.tensor_scalar(out=w16[:], in0=w16[:], scalar1=1.0 / 16, scalar2=None, op0=ALU.mult), r=[w16_d], w=[w16_d])
            S.dma(wuk[:], wuk_in, w=[wuk_d])
            S.dma(wst[:, 0:256], wuv_in.rearrange("p a b -> p (a b)"), r=[], w=[wst_d])
            S.op("dve", lambda E: E.tensor_copy(out=wuv[:].rearrange("p a b -> p (a b)"), in_=wst[:, 0:256]), r=[wst_d], w=[wuv_d])
        S.barrier()

        I = S.sb("I", [CH, T], F32); I_d = Dep()
        mask = S.sb("mask", [CH, T], BF16); mask_d = Dep()
        rt = [(S.sb("rt%d" % i, [CH, GRP], F32), Dep()) for i in range(3)]
        Et = [(S.sb("Et%d" % i, [CH, GRP], BF16), Dep()) for i in range(2)]
        Pt = [(S.sb("Pt%d" % i, [CH, 4, CH], BF16), Dep()) for i in range(2)]
        qlat = S.sb("qlat", [LAT, GRP], BF16); qlat_d = Dep()
        sv = S.sb("sv", [CH, 8], F32); sv_d = Dep()
        rden = S.sb("rden", [LAT, GRP], F32); rden_d = Dep()
        ob = S.sb("ob", [LAT, GRP], BF16); ob_d = Dep()
        ysb = S.sb("ysb", [64, GRP], F32); ysb_d = Dep()
        dqb = [(S.sb("dqb%d" % i, [64, 4, CH], F32), Dep()) for i in range(2)]
        iqb = [(S.sb("iqb%d" % i, [32, 8, CH], F32), Dep()) for i in range(2)]
        ri = 0
        ei = 0
        for m in range(NB):
            qs = slice(m * CH, (m + 1) * CH)
            ng = m + 1
            nk = ng * GRP
            dqT, dqT_d = dqb[m % 2]
            iqT, iqT_d = iqb[m % 2]
            S.dma(dqT[:], dq_in[:, :, qs], w=[dqT_d])
            S.dma(iqT[:], iq_in[:, :, qs], w=[iqT_d])
            pq, pq_d = nextA()
            for h in range(4):
                S.op("pe", lambda E: E.matmul(pq[:, h * CH:(h + 1) * CH], wuk[:, h, :], dqT[:, h, :], start=True, stop=True),
                     r=[wuk_d, dqT_d], w=[pq_d])
            S.op("act", lambda E: E.mul(out=qlat[:, :], in_=pq[:, 0:GRP], mul=0.125), r=[pq_d], w=[qlat_d])
            for g in range(ng):
                ks = slice(g * GRP, (g + 1) * GRP)
                for h in range(8):
                    px, px_d = nextA()
                    S.op("pe", lambda E: E.matmul(px[0:CH, 0:GRP], iqT[:, h, :], kiT[:, ks], start=True, stop=True),
                         r=[iqT_d, kiT_d], w=[px_d])
                    r_, r_d = rt[ri % 3]
                    ri += 1
                    S.op("act", lambda E: E.activation(out=r_[:, :], in_=px[0:CH, 0:GRP], func=AF.Relu), r=[px_d], w=[r_d])
                    if h == 0:
                        S.op("dve", lambda E: E.tensor_scalar(out=I[:, ks], in0=r_[:, :], scalar1=w16[:, m, 0:1], scalar2=None, op0=ALU.mult),
                             r=[r_d, w16_d], w=[I_d])
                    else:
                        S.op("dve", lambda E: E.scalar_tensor_tensor(out=I[:, ks], in0=r_[:, :], scalar=w16[:, m, h:h + 1], in1=I[:, ks],
                                                                     op0=ALU.mult, op1=ALU.add), r=[r_d, w16_d, I_d], w=[I_d])
            S.op("dve", lambda E: E.tensor_reduce(out=sv[:, 0:1], in_=I[:, 0:nk], axis=AX.X, op=ALU.min), r=[I_d], w=[sv_d])
            S.op("dve", lambda E: E.tensor_reduce(out=sv[:, 5:6], in_=I[:, 0:nk], axis=AX.X, op=ALU.max), r=[I_d], w=[sv_d])
            S.op("dve", lambda E: E.tensor_tensor(out=sv[:, 1:2], in0=sv[:, 5:6], in1=sv[:, 0:1], op=ALU.subtract), r=[sv_d], w=[sv_d])
            S.op("dve", lambda E: E.tensor_scalar(out=sv[:, 1:2], in0=sv[:, 1:2], scalar1=1.0001, scalar2=1e-20, op0=ALU.mult, op1=ALU.add),
                 r=[sv_d], w=[sv_d])
            S.op("dve", lambda E: E.tensor_tensor(out=I[:, m * GRP:nk], in0=I[:, m * GRP:nk], in1=negm[:, :], op=ALU.add),
                 r=[I_d, negm_d], w=[I_d])
            for n in range(1, NR + 1):
                S.op("dve", lambda E: E.scalar_tensor_tensor(out=sv[:, 2:3], in0=sv[:, 1:2], scalar=float(2.0 ** -n), in1=sv[:, 0:1],
                                                             op0=ALU.mult, op1=ALU.add), r=[sv_d], w=[sv_d])
                S.op("dve", lambda E: E.tensor_scalar(out=mask[:, 0:nk], in0=I[:, 0:nk], scalar1=sv[:, 2:3], scalar2=None,
                                                      op0=ALU.is_ge, op1=ALU.add, accum_out=sv[:, 3:4]), r=[I_d, sv_d], w=[mask_d, sv_d])
                S.op("dve", lambda E: E.tensor_scalar(out=sv[:, 4:5], in0=sv[:, 3:4], scalar1=float(TOPK), scalar2=float(2.0 ** -n),
                                                      op0=ALU.is_ge, op1=ALU.mult), r=[sv_d], w=[sv_d])
                S.op("dve", lambda E: E.scalar_tensor_tensor(out=sv[:, 0:1], in0=sv[:, 4:5], scalar=sv[:, 1:2], in1=sv[:, 0:1],
                                                             op0=ALU.mult, op1=ALU.add), r=[sv_d], w=[sv_d])
            S.op("dve", lambda E: E.tensor_scalar(out=mask[:, 0:nk], in0=I[:, 0:nk], scalar1=sv[:, 0:1], scalar2=None, op0=ALU.is_ge),
                 r=[I_d, sv_d], w=[mask_d])
            po, po_d = pso
            pd, pd_d = psd
            nck = 4 * ng
            for c in range(nck):
                cs = slice(c * CH, (c + 1) * CH)
                pT, pT_d = nextT()
                S.op("pe", lambda E: E.transpose(out=pT[0:CH, 0:CH], in_=mask[:, cs], identity=ident[0:CH, 0:CH]),
                     r=[mask_d, ident_d], w=[pT_d])
                pS, pS_d = nextA()
                S.op("pe", lambda E: E.matmul(pS[0:CH, 0:GRP], cT[:, cs], qlat[:, :], start=True, stop=True),
                     r=[cT_d, qlat_d], w=[pS_d])
                e_, e_d = Et[ei % 2]
                p_, p_d = Pt[ei % 2]
                ei += 1
                S.op("act", lambda E: E.activation(out=e_[:, :], in_=pS[0:CH, 0:GRP], func=AF.Exp), r=[pS_d], w=[e_d])
                S.op("dve", lambda E: E.tensor_tensor(out=p_[:, :, :], in0=e_[:, :].rearrange("p (h q) -> p h q", h=4),
                                                      in1=pT[0:CH, 0:CH].unsqueeze(1).to_broadcast([CH, 4, CH]), op=ALU.mult),
                     r=[e_d, pT_d], w=[p_d])
                pf = p_[:, :, :].rearrange("p h q -> p (h q)")
                S.op("pe", lambda E: E.matmul(po[:, 0:GRP], ctok[:, c, :], pf, start=(c == 0), stop=(c == nck - 1)),
                     r=[ctok_d, p_d], w=[po_d])
                S.op("pe", lambda E: E.matmul(pd[:, 0:GRP], onesb[0:CH, :], pf, start=(c == 0), stop=(c == nck - 1)),
                     r=[onesb_d, p_d], w=[pd_d])
            S.op("dve", lambda E: E.reciprocal(out=rden[:, :], in_=pd[:, 0:GRP]), r=[pd_d], w=[rden_d])
            S.op("dve", lambda E: E.tensor_tensor(out=ob[:, :], in0=po[:, 0:GRP], in1=rden[:, :], op=ALU.mult), r=[po_d, rden_d], w=[ob_d])
            py, py_d = nextA()
            for h in range(4):
                S.op("pe", lambda E: E.matmul(py[0:64, h * CH:(h + 1) * CH], wuv[:, h, :], ob[:, h * CH:(h + 1) * CH], start=True, stop=True),
                     r=[wuv_d, ob_d], w=[py_d])
            S.op("act", lambda E: E.copy(out=ysb[:, :], in_=py[0:64, 0:GRP]), r=[py_d], w=[ysb_d])
            S.dma(y_out[:, :, qs], ysb[:, :].rearrange("p (h q) -> p h q", h=4), r=[ysb_d])
        S.finish()
        print("dsa ops", S.nops, "waits", S.nwaits)
    return nc


_PROGS = {}


def _prog(name, fn):
    if name not in _PROGS:
        _PROGS[name] = fn()
    return _PROGS[name]


def _f32(a):
    return np.ascontiguousarray(a, dtype=np.float32)


def _run(nc, in_maps):
    res = run_bass_kernel_spmd(nc, in_maps, core_ids=list(range(NCORES)))
    return res.results


O_FQ, O_FK, O_FV, O_FF, O_CU, O_LX, O_LG, O_DQ, O_DKV, O_IQ, O_IK, O_IW = (
    0, 256, 512, 768, 772, 1284, 1540, 1796, 2052, 2180, 2436, 2468)


def kernel(x, meta_tokens, norm_g, ffn_w_in, ffn_w_out, w_in, w_out, fox_b_f,
           conv_dw_w, conv_dw_b, conv_ln_g, conv_ln_b,
           lru_conv_w, lru_conv_b, lru_w_a, lru_b_a, lru_w_i, lru_b_i, lru_lambda,
           dsa_kv_norm_g, dsa_w_uk, dsa_w_uv, idx_k_ln_g, idx_k_ln_b):
    x = np.asarray(x, dtype=np.float32)
    B = x.shape[0]
    T = T_FULL
    NTK = T // 4
    NCH = T // CH
    NB = NCH // 4
    NQT = NB * CH
    depth = norm_g.shape[0]
    h = np.concatenate([np.broadcast_to(np.asarray(meta_tokens, np.float32)[None], (B, NMETA, D)), x], axis=1)
    hT = [_f32(h[c // 4, (c % 4) * NTK:(c % 4 + 1) * NTK].T) for c in range(NCORES)]
    ncT1 = _prog("T1", lambda: build_T1(NTK))
    ncT2 = _prog("T2", lambda: build_T2(NTK))
    ncFX = _prog("FX", lambda: build_fox(T))
    ncCL = _prog("CL", lambda: build_cl(T, NTK))
    ncDS = _prog("DS", lambda: build_dsa(T))
    A = lambda a: np.asarray(a, dtype=np.float32)
    for l in range(depth):
        g = lay_gains(A(norm_g[l]))
        fwin = lay_kc(A(ffn_w_in[l, 0])); fwout = lay_kc(A(ffn_w_out[l, 0])); pw = lay_kc(A(w_in[l]))
        res = _run(ncT1, [{"h_in": hT[c], "g_in": g, "fwin": fwin, "fwout": fwout, "pw": pw} for c in range(NCORES)])
        h1T = [res[c]["h_out"] for c in range(NCORES)]
        zT = [np.concatenate([res[4 * b + j]["z_out"] for j in range(4)], axis=1) for b in range(B)]
        yT = [np.zeros((D, T), np.float32) for _ in range(B)]
        maps = []
        for c in range(NCORES):
            b, j = c // 4, c % 4
            z = zT[b]
            v = z[O_FV + 64 * j:O_FV + 64 * j + 64].T
            maps.append({"q_in": _f32(z[O_FQ + 64 * j:O_FQ + 64 * j + 64]), "k_in": _f32(z[O_FK + 64 * j:O_FK + 64 * j + 64]),
                         "v_in": _f32(v.reshape(NCH, CH, 64).transpose(1, 0, 2)),
                         "ff_in": _f32(z[O_FF + j:O_FF + j + 1]), "bf_in": _f32(A(fox_b_f[l])[j].reshape(1, 1))})
        res = _run(ncFX, maps)
        for c in range(NCORES):
            b, j = c // 4, c % 4
            yT[b][64 * j:64 * j + 64] = res[c]["y_out"]
        cw = _f32(A(conv_dw_w[l]).T.reshape(2, 128, CW).transpose(1, 0, 2))
        cv = _f32(np.stack([A(conv_dw_b[l]), A(conv_ln_g[l]), A(conv_ln_b[l])], -1).reshape(2, 128, 3).transpose(1, 0, 2))
        lvec = np.stack([A(lru_conv_b[l]), A(lru_b_a[l]), A(lru_b_i[l]), A(lru_lambda[l])], -1)
        maps = []
        for c in range(NCORES):
            b, j = c // 4, c % 4
            z = zT[b]
            cup = np.concatenate([np.zeros((2 * GW, HAL), np.float32), z[O_CU:O_CU + 2 * GW]], axis=1)
            lxp = np.concatenate([np.zeros((64, 3), np.float32), z[O_LX + 64 * j:O_LX + 64 * j + 64]], axis=1)
            maps.append({"cu_in": _f32(cup[:, j * NTK:j * NTK + HAL + NTK]), "cw_in": cw, "cv_in": cv,
                         "lx_in": _f32(lxp), "lg_in": _f32(z[O_LG + 64 * j:O_LG + 64 * j + 64]),
                         "lcw_in": _f32(A(lru_conv_w[l])[:, 64 * j:64 * j + 64].T), "lv_in": _f32(lvec[64 * j:64 * j + 64]),
                         "lwa_in": _f32(A(lru_w_a[l])[j]), "lwi_in": _f32(A(lru_w_i[l])[j])})
        res = _run(ncCL, maps)
        for c in range(NCORES):
            b, j = c // 4, c % 4
            yT[b][256:512, j * NTK:(j + 1) * NTK] = res[c]["yc_out"]
            yT[b][512 + 64 * j:512 + 64 * j + 64] = res[c]["yl_out"]
        kvg = _f32(np.tile(A(dsa_kv_norm_g[l])[None], (CH, 1)))
        ikg = _f32(np.tile(np.stack([A(idx_k_ln_g[l]), A(idx_k_ln_b[l])])[None], (CH, 1, 1)))
        wuk = _f32(A(dsa_w_uk[l]).transpose(2, 0, 1))
        wuv = _f32(A(dsa_w_uv[l]).transpose(1, 0, 2))
        maps = []
        toks_of = []
        for c in range(NCORES):
            b, j = c // 4, c % 4
            z = zT[b]
            toks = np.concatenate([np.arange((4 * m + j) * CH, (4 * m + j + 1) * CH) for m in range(NB)])
            toks_of.append(toks)
            maps.append({"dkv_in": _f32(z[O_DKV:O_DKV + 128].T.reshape(NCH, CH, 128).transpose(1, 0, 2)), "kvg_in": kvg,
                         "ik_in": _f32(z[O_IK:O_IK + 32].T.reshape(NCH, CH, 32).transpose(1, 0, 2)), "ikg_in": ikg,
                         "dq_in": _f32(z[O_DQ:O_DQ + 256][:, toks].reshape(4, 64, NQT).transpose(1, 0, 2)),
                         "iq_in": _f32(z[O_IQ:O_IQ + 256][:, toks].reshape(8, 32, NQT).transpose(1, 0, 2)),
                         "iw_in": _f32(z[O_IW:O_IW + 8][:, toks].T.reshape(NB, CH, 8).transpose(1, 0, 2)),
                         "wuk_in": wuk, "wuv_in": wuv, "joff_in": _f32(np.full((CH, 1), CH * j))})
        res = _run(ncDS, maps)
        for c in range(NCORES):
            b, j = c // 4, c % 4
            o = res[c]["y_out"]
            yT[b][768:1024][:, toks_of[c]] = o.transpose(1, 0, 2).reshape(256, NQT)
        ow = lay_kc(A(w_out[l])); fwin = lay_kc(A(ffn_w_in[l, 1])); fwout = lay_kc(A(ffn_w_out[l, 1]))
        res = _run(ncT2, [{"h_in": h1T[c], "y_in": _f32(yT[c // 4][:, (c % 4) * NTK:(c % 4 + 1) * NTK]), "g_in": g,
                           "ow": ow, "fwin": fwin, "fwout": fwout} for c in range(NCORES)])
        hT = [res[c]["h_out"] for c in range(NCORES)]
    out = np.stack([np.concatenate([hT[4 * b + j].T for j in range(4)], axis=0)[NMETA:] for b in range(B)], axis=0)
    return np.ascontiguousarray(out, dtype=np.float32)
```

```python
import numpy as np
from contextlib import ExitStack
import concourse.bass as bass
import concourse.mybir as mybir
from concourse.bass_utils import run_bass_kernel_spmd

F32 = mybir.dt.float32
BF16 = mybir.dt.bfloat16
I32 = mybir.dt.int32
AF = mybir.ActivationFunctionType
ALU = mybir.AluOpType
AX = mybir.AxisListType

D = 1024
DFF = 2560
KC = D // 128
JC = DFF // 128
DIN = 2476
NCORES = 8
NMETA = 16
SEQ = 8192
T_FULL = SEQ + NMETA
CH = 114
GRP = 4 * CH


class Dep:
    __slots__ = ("w", "r")

    def __init__(self):
        self.w = []
        self.r = {}


class Sched:
    def __init__(self, nc, es, n_dma_sems=28):
        self.nc = nc
        self.es = es
        self.eng = {"pe": nc.tensor, "act": nc.scalar, "dve": nc.vector,
                    "pool": nc.gpsimd, "sp": nc.sync}
        self.sems = {}
        for k in self.eng:
            self.sems[k] = es.enter_context(nc.semaphore("c_" + k))
        self.cnt = {k: 0 for k in self.eng}
        self.seen = {k: {} for k in self.eng}
        self.dpool = []
        for i in range(n_dma_sems):
            key = "d%d" % i
            self.sems[key] = es.enter_context(nc.semaphore(key))
            self.dpool.append([key, 0])
        self.dnext = 0
        self.ccs = []
        self.nwaits = 0
        self.nops = 0
        self._uid = 0

    def sb(self, name, shape, dtype, es=None):
        self._uid += 1
        return (es or self.es).enter_context(
            self.nc.sbuf_tensor("sb%d_%s" % (self._uid, name), list(shape), dtype))

    def ps(self, name, shape, dtype=F32, es=None):
        self._uid += 1
        return (es or self.es).enter_context(
            self.nc.psum_tensor("ps%d_%s" % (self._uid, name), list(shape), dtype))

    def _wait(self, eng, ev):
        if ev is None:
            return
        key, val = ev
        if key == eng and eng in ("pe", "sp"):
            return
        if self.seen[eng].get(key, 0) >= val:
            return
        self.eng[eng].wait_ge(self.sems[key], val)
        self.seen[eng][key] = val
        self.nwaits += 1

    def _deps(self, eng, r, w, acc=False):
        for d in r:
            for ev in d.w:
                self._wait(eng, ev)
        for d in w:
            for ev in d.w:
                if acc and ev[0] not in self.eng:
                    continue
                self._wait(eng, ev)
            for e, ev in d.r.items():
                if e != eng:
                    self._wait(eng, ev)

    def op(self, eng, fn, r=(), w=()):
        self._deps(eng, r, w)
        ins = fn(self.eng[eng])
        self.cnt[eng] += 1
        ins.then_inc(self.sems[eng], 1)
        ev = (eng, self.cnt[eng])
        for d in r:
            d.r[eng] = ev
        for d in w:
            d.w = [ev]
            d.r = {}
        self.nops += 1
        return ev

    def dma(self, out, in_, r=(), w=(), q="sp", acc=False, fn=None, **kw):
        self._deps(q, r, w, acc=acc)
        slot = self.dpool[self.dnext]
        self.dnext = (self.dnext + 1) % len(self.dpool)
        key, val = slot
        if val > 0:
            self._wait(q, (key, val))
        if fn is None:
            ins = self.eng[q].dma_start(out=out, in_=in_, **kw)
        else:
            ins = fn(self.eng[q])
        ins.then_inc(self.sems[key], 16)
        slot[1] = val + 16
        ev = (key, val + 16)
        for d in r:
            d.r[key] = ev
        for d in w:
            if acc:
                d.w = d.w + [ev]
            else:
                d.w = [ev]
                d.r = {}
        self.nops += 1
        return ev

    def collective(self, kind, in_ap, out_ap, w_dep):
        key = "cc%d" % len(self.ccs)
        self.sems[key] = self.es.enter_context(self.nc.semaphore(key))
        self.ccs.append(key)
        ins = self.nc.gpsimd.collective_compute(kind, ALU.bypass, replica_groups=[list(range(NCORES))],
                                                ins=[in_ap], outs=[out_ap])
        ins.then_inc(self.sems[key], 1)
        w_dep.w = [(key, 1)]
        w_dep.r = {}
        self.nops += 1

    def barrier(self):
        for e in ("pe", "act", "dve", "pool", "sp"):
            for key, val in self.dpool:
                if val > 0:
                    self._wait(e, (key, val))
            for key in self.ccs:
                self._wait(e, (key, 1))
            for k in ("pe", "act", "dve", "pool"):
                if k != e and self.cnt[k] > 0:
                    self._wait(e, (k, self.cnt[k]))
            if e not in ("pe", "sp") and self.cnt[e] > 0:
                self._wait(e, (e, self.cnt[e]))

    def finish(self):
        self.barrier()


class PsumPool:
    def __init__(self, S):
        self.f = [(S.ps("pf%d" % i, [128, 512]), Dep()) for i in range(6)]
        self.b = [(S.ps("pb%d" % i, [128, 1024], BF16), Dep()) for i in range(2)]
        self.fi = 0
        self.bi = 0

    def nf(self, lo=0, hi=6):
        p = self.f[lo + self.fi % (hi - lo)]
        self.fi += 1
        return p

    def nb(self):
        p = self.b[self.bi % 2]
        self.bi += 1
        return p


class Gather:
    def __init__(self, S, ncols):
        self.S = S
        self.ncols = ncols
        self.idx = S.sb("gidx", [128, ncols], I32)
        self.idx_d = Dep()
        self.specs = []
        self.memo = {}

    def load(self, idx_dram):
        self.S.dma(self.idx[:], idx_dram, w=[self.idx_d])

    def col(self, key, P, fn):
        if key not in self.memo:
            assert len(self.specs) < self.ncols
            self.memo[key] = len(self.specs)
            self.specs.append((P, fn))
        return self.memo[key]

    def gather(self, out_ap, P, src_ap, key, fn, r=(), w=(), acc=False):
        c = self.col(key, P, fn)
        off = bass.IndirectOffsetOnAxis(ap=self.idx[0:P, c:c + 1], axis=0)
        self.S.dma(None, None, r=list(r) + [self.idx_d], w=w, q="pool", acc=acc,
                   fn=lambda E: E.indirect_dma_start(out=out_ap, out_offset=None, in_=src_ap, in_offset=off))

    def table(self, b, j):
        t = np.zeros((128, self.ncols), np.int32)
        for c, (P, fn) in enumerate(self.specs):
            t[:P, c] = np.asarray(fn(b, j), dtype=np.int64)
        return t


O_FQ, O_FK, O_FV, O_FF, O_CU, O_LX, O_LG, O_DQ, O_DKV, O_IQ, O_IK, O_IW = (
    0, 256, 512, 768, 772, 1284, 1540, 1796, 2052, 2180, 2436, 2468)
ZQ0 = O_DQ
ZQR = DIN - ZQ0
CW = 31
HAL = CW - 1
GW = 256
NEG = -1.0e30
TOPK = 256


class TStage:
    def __init__(self, S, PS, NT, es):
        self.S = S
        self.PS = PS
        self.NT = NT
        sb = lambda n, s, d: S.sb(n, s, d, es)
        self.ones = sb("ones", [128, 128], F32)
        self.ones_d = Dep()
        S.op("dve", lambda E: E.memset(self.ones[:], 1.0), w=[self.ones_d])
        self.eps = sb("eps", [128, 1], F32)
        self.eps_d = Dep()
        S.op("dve", lambda E: E.memset(self.eps[:], 1e-6), w=[self.eps_d])
        self.win = sb("win", [128, KC, 2 * DFF], BF16)
        self.win_d = Dep()
        self.wout = sb("wout", [128, JC, D], BF16)
        self.wout_d = Dep()
        self.stages = [(sb("stg%d" % i, [128, 1280], F32), Dep()) for i in range(3)]
        self.gt = sb("gt", [128, 6 * KC], F32)
        self.gt_d = Dep()
        self.x32 = [(sb("x32_%d" % i, [128, KC, NT], F32), Dep()) for i in range(2)]
        self.sq = (sb("sq", [128, KC, NT], F32), Dep())
        self.xn = (sb("xn", [128, KC, NT], BF16), Dep())
        self.act = (sb("actb", [128, JC, NT], BF16), Dep())
        self.sg = [(sb("sg%d" % i, [128, NT], F32), Dep()) for i in range(2)]
        self.y32 = (sb("y32", [128, KC, NT], F32), Dep())
        self.rstd = (sb("rstd", [128, NT], F32), Dep())
        self.tmp = [(sb("tmp%d" % i, [128, NT], F32), Dep()) for i in range(2)]
        self.lc = 0
        self.ddeps = {}

    def ddep(self, key, t0):
        k = (key, t0)
        if k not in self.ddeps:
            self.ddeps[k] = Dep()
        return self.ddeps[k]

    def nextps(self):
        return self.PS.nf()

    def load_cast(self, dst, dst_dep, src_dram, F, scale=None, scale_dep=None):
        S = self.S
        i = self.lc
        self.lc += 1
        st, sd = self.stages[i % len(self.stages)]
        S.dma(st[:, 0:F], src_dram, w=[sd])
        e = ("act", "dve", "pool")[i % 3]
        r = [sd] + ([scale_dep] if scale_dep is not None else [])
        if scale is None:
            if e == "act":
                S.op(e, lambda E: E.copy(out=dst, in_=st[:, 0:F]), r=r, w=[dst_dep])
            else:
                S.op(e, lambda E: E.tensor_copy(out=dst, in_=st[:, 0:F]), r=r, w=[dst_dep])
        else:
            if e == "act":
                S.op(e, lambda E: E.activation(out=dst, in_=st[:, 0:F], func=AF.Copy, scale=scale), r=r, w=[dst_dep])
            else:
                S.op(e, lambda E: E.tensor_scalar(out=dst, in0=st[:, 0:F], scalar1=scale, scalar2=None, op0=ALU.mult),
                     r=r, w=[dst_dep])

    def rms_rstd(self, src, src_d, n, out_scale=1.0):
        S = self.S
        sq, sq_d = self.sq
        S.op("act", lambda E: E.activation(out=sq[:, :, 0:n], in_=src[:, :, 0:n], func=AF.Square), r=[src_d], w=[sq_d])
        ps, ps_d = self.nextps()
        for c in range(KC):
            S.op("pe", lambda E: E.matmul(ps[:, 0:n], self.ones[:], sq[:, c, 0:n], start=(c == 0), stop=(c == KC - 1)),
                 r=[self.ones_d, sq_d], w=[ps_d])
        rs, rs_d = self.rstd
        S.op("act", lambda E: E.activation(out=rs[:, 0:n], in_=ps[:, 0:n], func=AF.Sqrt, scale=1.0 / D, bias=self.eps[:, 0:1]),
             r=[ps_d, self.eps_d], w=[rs_d])
        S.op("dve", lambda E: E.reciprocal(out=rs[:, 0:n], in_=rs[:, 0:n]), r=[rs_d], w=[rs_d])
        if out_scale != 1.0:
            S.op("dve", lambda E: E.tensor_scalar(out=rs[:, 0:n], in0=rs[:, 0:n], scalar1=float(out_scale), scalar2=None, op0=ALU.mult),
                 r=[rs_d], w=[rs_d])
        return rs, rs_d

    def load_gains(self, g_dram):
        self.S.dma(self.gt[:], g_dram, w=[self.gt_d])

    def gcol(self, gi, c):
        return self.gt[:, gi * KC + c:gi * KC + c + 1]

    def load_ffn_weights(self, win_dram, wout_dram, gi):
        for c in range(KC):
            for hh in range(4):
                self.load_cast(self.win[:, c, hh * 1280:(hh + 1) * 1280], self.win_d,
                               win_dram[:, c, hh * 1280:(hh + 1) * 1280], 1280,
                               scale=self.gcol(gi, c), scale_dep=self.gt_d)
        for j in range(JC):
            self.load_cast(self.wout[:, j, :], self.wout_d, wout_dram[:, j, :], D)

    def load_sq_weights(self, w_dram, ncols, gi=None):
        for c in range(KC):
            for f0 in range(0, ncols, 1280):
                f = min(1280, ncols - f0)
                self.load_cast(self.win[:, c, f0:f0 + f], self.win_d, w_dram[:, c, f0:f0 + f], f,
                               scale=(self.gcol(gi, c) if gi is not None else None),
                               scale_dep=(self.gt_d if gi is not None else None))

    def normalize(self, x32, x_d, n):
        S = self.S
        rs, rs_d = self.rms_rstd(x32, x_d, n)
        xn, xn_d = self.xn
        for c in range(KC):
            e = "dve" if c % 2 == 0 else "pool"
            S.op(e, lambda E: E.tensor_tensor(out=xn[:, c, 0:n], in0=x32[:, c, 0:n], in1=rs[:, 0:n], op=ALU.mult),
                 r=[x_d, rs_d], w=[xn_d])
        return xn, xn_d

    def add_normed(self, x32, x_d, n, gpost, half):
        S = self.S
        y32, y_d = self.y32
        rs2, rs2_d = self.rms_rstd(y32, y_d, n, out_scale=half)
        for c in range(KC):
            tp, tp_d = self.tmp[c % 2]
            e = "dve" if c % 2 == 0 else "pool"
            S.op("dve", lambda E: E.scalar_tensor_tensor(out=tp[:, 0:n], in0=y32[:, c, 0:n], scalar=self.gcol(gpost, c),
                                                         in1=rs2[:, 0:n], op0=ALU.mult, op1=ALU.mult),
                 r=[y_d, rs2_d, self.gt_d], w=[tp_d])
            S.op(e, lambda E: E.tensor_tensor(out=x32[:, c, 0:n], in0=tp[:, 0:n], in1=x32[:, c, 0:n], op=ALU.add),
                 r=[tp_d, x_d], w=[x_d])

    def ffn_compute(self, x32, x_d, n, gpost):
        S = self.S
        xn, xn_d = self.normalize(x32, x_d, n)
        act, act_d = self.act
        for j in range(JC):
            pg, pg_d = self.nextps()
            for c in range(KC):
                S.op("pe", lambda E: E.matmul(pg[:, 0:n], self.win[:, c, j * 128:(j + 1) * 128], xn[:, c, 0:n],
                                              start=(c == 0), stop=(c == KC - 1)),
                     r=[self.win_d, xn_d], w=[pg_d])
            pu, pu_d = self.nextps()
            for c in range(KC):
                S.op("pe", lambda E: E.matmul(pu[:, 0:n], self.win[:, c, DFF + j * 128:DFF + (j + 1) * 128], xn[:, c, 0:n],
                                              start=(c == 0), stop=(c == KC - 1)),
                     r=[self.win_d, xn_d], w=[pu_d])
            sg, sg_d = self.sg[j % 2]
            S.op("act", lambda E: E.activation(out=sg[:, 0:n], in_=pg[:, 0:n], func=AF.Silu), r=[pg_d], w=[sg_d])
            S.op("dve", lambda E: E.tensor_tensor(out=act[:, j, 0:n], in0=sg[:, 0:n], in1=pu[:, 0:n], op=ALU.mult),
                 r=[sg_d, pu_d], w=[act_d])
        y32, y_d = self.y32
        for c in range(KC):
            po, po_d = self.nextps()
            for j in range(JC):
                S.op("pe", lambda E: E.matmul(po[:, 0:n], self.wout[:, j, c * 128:(c + 1) * 128], act[:, j, 0:n],
                                              start=(j == 0), stop=(j == JC - 1)),
                     r=[self.wout_d, act_d], w=[po_d])
            S.op("act", lambda E: E.copy(out=y32[:, c, 0:n], in_=po[:, 0:n]), r=[po_d], w=[y_d])
        self.add_normed(x32, x_d, n, gpost, 0.5)

    def ffn_phase(self, h_in, h_out, ntok, gpost, kin="h", kout="h"):
        S = self.S
        it = 0
        for t0 in range(0, ntok, self.NT):
            n = min(self.NT, ntok - t0)
            x32, x_d = self.x32[it % 2]
            S.dma(x32[:, :, 0:n], h_in[:, t0:t0 + n].rearrange("(c p) n -> p c n", p=128), r=[self.ddep(kin, t0)], w=[x_d])
            self.ffn_compute(x32, x_d, n, gpost)
            S.dma(h_out[:, t0:t0 + n].rearrange("(c p) n -> p c n", p=128), x32[:, :, 0:n], r=[x_d], w=[self.ddep(kout, t0)])
            it += 1

    def proj_phase(self, h_in, z_out, zq_out, halo_out, ntok, kin="h"):
        S = self.S
        it = 0
        for t0 in range(0, ntok, self.NT):
            n = min(self.NT, ntok - t0)
            x32, x_d = self.x32[it % 2]
            S.dma(x32[:, :, 0:n], h_in[:, t0:t0 + n].rearrange("(c p) n -> p c n", p=128), r=[self.ddep(kin, t0)], w=[x_d])
            xn, xn_d = self.normalize(x32, x_d, n)
            k = 0
            for m0 in range(0, DIN, 128):
                m = min(128, DIN - m0)
                pz, pz_d = self.nextps()
                for c in range(KC):
                    S.op("pe", lambda E: E.matmul(pz[0:m, 0:n], self.win[:, c, m0:m0 + m], xn[:, c, 0:n],
                                                  start=(c == 0), stop=(c == KC - 1)),
                         r=[self.win_d, xn_d], w=[pz_d])
                zs, zs_d = self.sg[k % 2]
                if k % 2 == 0:
                    S.op("act", lambda E: E.copy(out=zs[0:m, 0:n], in_=pz[0:m, 0:n]), r=[pz_d], w=[zs_d])
                else:
                    S.op("dve", lambda E: E.tensor_copy(out=zs[0:m, 0:n], in_=pz[0:m, 0:n]), r=[pz_d], w=[zs_d])
                S.dma(z_out[m0:m0 + m, t0:t0 + n], zs[0:m, 0:n], r=[zs_d])
                lo, hi = max(m0, ZQ0), min(m0 + m, DIN)
                if lo < hi:
                    for kb in range(n // CH):
                        blk = t0 // CH + kb
                        S.dma(zq_out[blk * ZQR + lo - ZQ0:blk * ZQR + hi - ZQ0, :], zs[lo - m0:hi - m0, kb * CH:(kb + 1) * CH], r=[zs_d])
                lo, hi = max(m0, O_CU), min(m0 + m, O_CU + 2 * GW)
                if lo < hi and t0 + n == ntok:
                    S.dma(halo_out[lo - O_CU:hi - O_CU, :], zs[lo - m0:hi - m0, n - HAL:n], r=[zs_d])
                k += 1
            it += 1

    def mixin_phase(self, h_in, h_out, ntok, gpost, fill_y, kin="h", kout="h"):
        S = self.S
        it = 0
        for t0 in range(0, ntok, self.NT):
            n = min(self.NT, ntok - t0)
            x32, x_d = self.x32[it % 2]
            S.dma(x32[:, :, 0:n], h_in[:, t0:t0 + n].rearrange("(c p) n -> p c n", p=128), r=[self.ddep(kin, t0)], w=[x_d])
            sq, sq_d = self.sq
            fill_y(sq, sq_d, t0, n)
            xn, xn_d = self.xn
            S.op("dve", lambda E: E.tensor_copy(out=xn[:, :, 0:n], in_=sq[:, :, 0:n]), r=[sq_d], w=[xn_d])
            y32, y_d = self.y32
            for c in range(KC):
                po, po_d = self.nextps()
                for k in range(KC):
                    S.op("pe", lambda E: E.matmul(po[:, 0:n], self.win[:, k, c * 128:(c + 1) * 128], xn[:, k, 0:n],
                                                  start=(k == 0), stop=(k == KC - 1)),
                         r=[self.win_d, xn_d], w=[po_d])
                S.op("act", lambda E: E.copy(out=y32[:, c, 0:n], in_=po[:, 0:n]), r=[po_d], w=[y_d])
            self.add_normed(x32, x_d, n, gpost, 1.0)
            S.dma(h_out[:, t0:t0 + n].rearrange("(c p) n -> p c n", p=128), x32[:, :, 0:n], r=[x_d], w=[self.ddep(kout, t0)])
            it += 1


def lay_kc(w):
    k = w.shape[0] // 128
    return np.ascontiguousarray(w.reshape(k, 128, w.shape[1]).transpose(1, 0, 2))


def lay_gains(g):
    return np.ascontiguousarray(g.reshape(6, KC, 128).transpose(2, 0, 1).reshape(128, 6 * KC))


def fox_phase(S, PS, G, es, T, zall, zall_d, bf_dram, yx_loc):
    NTK = T // 4
    NCH = T // CH
    NQ = T // GRP
    HD = 64
    sb = lambda n, s, d: S.sb(n, s, d, es)
    R1 = sb("R1", [65, T], F32); R1_d = Dep()
    R2 = sb("R2", [65, T], F32); R2_d = Dep()
    qa = sb("qa", [65, T], BF16); qa_d = Dep(); qar_d = Dep()
    ka = sb("ka", [65, T], BF16); ka_d = Dep(); kar_d = Dep()
    vT = sb("vT", [HD, T], BF16); vT_d = Dep()
    va = sb("va", [CH, NCH, HD + 1], BF16); va_d = Dep()
    stg = [(sb("stg%d" % i, [HD, NTK], F32), Dep()) for i in range(2)]
    ffs = sb("ffs", [4, NTK], F32); ffs_d = Dep()
    sm = sb("sm", [65, 8], F32); sm_d = Dep()
    ones = sb("ones", [65, 128], F32); ones_d = Dep()
    identb = sb("identb", [HD, HD], BF16); identb_d = Dep()
    idf = sb("idf", [HD, HD], F32); idf_d = Dep()
    cc = sb("cc", [CH, NCH + NQ], F32); cc_d = Dep()
    biasT = sb("biasT", [CH, NQ, NCH], F32); biasT_d = Dep()
    mf = sb("mf", [CH, 4, GRP], F32); mf_d = Dep()
    mask = sb("mask", [CH, 4, GRP], BF16); mask_d = Dep()
    pt = [(sb("pt%d" % i, [CH, GRP], BF16), Dep()) for i in range(4)]
    osb = [(sb("osb%d" % i, [65, GRP], F32), Dep()) for i in range(2)]
    ysb = [(sb("ysb%d" % i, [HD, GRP], F32), Dep()) for i in range(2)]
    ps_s = PS.f[0:3]
    ps_o = PS.f[3:5]
    ps_m = PS.f[5]

    S.op("dve", lambda E: E.memset(ones[:], 1.0), w=[ones_d])
    S.op("pool", lambda E: E.iota(mf[:], [[-CH, 4], [1, GRP]], base=0, channel_multiplier=-1,
                                  allow_small_or_imprecise_dtypes=True), w=[mf_d])
    S.op("dve", lambda E: E.tensor_scalar(out=mask[:], in0=mf[:], scalar1=0.0, scalar2=None, op0=ALU.is_ge),
         r=[mf_d], w=[mask_d])
    S.op("pool", lambda E: E.iota(idf[:], [[1, HD]], base=0, channel_multiplier=-1, allow_small_or_imprecise_dtypes=True), w=[idf_d])
    S.op("dve", lambda E: E.tensor_scalar(out=identb[:], in0=idf[:], scalar1=0.0, scalar2=None, op0=ALU.is_equal), r=[idf_d], w=[identb_d])
    for jc in range(4):
        G.gather(ffs[:, :], 4, zall, ("ff", jc), (lambda b, j, jc=jc: (4 * b + jc) * DIN + O_FF + (j + np.arange(4)) % 4),
                 r=[zall_d], w=[ffs_d])
        S.dma(R1[64:65, jc * NTK:(jc + 1) * NTK], ffs[0:1, :], r=[ffs_d], w=[R1_d], acc=(jc > 0))
    S.dma(sm[64:65, 0:1], bf_dram, w=[sm_d])
    S.op("dve", lambda E: E.tensor_scalar(out=sm[64:65, 1:2], in0=sm[64:65, 0:1], scalar1=-1.0, scalar2=None, op0=ALU.mult),
         r=[sm_d], w=[sm_d])
    S.op("dve", lambda E: E.memset(sm[64:65, 2:3], 1.0), r=[sm_d], w=[sm_d])
    S.op("act", lambda E: E.activation(out=R1[64:65, :], in_=R1[64:65, :], func=AF.Exp, scale=-1.0, bias=sm[64:65, 1:2]),
         r=[R1_d, sm_d], w=[R1_d])
    S.op("act", lambda E: E.activation(out=R1[64:65, :], in_=R1[64:65, :], func=AF.Ln, scale=1.0, bias=sm[64:65, 2:3]),
         r=[R1_d, sm_d], w=[R1_d])
    S.op("dve", lambda E: E.tensor_scalar(out=R1[64:65, :], in0=R1[64:65, :], scalar1=-1.0, scalar2=None, op0=ALU.mult),
         r=[R1_d], w=[R1_d])
    S.op("dve", lambda E: E.tensor_tensor_scan(out=R2[64:65, :], data0=R1[64:65, :], data1=R1[64:65, :], initial=0.0,
                                               op0=ALU.add, op1=ALU.min),
         r=[R1_d], w=[R2_d])
    i = 0
    for off, dst, dd, sc in ((O_FQ, qa, qa_d, HD ** -0.5), (O_FK, ka, ka_d, 1.0), (O_FV, vT, vT_d, 1.0)):
        for jc in range(4):
            st, sd = stg[i % 2]
            G.gather(st[:, :], HD, zall, ("fqkv", off, jc),
                     (lambda b, j, jc=jc, off=off: (4 * b + jc) * DIN + off + HD * j + np.arange(HD)), r=[zall_d], w=[sd])
            sl = slice(jc * NTK, (jc + 1) * NTK)
            if i % 2 == 0:
                S.op("act", lambda E: E.mul(out=dst[0:HD, sl], in_=st[:, :], mul=float(sc)), r=[sd], w=[dd])
            else:
                S.op("dve", lambda E: E.tensor_scalar(out=dst[0:HD, sl], in0=st[:, :], scalar1=float(sc),
                                                      scalar2=None, op0=ALU.mult), r=[sd], w=[dd])
            i += 1
    S.op("pool", lambda E: E.memset(ka[64:65, :], 1.0), w=[kar_d])
    for Q in range(NQ):
        sl = slice(Q * GRP, (Q + 1) * GRP)
        S.op("dve", lambda E: E.tensor_scalar(out=qa[64:65, sl], in0=R2[64:65, sl], scalar1=R2[64:65, Q * GRP:Q * GRP + 1],
                                              scalar2=None, op0=ALU.subtract), r=[R2_d], w=[qar_d])
    pm, pm_d = ps_m
    for c in range(NCH):
        S.op("pe", lambda E: E.matmul(pm[0:CH, c:c + 1], R2[64:65, c * CH:(c + 1) * CH], ones[64:65, 0:1], start=True, stop=True),
             r=[R2_d, ones_d], w=[pm_d])
    for Q in range(NQ):
        S.op("pe", lambda E: E.matmul(pm[0:CH, NCH + Q:NCH + Q + 1], ones[64:65, 0:CH], R2[64:65, Q * GRP:Q * GRP + 1],
                                      start=True, stop=True), r=[R2_d, ones_d], w=[pm_d])
    S.op("act", lambda E: E.copy(out=cc[:, :], in_=pm[0:CH, 0:NCH + NQ]), r=[pm_d], w=[cc_d])
    for Q in range(NQ):
        S.op("dve", lambda E: E.tensor_scalar(out=biasT[:, Q, :], in0=cc[:, 0:NCH], scalar1=cc[:, NCH + Q:NCH + Q + 1],
                                              scalar2=-1.0, op0=ALU.subtract, op1=ALU.mult), r=[cc_d], w=[biasT_d])
    S.op("pool", lambda E: E.memset(va[:, :, HD:HD + 1], 1.0), w=[va_d])
    for c0 in range(0, NCH, 4):
        pT, pT_d = PS.nb()
        for k in range(4):
            S.op("pe", lambda E: E.transpose(out=pT[0:CH, k * HD:(k + 1) * HD], in_=vT[:, (c0 + k) * CH:(c0 + k + 1) * CH],
                                             identity=identb[:, :]), r=[vT_d, identb_d], w=[pT_d])
        S.op("act", lambda E: E.copy(out=va[:, c0:c0 + 4, 0:HD], in_=pT[0:CH, 0:4 * HD].rearrange("p (a b) -> p a b", a=4)),
             r=[pT_d], w=[va_d])

    si = 0
    for Q in range(NQ):
        qsl = slice(Q * GRP, (Q + 1) * GRP)
        po, po_d = ps_o[Q % 2]
        nck = 4 * Q + 4
        pend = None

        def qk(c):
            nonlocal si
            pS, pS_d = ps_s[si % 3]
            ptile, pt_d = pt[si % 4]
            si += 1
            S.op("pe", lambda E: E.matmul(pS[0:CH, 0:GRP], ka[0:65, c * CH:(c + 1) * CH], qa[0:65, qsl], start=True, stop=True),
                 r=[ka_d, kar_d, qa_d, qar_d], w=[pS_d])
            S.op("act", lambda E: E.activation(out=ptile[:, :], in_=pS[0:CH, 0:GRP], func=AF.Exp, scale=1.0,
                                               bias=biasT[:, Q, c:c + 1]), r=[pS_d, biasT_d], w=[pt_d])
            d = c - 4 * Q
            if d >= 0:
                S.op("dve", lambda E: E.tensor_tensor(out=ptile[:, :], in0=ptile[:, :], in1=mask[:, d, :], op=ALU.mult),
                     r=[pt_d, mask_d], w=[pt_d])
            return ptile, pt_d

        def pv(c, ptile, pt_d):
            S.op("pe", lambda E: E.matmul(po[0:HD + 1, 0:GRP], va[:, c, :], ptile[:, :], start=(c == 0), stop=(c == nck - 1)),
                 r=[va_d, pt_d], w=[po_d])

        for c in range(nck):
            cur = qk(c)
            if pend is not None:
                pv(c - 1, *pend)
            pend = cur
        pv(nck - 1, *pend)
        ob, ob_d = osb[Q % 2]
        S.op("act", lambda E: E.copy(out=ob[:, :], in_=po[0:HD + 1, 0:GRP]), r=[po_d], w=[ob_d])
        S.op("dve", lambda E: E.reciprocal(out=ob[64:65, :], in_=ob[64:65, :]), r=[ob_d], w=[ob_d])
        pb, pb_d = ps_m
        S.op("pe", lambda E: E.matmul(pb[0:HD, 0:GRP], ones[64:65, 0:HD], ob[64:65, :], start=True, stop=True),
             r=[ones_d, ob_d], w=[pb_d])
        yb, yb_d = ysb[Q % 2]
        S.op("dve", lambda E: E.tensor_tensor(out=yb[:, :], in0=ob[0:HD, :], in1=pb[0:HD, 0:GRP], op=ALU.mult),
             r=[ob_d, pb_d], w=[yb_d])
        S.dma(yx_loc[4 * Q * HD:(4 * Q + 4) * HD, :].rearrange("(i d) t -> d i t", d=HD),
              yb[:, :].rearrange("d (i t) -> d i t", i=4), r=[yb_d])


def cl_phase(S, PS, G, es_outer, T, zloc, zall, zall_d, halo_all, halo_d, halo_on, halo_on_d,
             cw_dram, cv_dram, lcw_dram, lv_dram, lwa_dram, lwi_dram, yc_loc, yx_loc, YL0):
    NTK = T // 4
    SEG = NTK
    NBK = NTK // CH
    SUB = 342 if SEG % 342 == 0 else 228
    NT = SUB
    nextps = PS.nf
    with ExitStack() as es2:
        sb = lambda n, s, d: S.sb(n, s, d, es2)
        cst = sb("cst", [128, 4], F32); cst_d = Dep()
        S.op("dve", lambda E: E.memset(cst[:, 0:1], 1e-5), w=[cst_d])
        S.op("dve", lambda E: E.memset(cst[:, 1:2], 1.0), w=[cst_d])
        o256 = sb("o256", [128, 128], F32); o256_d = Dep()
        S.op("dve", lambda E: E.memset(o256[:], 1.0 / GW), w=[o256_d])
        A = sb("A", [128, 2, HAL + NTK], F32); A_d = [Dep(), Dep()]
        Gt = sb("G", [128, 2, HAL + NTK], F32); G_d = [Dep(), Dep()]
        acc = sb("acc", [128, 2, NTK], F32); acc_d = [Dep(), Dep()]
        cw = sb("cw", [128, 2, CW], F32); cw_d = Dep()
        cv = sb("cv", [128, 2, 3], F32); cv_d = Dep()
        yc = [(sb("yc%d" % i, [128, NT], F32), Dep()) for i in range(2)]
        sq = [(sb("sqc%d" % i, [128, NT], F32), Dep()) for i in range(2)]
        rs = (sb("rsc", [128, NT], F32), Dep())
        yo = [(sb("yo%d" % i, [128, NT], F32), Dep()) for i in range(2)]
        S.dma(cw[:], cw_dram, w=[cw_d])
        S.dma(cv[:], cv_dram, w=[cv_d])
        for cc in range(2):
            G.gather(A[:, cc, 0:HAL], 128, halo_all, ("halo", cc),
                     (lambda b, j, cc=cc: (4 * b + max(j - 1, 0)) * 2 * GW + cc * 128 + np.arange(128)), r=[halo_d], w=[A_d[cc]])
            G.gather(Gt[:, cc, 0:HAL], 128, halo_all, ("halo", 2 + cc),
                     (lambda b, j, cc=cc: (4 * b + max(j - 1, 0)) * 2 * GW + GW + cc * 128 + np.arange(128)), r=[halo_d], w=[G_d[cc]])
            S.dma(A[:, cc, HAL:], zloc[O_CU + cc * 128:O_CU + (cc + 1) * 128, :], w=[A_d[cc]], acc=True)
            S.dma(Gt[:, cc, HAL:], zloc[O_CU + GW + cc * 128:O_CU + GW + (cc + 1) * 128, :], w=[G_d[cc]], acc=True)
            S.op("dve", lambda E: E.tensor_scalar(out=A[:, cc, 0:HAL], in0=A[:, cc, 0:HAL], scalar1=halo_on[:, 0:1], scalar2=None,
                                                  op0=ALU.mult), r=[A_d[cc], halo_on_d], w=[A_d[cc]])
        for cc in range(2):
            e = "dve" if cc == 0 else "pool"
            S.op("act", lambda E: E.activation(out=Gt[:, cc, :], in_=Gt[:, cc, :], func=AF.Sigmoid), r=[G_d[cc]], w=[G_d[cc]])
            S.op(e, lambda E: E.tensor_tensor(out=A[:, cc, :], in0=A[:, cc, :], in1=Gt[:, cc, :], op=ALU.mult),
                 r=[A_d[cc], G_d[cc]], w=[A_d[cc]])
        for k in range(CW):
            for cc in range(2):
                if k == 0:
                    S.op("dve", lambda E: E.tensor_scalar(out=acc[:, cc, :], in0=A[:, cc, 0:NTK], scalar1=cw[:, cc, 0:1],
                                                          scalar2=cv[:, cc, 0:1], op0=ALU.mult, op1=ALU.add),
                         r=[A_d[cc], cw_d, cv_d], w=[acc_d[cc]])
                else:
                    S.op("dve", lambda E: E.scalar_tensor_tensor(out=acc[:, cc, :], in0=A[:, cc, k:k + NTK], scalar=cw[:, cc, k:k + 1],
                                                                 in1=acc[:, cc, :], op0=ALU.mult, op1=ALU.add),
                         r=[A_d[cc], cw_d, acc_d[cc]], w=[acc_d[cc]])
        for t0 in range(0, NTK, NT):
            n = min(NT, NTK - t0)
            pm, pm_d = nextps()
            for cc in range(2):
                S.op("pe", lambda E: E.matmul(pm[:, 0:n], o256[:], acc[:, cc, t0:t0 + n], start=(cc == 0), stop=(cc == 1)),
                     r=[o256_d, acc_d[cc]], w=[pm_d])
            pv, pv_d = nextps()
            for cc in range(2):
                y_, y_d = yc[cc]
                s_, s_d = sq[cc]
                S.op("dve", lambda E: E.tensor_tensor(out=y_[:, 0:n], in0=acc[:, cc, t0:t0 + n], in1=pm[:, 0:n], op=ALU.subtract),
                     r=[acc_d[cc], pm_d], w=[y_d])
                S.op("act", lambda E: E.activation(out=s_[:, 0:n], in_=y_[:, 0:n], func=AF.Square), r=[y_d], w=[s_d])
                S.op("pe", lambda E: E.matmul(pv[:, 0:n], o256[:], s_[:, 0:n], start=(cc == 0), stop=(cc == 1)),
                     r=[o256_d, s_d], w=[pv_d])
            r_, r_d = rs
            S.op("act", lambda E: E.activation(out=r_[:, 0:n], in_=pv[:, 0:n], func=AF.Sqrt, scale=1.0, bias=cst[:, 0:1]),
                 r=[pv_d, cst_d], w=[r_d])
            S.op("dve", lambda E: E.reciprocal(out=r_[:, 0:n], in_=r_[:, 0:n]), r=[r_d], w=[r_d])
            for cc in range(2):
                y_, y_d = yc[cc]
                o_, o_d = yo[cc]
                e = "dve" if cc == 0 else "pool"
                S.op(e, lambda E: E.tensor_tensor(out=y_[:, 0:n], in0=y_[:, 0:n], in1=r_[:, 0:n], op=ALU.mult),
                     r=[y_d, r_d], w=[y_d])
                S.op("act", lambda E: E.activation(out=o_[:, 0:n], in_=y_[:, 0:n], func=AF.Silu, scale=cv[:, cc, 1:2],
                                                   bias=cv[:, cc, 2:3]), r=[y_d, cv_d], w=[o_d])
                S.dma(yc_loc[cc * 128:(cc + 1) * 128, t0:t0 + n], o_[:, 0:n], r=[o_d])
    S.barrier()
    with ExitStack() as es3:
        sb = lambda n, s, d: S.sb(n, s, d, es3)
        cst = sb("cst2", [128, 4], F32); cst_d = Dep()
        S.op("dve", lambda E: E.memset(cst[:, 1:2], 1.0), w=[cst_d])
        X = sb("X", [64, 3 + SEG], F32); X_d = Dep()
        Gt = sb("Gl", [64, SEG], F32); Gt_d = Dep()
        xc = sb("xc", [64, SEG], F32); xc_d = Dep()
        rt = sb("rt", [64, SEG], F32); rt_d = Dep()
        itl = sb("itl", [64, SEG], F32); it_d = Dep()
        at = sb("at", [64, SEG], F32); at_d = Dep()
        ut = sb("ut", [64, SEG], F32); ut_d = Dep()
        hs = sb("hs", [64, SEG], F32); hs_d = Dep()
        g1 = sb("g1", [64, SEG], F32); g1_d = Dep()
        lcw = sb("lcw", [64, 4], F32); lcw_d = Dep()
        lv = sb("lv", [64, 8], F32); lv_d = Dep()
        lwa = sb("lwa", [64, 64], F32); lwa_d = Dep()
        lwi = sb("lwi", [64, 64], F32); lwi_d = Dep()
        carry = sb("carry", [64, 1], F32); carry_d = Dep()
        S.dma(lcw[:], lcw_dram, w=[lcw_d])
        S.dma(lv[:, 0:4], lv_dram, w=[lv_d])
        S.dma(lwa[:], lwa_dram, w=[lwa_d])
        S.dma(lwi[:], lwi_dram, w=[lwi_d])
        S.op("act", lambda E: E.activation(out=lv[:, 4:5], in_=lv[:, 3:4], func=AF.Exp, scale=-1.0), r=[lv_d], w=[lv_d])
        S.op("act", lambda E: E.activation(out=lv[:, 4:5], in_=lv[:, 4:5], func=AF.Ln, scale=1.0, bias=cst[0:64, 1:2]),
             r=[lv_d, cst_d], w=[lv_d])
        S.op("dve", lambda E: E.tensor_scalar(out=lv[:, 5:6], in0=lv[:, 4:5], scalar1=-16.0, scalar2=None, op0=ALU.mult),
             r=[lv_d], w=[lv_d])
        S.op("dve", lambda E: E.tensor_scalar(out=lv[:, 4:5], in0=lv[:, 4:5], scalar1=-8.0, scalar2=None, op0=ALU.mult),
             r=[lv_d], w=[lv_d])
        S.op("dve", lambda E: E.memset(carry[:], 0.0), w=[carry_d])
        S.op("dve", lambda E: E.memset(X[:, 0:3], 0.0), w=[X_d])
        for s in range(4):
            t0 = s * SEG
            if s > 0:
                S.op("dve", lambda E: E.tensor_copy(out=X[:, 0:3], in_=X[:, SEG:SEG + 3]), r=[X_d], w=[X_d])
            G.gather(X[:, 3:3 + SEG], 64, zall, ("lx", s), (lambda b, j, s=s: (4 * b + s) * DIN + O_LX + 64 * j + np.arange(64)),
                     r=[zall_d], w=[X_d], acc=True)
            G.gather(Gt[:, :], 64, zall, ("lg", s), (lambda b, j, s=s: (4 * b + s) * DIN + O_LG + 64 * j + np.arange(64)),
                     r=[zall_d], w=[Gt_d])
            S.op("dve", lambda E: E.tensor_scalar(out=xc[:, :], in0=X[:, 0:SEG], scalar1=lcw[:, 0:1], scalar2=lv[:, 0:1],
                                                  op0=ALU.mult, op1=ALU.add), r=[X_d, lcw_d, lv_d], w=[xc_d])
            for k in range(1, 4):
                S.op("dve", lambda E: E.scalar_tensor_tensor(out=xc[:, :], in0=X[:, k:k + SEG], scalar=lcw[:, k:k + 1], in1=xc[:, :],
                                                             op0=ALU.mult, op1=ALU.add), r=[X_d, lcw_d, xc_d], w=[xc_d])
            for u0 in range(0, SEG, SUB):
                pa, pa_d = nextps()
                S.op("pe", lambda E: E.matmul(pa[0:64, 0:SUB], lwa[:, :], xc[:, u0:u0 + SUB], start=True, stop=True),
                     r=[lwa_d, xc_d], w=[pa_d])
                S.op("act", lambda E: E.activation(out=rt[:, u0:u0 + SUB], in_=pa[0:64, 0:SUB], func=AF.Sigmoid, scale=1.0,
                                                   bias=lv[:, 1:2]), r=[pa_d, lv_d], w=[rt_d])
                pb, pb_d = nextps()
                S.op("pe", lambda E: E.matmul(pb[0:64, 0:SUB], lwi[:, :], xc[:, u0:u0 + SUB], start=True, stop=True),
                     r=[lwi_d, xc_d], w=[pb_d])
                S.op("act", lambda E: E.activation(out=itl[:, u0:u0 + SUB], in_=pb[0:64, 0:SUB], func=AF.Sigmoid, scale=1.0,
                                                   bias=lv[:, 2:3]), r=[pb_d, lv_d], w=[it_d])
            S.op("act", lambda E: E.activation(out=at[:, :], in_=rt[:, :], func=AF.Exp, scale=lv[:, 4:5]), r=[rt_d, lv_d], w=[at_d])
            S.op("act", lambda E: E.activation(out=ut[:, :], in_=rt[:, :], func=AF.Exp, scale=lv[:, 5:6]), r=[rt_d, lv_d], w=[ut_d])
            S.op("pool", lambda E: E.tensor_scalar(out=ut[:, :], in0=ut[:, :], scalar1=-1.0, scalar2=1.0, op0=ALU.mult, op1=ALU.add),
                 r=[ut_d], w=[ut_d])
            S.op("act", lambda E: E.activation(out=ut[:, :], in_=ut[:, :], func=AF.Sqrt), r=[ut_d], w=[ut_d])
            S.op("pool", lambda E: E.tensor_tensor(out=itl[:, :], in0=itl[:, :], in1=xc[:, :], op=ALU.mult), r=[it_d, xc_d], w=[it_d])
            S.op("pool", lambda E: E.tensor_tensor(out=ut[:, :], in0=ut[:, :], in1=itl[:, :], op=ALU.mult), r=[ut_d, it_d], w=[ut_d])
            S.op("dve", lambda E: E.tensor_tensor_scan(out=hs[:, :], data0=at[:, :], data1=ut[:, :], initial=carry[:, 0:1],
                                                       op0=ALU.mult, op1=ALU.add), r=[at_d, ut_d, carry_d], w=[hs_d])
            S.op("dve", lambda E: E.tensor_copy(out=carry[:, :], in_=hs[:, SEG - 1:SEG]), r=[hs_d], w=[carry_d])
            S.op("act", lambda E: E.activation(out=g1[:, :], in_=Gt[:, :], func=AF.Square), r=[Gt_d], w=[g1_d])
            S.op("pool", lambda E: E.tensor_scalar(out=g1[:, :], in0=g1[:, :], scalar1=0.044715, scalar2=1.0, op0=ALU.mult, op1=ALU.add),
                 r=[g1_d], w=[g1_d])
            S.op("pool", lambda E: E.tensor_tensor(out=g1[:, :], in0=g1[:, :], in1=Gt[:, :], op=ALU.mult), r=[g1_d, Gt_d], w=[g1_d])
            S.op("act", lambda E: E.activation(out=g1[:, :], in_=g1[:, :], func=AF.Sigmoid, scale=1.5957691216057308), r=[g1_d], w=[g1_d])
            S.op("pool", lambda E: E.tensor_tensor(out=g1[:, :], in0=g1[:, :], in1=Gt[:, :], op=ALU.mult), r=[g1_d, Gt_d], w=[g1_d])
            S.op("dve", lambda E: E.tensor_tensor(out=hs[:, :], in0=hs[:, :], in1=g1[:, :], op=ALU.mult), r=[hs_d, g1_d], w=[hs_d])
            S.dma(yx_loc[YL0 + s * NBK * 64:YL0 + (s + 1) * NBK * 64, :].rearrange("(i d) t -> d i t", d=64),
                  hs[:, :].rearrange("d (i t) -> d i t", t=CH), r=[hs_d])


def dsa_phase(S, PS, G, es, T, zall, zall_d, zq_all, zq_d, joff, joff_d, kvg_dram, ikg_dram, wuk_dram, wuv_dram,
              yx_loc, YD0, NR=22):
    NTK = T // 4
    NCH = T // CH
    NB = NCH // 4
    NBK = NTK // CH
    LAT = 128
    sb = lambda n, s, d: S.sb(n, s, d, es)
    psA = PS.f[0:3]
    pso = PS.f[3]
    psd = PS.f[4]
    ai = [0]

    def nextA():
        p = psA[ai[0] % 3]
        ai[0] += 1
        return p

    nextT = PS.nb
    cst = sb("cst", [128, 4], F32); cst_d = Dep()
    S.op("dve", lambda E: E.memset(cst[:, 0:1], 1e-6), w=[cst_d])
    S.op("dve", lambda E: E.memset(cst[:, 1:2], 1e-5), w=[cst_d])
    idf = sb("idf", [128, 128], F32); idf_d = Dep()
    ident = sb("ident", [128, 128], BF16); ident_d = Dep()
    identf = sb("identf", [128, 128], F32); identf_d = Dep()
    S.op("pool", lambda E: E.iota(idf[:], [[1, 128]], base=0, channel_multiplier=-1, allow_small_or_imprecise_dtypes=True), w=[idf_d])
    S.op("dve", lambda E: E.tensor_scalar(out=ident[:], in0=idf[:], scalar1=0.0, scalar2=None, op0=ALU.is_equal), r=[idf_d], w=[ident_d])
    S.op("dve", lambda E: E.tensor_scalar(out=identf[:], in0=idf[:], scalar1=0.0, scalar2=None, op0=ALU.is_equal), r=[idf_d], w=[identf_d])
    onesb = sb("onesb", [128, 128], BF16); onesb_d = Dep()
    S.op("dve", lambda E: E.memset(onesb[:], 1.0), w=[onesb_d])
    onesL = sb("onesL", [128, 128], F32); onesL_d = Dep()
    S.op("dve", lambda E: E.memset(onesL[:], 1.0 / LAT), w=[onesL_d])
    ones32 = sb("ones32", [32, 32], F32); ones32_d = Dep()
    S.op("dve", lambda E: E.memset(ones32[:], 1.0 / 32), w=[ones32_d])
    negm = sb("negm", [CH, GRP], F32); negm_d = Dep()
    S.op("pool", lambda E: E.iota(negm[:], [[1, GRP]], base=0, channel_multiplier=-1, allow_small_or_imprecise_dtypes=True), w=[negm_d])
    S.op("dve", lambda E: E.tensor_scalar(out=negm[:], in0=negm[:], scalar1=joff[0:CH, 0:1], scalar2=NEG, op0=ALU.is_gt, op1=ALU.mult),
         r=[negm_d, joff_d], w=[negm_d])
    ctok = sb("ctok", [CH, NCH, LAT], BF16); ctok_d = Dep()
    cT = sb("cT", [LAT, T], BF16); cT_d = Dep()
    kiT = sb("kiT", [32, T], F32); kiT_d = Dep()
    wuk = sb("wuk", [64, 4, LAT], F32); wuk_d = Dep()
    wuv = sb("wuv", [LAT, 4, 64], BF16); wuv_d = Dep()
    kvg = sb("kvg", [LAT, 1], F32); kvg_d = Dep()
    ikg = sb("ikg", [32, 2], F32); ikg_d = Dep()
    S.dma(kvg[:], kvg_dram, w=[kvg_d])
    S.dma(ikg[:], ikg_dram, w=[ikg_d])
    S.dma(wuk[:], wuk_dram, w=[wuk_d])

    with ExitStack() as es2:
        sb2 = lambda n, s, d: S.sb(n, s, d, es2)
        dkvT = sb2("dkvT", [LAT, T], F32); dkvT_d = Dep()
        ikT = sb2("ikT", [32, T], F32); ikT_d = Dep()
        sq = [(sb2("sqp%d" % i, [LAT, GRP], F32), Dep()) for i in range(2)]
        rs = [(sb2("rsp%d" % i, [LAT, GRP], F32), Dep()) for i in range(2)]
        wst = sb2("wst", [128, 256], F32); wst_d = Dep()
        S.dma(wst[:, :], wuv_dram.rearrange("p a b -> p (a b)"), w=[wst_d])
        S.op("dve", lambda E: E.tensor_copy(out=wuv[:].rearrange("p a b -> p (a b)"), in_=wst[:, :]), r=[wst_d], w=[wuv_d])
        for s in range(4):
            G.gather(dkvT[:, s * NTK:(s + 1) * NTK], LAT, zall, ("dkv", s),
                     (lambda b, j, s=s: (4 * b + s) * DIN + O_DKV + np.arange(LAT)), r=[zall_d], w=[dkvT_d], acc=(s > 0))
            G.gather(ikT[:, s * NTK:(s + 1) * NTK], 32, zall, ("ik", s),
                     (lambda b, j, s=s: (4 * b + s) * DIN + O_IK + np.arange(32)), r=[zall_d], w=[ikT_d], acc=(s > 0))
        for g in range(T // GRP):
            gs = slice(g * GRP, (g + 1) * GRP)
            s_, s_d = sq[g % 2]
            r_, r_d = rs[g % 2]
            S.op("act", lambda E: E.activation(out=s_[:, :], in_=dkvT[:, gs], func=AF.Square), r=[dkvT_d], w=[s_d])
            pm, pm_d = nextA()
            S.op("pe", lambda E: E.matmul(pm[:, 0:GRP], onesL[:, :], s_[:, :], start=True, stop=True), r=[onesL_d, s_d], w=[pm_d])
            S.op("act", lambda E: E.activation(out=r_[:, :], in_=pm[:, 0:GRP], func=AF.Sqrt, scale=1.0, bias=cst[:, 0:1]),
                 r=[pm_d, cst_d], w=[r_d])
            S.op("dve", lambda E: E.reciprocal(out=r_[:, :], in_=r_[:, :]), r=[r_d], w=[r_d])
            S.op("dve", lambda E: E.scalar_tensor_tensor(out=cT[:, gs], in0=dkvT[:, gs], scalar=kvg[:, 0:1], in1=r_[:, :],
                                                         op0=ALU.mult, op1=ALU.mult), r=[dkvT_d, kvg_d, r_d], w=[cT_d])
            pmu, pmu_d = nextA()
            S.op("pe", lambda E: E.matmul(pmu[0:32, 0:GRP], ones32[:, :], ikT[:, gs], start=True, stop=True), r=[ones32_d, ikT_d], w=[pmu_d])
            S.op("dve", lambda E: E.tensor_tensor(out=ikT[:, gs], in0=ikT[:, gs], in1=pmu[0:32, 0:GRP], op=ALU.subtract),
                 r=[ikT_d, pmu_d], w=[ikT_d])
            s2_, s2_d = sq[(g + 1) % 2]
            S.op("act", lambda E: E.activation(out=s2_[0:32, :], in_=ikT[:, gs], func=AF.Square), r=[ikT_d], w=[s2_d])
            pvr, pvr_d = nextA()
            S.op("pe", lambda E: E.matmul(pvr[0:32, 0:GRP], ones32[:, :], s2_[0:32, :], start=True, stop=True), r=[ones32_d, s2_d], w=[pvr_d])
            r2_, r2_d = rs[(g + 1) % 2]
            S.op("act", lambda E: E.activation(out=r2_[0:32, :], in_=pvr[0:32, 0:GRP], func=AF.Sqrt, scale=1.0, bias=cst[0:32, 1:2]),
                 r=[pvr_d, cst_d], w=[r2_d])
            S.op("dve", lambda E: E.reciprocal(out=r2_[0:32, :], in_=r2_[0:32, :]), r=[r2_d], w=[r2_d])
            S.op("dve", lambda E: E.tensor_tensor(out=ikT[:, gs], in0=ikT[:, gs], in1=r2_[0:32, :], op=ALU.mult), r=[ikT_d, r2_d], w=[ikT_d])
            S.op("dve", lambda E: E.tensor_scalar(out=kiT[:, gs], in0=ikT[:, gs], scalar1=ikg[:, 0:1], scalar2=ikg[:, 1:2],
                                                  op0=ALU.mult, op1=ALU.add), r=[ikT_d, ikg_d], w=[kiT_d])
            pT, pT_d = nextT()
            for k in range(4):
                c = 4 * g + k
                S.op("pe", lambda E: E.transpose(out=pT[0:CH, k * LAT:(k + 1) * LAT], in_=cT[:, c * CH:(c + 1) * CH], identity=ident[:, :]),
                     r=[cT_d, ident_d], w=[pT_d])
            S.op("act", lambda E: E.copy(out=ctok[:, 4 * g:4 * g + 4, :], in_=pT[0:CH, 0:4 * LAT].rearrange("p (a b) -> p a b", a=4)),
                 r=[pT_d], w=[ctok_d])
    S.barrier()

    I = sb("I", [CH, T], F32); I_d = Dep()
    mask = sb("mask", [CH, T], BF16); mask_d = Dep()
    rt = [(sb("rt%d" % i, [CH, GRP], F32), Dep()) for i in range(3)]
    Et = [(sb("Et%d" % i, [CH, GRP], BF16), Dep()) for i in range(2)]
    Pt = [(sb("Pt%d" % i, [CH, 4, CH], BF16), Dep()) for i in range(2)]
    qlat = sb("qlat", [LAT, GRP], BF16); qlat_d = Dep()
    sv = sb("sv", [CH, 8], F32); sv_d = Dep()
    rden = sb("rden", [LAT, GRP], F32); rden_d = Dep()
    ob = sb("ob", [LAT, GRP], BF16); ob_d = Dep()
    ysb = sb("ysb", [64, GRP], F32); ysb_d = Dep()
    dqb = [(sb("dqb%d" % i, [64, 4, CH], F32), Dep()) for i in range(2)]
    iqb = [(sb("iqb%d" % i, [32, 8, CH], F32), Dep()) for i in range(2)]
    iwb = [(sb("iwb%d" % i, [8, CH], F32), Dep()) for i in range(2)]
    w16 = [(sb("w16_%d" % i, [CH, 8], F32), Dep()) for i in range(2)]
    ri = 0
    ei = 0

    def qrow(m, f0):
        return lambda b, j, m=m, f0=f0: ((4 * b + (4 * m + j) // NBK) * NBK + (4 * m + j) % NBK) * ZQR + f0

    for m in range(NB):
        ng = m + 1
        nk = ng * GRP
        dqT, dqT_d = dqb[m % 2]
        iqT, iqT_d = iqb[m % 2]
        iwT, iwT_d = iwb[m % 2]
        w_, w_d = w16[m % 2]
        for h in range(4):
            G.gather(dqT[:, h, :], 64, zq_all, ("dq", m, h),
                     (lambda b, j, m=m, h=h: qrow(m, O_DQ - ZQ0 + 64 * h)(b, j) + np.arange(64)), r=[zq_d], w=[dqT_d], acc=(h > 0))
        for h in range(8):
            G.gather(iqT[:, h, :], 32, zq_all, ("iq", m, h),
                     (lambda b, j, m=m, h=h: qrow(m, O_IQ - ZQ0 + 32 * h)(b, j) + np.arange(32)), r=[zq_d], w=[iqT_d], acc=(h > 0))
        G.gather(iwT[:, :], 8, zq_all, ("iw", m), (lambda b, j, m=m: qrow(m, O_IW - ZQ0)(b, j) + np.arange(8)), r=[zq_d], w=[iwT_d])
        pw_, pw_d = nextA()
        S.op("pe", lambda E: E.transpose(out=pw_[0:CH, 0:8], in_=iwT[:, :], identity=identf[0:8, 0:8]), r=[iwT_d, identf_d], w=[pw_d])
        S.op("act", lambda E: E.mul(out=w_[:, :], in_=pw_[0:CH, 0:8], mul=1.0 / 16), r=[pw_d], w=[w_d])
        pq, pq_d = nextA()
        for h in range(4):
            S.op("pe", lambda E: E.matmul(pq[:, h * CH:(h + 1) * CH], wuk[:, h, :], dqT[:, h, :], start=True, stop=True),
                 r=[wuk_d, dqT_d], w=[pq_d])
        S.op("act", lambda E: E.mul(out=qlat[:, :], in_=pq[:, 0:GRP], mul=0.125), r=[pq_d], w=[qlat_d])
        for g in range(ng):
            ks = slice(g * GRP, (g + 1) * GRP)
            for h in range(8):
                px, px_d = nextA()
                S.op("pe", lambda E: E.matmul(px[0:CH, 0:GRP], iqT[:, h, :], kiT[:, ks], start=True, stop=True),
                     r=[iqT_d, kiT_d], w=[px_d])
                r_, r_d = rt[ri % 3]
                ri += 1
                S.op("act", lambda E: E.activation(out=r_[:, :], in_=px[0:CH, 0:GRP], func=AF.Relu), r=[px_d], w=[r_d])
                if h == 0:
                    S.op("dve", lambda E: E.tensor_scalar(out=I[:, ks], in0=r_[:, :], scalar1=w_[:, 0:1], scalar2=None, op0=ALU.mult),
                         r=[r_d, w_d], w=[I_d])
                else:
                    S.op("dve", lambda E: E.scalar_tensor_tensor(out=I[:, ks], in0=r_[:, :], scalar=w_[:, h:h + 1], in1=I[:, ks],
                                                                 op0=ALU.mult, op1=ALU.add), r=[r_d, w_d, I_d], w=[I_d])
        S.op("dve", lambda E: E.tensor_reduce(out=sv[:, 0:1], in_=I[:, 0:nk], axis=AX.X, op=ALU.min), r=[I_d], w=[sv_d])
        S.op("dve", lambda E: E.tensor_reduce(out=sv[:, 5:6], in_=I[:, 0:nk], axis=AX.X, op=ALU.max), r=[I_d], w=[sv_d])
        S.op("dve", lambda E: E.tensor_tensor(out=sv[:, 1:2], in0=sv[:, 5:6], in1=sv[:, 0:1], op=ALU.subtract), r=[sv_d], w=[sv_d])
        S.op("dve", lambda E: E.tensor_scalar(out=sv[:, 1:2], in0=sv[:, 1:2], scalar1=1.0001, scalar2=1e-20, op0=ALU.mult, op1=ALU.add),
             r=[sv_d], w=[sv_d])
        S.op("dve", lambda E: E.tensor_tensor(out=I[:, m * GRP:nk], in0=I[:, m * GRP:nk], in1=negm[:, :], op=ALU.add),
             r=[I_d, negm_d], w=[I_d])
        for n in range(1, NR + 1):
            S.op("dve", lambda E: E.scalar_tensor_tensor(out=sv[:, 2:3], in0=sv[:, 1:2], scalar=float(2.0 ** -n), in1=sv[:, 0:1],
                                                         op0=ALU.mult, op1=ALU.add), r=[sv_d], w=[sv_d])
            S.op("dve", lambda E: E.tensor_scalar(out=mask[:, 0:nk], in0=I[:, 0:nk], scalar1=sv[:, 2:3], scalar2=None,
                                                  op0=ALU.is_ge, op1=ALU.add, accum_out=sv[:, 3:4]), r=[I_d, sv_d], w=[mask_d, sv_d])
            S.op("dve", lambda E: E.tensor_scalar(out=sv[:, 4:5], in0=sv[:, 3:4], scalar1=float(TOPK), scalar2=float(2.0 ** -n),
                                                  op0=ALU.is_ge, op1=ALU.mult), r=[sv_d], w=[sv_d])
            S.op("dve", lambda E: E.scalar_tensor_tensor(out=sv[:, 0:1], in0=sv[:, 4:5], scalar=sv[:, 1:2], in1=sv[:, 0:1],
                                                         op0=ALU.mult, op1=ALU.add), r=[sv_d], w=[sv_d])
        S.op("dve", lambda E: E.tensor_scalar(out=mask[:, 0:nk], in0=I[:, 0:nk], scalar1=sv[:, 0:1], scalar2=None, op0=ALU.is_ge),
             r=[I_d, sv_d], w=[mask_d])
        po, po_d = pso
        pd, pd_d = psd
        nck = 4 * ng
        for c in range(nck):
            cs = slice(c * CH, (c + 1) * CH)
            pT, pT_d = nextT()
            S.op("pe", lambda E: E.transpose(out=pT[0:CH, 0:CH], in_=mask[:, cs], identity=ident[0:CH, 0:CH]),
                 r=[mask_d, ident_d], w=[pT_d])
            pS, pS_d = nextA()
            S.op("pe", lambda E: E.matmul(pS[0:CH, 0:GRP], cT[:, cs], qlat[:, :], start=True, stop=True),
                 r=[cT_d, qlat_d], w=[pS_d])
            e_, e_d = Et[ei % 2]
            p_, p_d = Pt[ei % 2]
            ei += 1
            S.op("act", lambda E: E.activation(out=e_[:, :], in_=pS[0:CH, 0:GRP], func=AF.Exp), r=[pS_d], w=[e_d])
            S.op("dve", lambda E: E.tensor_tensor(out=p_[:, :, :], in0=e_[:, :].rearrange("p (h q) -> p h q", h=4),
                                                  in1=pT[0:CH, 0:CH].unsqueeze(1).to_broadcast([CH, 4, CH]), op=ALU.mult),
                 r=[e_d, pT_d], w=[p_d])
            pf = p_[:, :, :].rearrange("p h q -> p (h q)")
            S.op("pe", lambda E: E.matmul(po[:, 0:GRP], ctok[:, c, :], pf, start=(c == 0), stop=(c == nck - 1)),
                 r=[ctok_d, p_d], w=[po_d])
            S.op("pe", lambda E: E.matmul(pd[:, 0:GRP], onesb[0:CH, :], pf, start=(c == 0), stop=(c == nck - 1)),
                 r=[onesb_d, p_d], w=[pd_d])
        S.op("dve", lambda E: E.reciprocal(out=rden[:, :], in_=pd[:, 0:GRP]), r=[pd_d], w=[rden_d])
        S.op("dve", lambda E: E.tensor_tensor(out=ob[:, :], in0=po[:, 0:GRP], in1=rden[:, :], op=ALU.mult), r=[po_d, rden_d], w=[ob_d])
        py, py_d = nextA()
        for h in range(4):
            S.op("pe", lambda E: E.matmul(py[0:64, h * CH:(h + 1) * CH], wuv[:, h, :], ob[:, h * CH:(h + 1) * CH], start=True, stop=True),
                 r=[wuv_d, ob_d], w=[py_d])
        S.op("act", lambda E: E.copy(out=ysb[:, :], in_=py[0:64, 0:GRP]), r=[py_d], w=[ysb_d])
        S.dma(yx_loc[YD0 + m * 256:YD0 + (m + 1) * 256, :].rearrange("(h d) t -> d h t", d=64),
              ysb[:, :].rearrange("d (h t) -> d h t", h=4), r=[ysb_d])


NGCOLS = 448


def build_fused(T, NL):
    NTK = T // 4
    NCH = T // CH
    NB = NCH // 4
    NBK = NTK // CH
    NT = 342 if NTK % 342 == 0 else 228
    YF0 = 0
    YL0 = NCH * 64
    YD0 = 2 * NCH * 64
    YR = YD0 + NB * 256
    nc = bass.Bass("TRN2", target_bir_lowering=False)
    dt = lambda name, shape, kind, dtype=F32: nc.dram_tensor(name, list(shape), dtype, kind=kind).ap()
    h0 = dt("h0", [D, NTK], "ExternalInput")
    gains = dt("gains", [NL, 128, 6 * KC], "ExternalInput")
    fwin = dt("fwin", [NL * 2, 128, KC, 2 * DFF], "ExternalInput")
    fwout = dt("fwout", [NL * 2, 128, JC, D], "ExternalInput")
    pw = dt("pw", [NL, 128, KC, DIN], "ExternalInput")
    ow = dt("ow", [NL, 128, KC, D], "ExternalInput")
    bfi = dt("bfi", [NL, 1, 1], "ExternalInput")
    cwi = dt("cwi", [NL, 128, 2, CW], "ExternalInput")
    cvi = dt("cvi", [NL, 128, 2, 3], "ExternalInput")
    lcwi = dt("lcwi", [NL, 64, 4], "ExternalInput")
    lvi = dt("lvi", [NL, 64, 4], "ExternalInput")
    lwai = dt("lwai", [NL, 64, 64], "ExternalInput")
    lwii = dt("lwii", [NL, 64, 64], "ExternalInput")
    kvgi = dt("kvgi", [NL, 128, 1], "ExternalInput")
    ikgi = dt("ikgi", [NL, 32, 2], "ExternalInput")
    wuki = dt("wuki", [NL, 64, 4, 128], "ExternalInput")
    wuvi = dt("wuvi", [NL, 128, 4, 64], "ExternalInput")
    cori = dt("cori", [128, 2], "ExternalInput")
    gidx = dt("gidx", [128, NGCOLS], "ExternalInput", I32)
    h_out = dt("h_out", [D, NTK], "ExternalOutput")
    hbuf = dt("hbuf", [D, NTK], "Internal")
    zloc = dt("zloc", [DIN, NTK], "Internal")
    zq_loc = dt("zq_loc", [NBK * ZQR, CH], "Internal")
    halo_loc = dt("halo_loc", [2 * GW, HAL], "Internal")
    yx_loc = dt("yx_loc", [YR, CH], "Internal")
    yc_loc = dt("yc_loc", [GW, NTK], "Internal")
    zall = [dt("zall%d" % i, [NCORES * DIN, NTK], "Internal") for i in range(2)]
    zq_all = [dt("zq_all%d" % i, [NCORES * NBK * ZQR, CH], "Internal") for i in range(2)]
    halo_all = [dt("halo_all%d" % i, [NCORES * 2 * GW, HAL], "Internal") for i in range(2)]
    yx_all = [dt("yx_all%d" % i, [NCORES * YR, CH], "Internal") for i in range(2)]
    es = ExitStack()
    with es:
        S = Sched(nc, es)
        PS = PsumPool(S)
        G = Gather(S, NGCOLS)
        G.load(gidx)
        cor = S.sb("cor", [128, 2], F32); cor_d = Dep()
        S.dma(cor[:], cori, w=[cor_d])
        for l in range(NL):
            zA, zqA, hA, yA = zall[l % 2], zq_all[l % 2], halo_all[l % 2], yx_all[l % 2]
            zall_d, zq_d, halo_d, yx_d = Dep(), Dep(), Dep(), Dep()
            with ExitStack() as e1:
                TS = TStage(S, PS, NT, e1)
                TS.load_gains(gains[l])
                TS.load_ffn_weights(fwin[2 * l], fwout[2 * l], 0)
                TS.ffn_phase(h0 if l == 0 else hbuf, hbuf, NTK, 1)
                TS.load_sq_weights(pw[l], DIN, gi=2)
                TS.proj_phase(hbuf, zloc, zq_loc, halo_loc, NTK)
            S.barrier()
            S.collective("AllGather", zloc, zA, zall_d)
            S.collective("AllGather", zq_loc, zqA, zq_d)
            S.collective("AllGather", halo_loc, hA, halo_d)
            with ExitStack() as e2:
                fox_phase(S, PS, G, e2, T, zA, zall_d, bfi[l], yx_loc)
            S.barrier()
            cl_phase(S, PS, G, None, T, zloc, zA, zall_d, hA, halo_d, cor[:, 1:2], cor_d,
                     cwi[l], cvi[l], lcwi[l], lvi[l], lwai[l], lwii[l], yc_loc, yx_loc, YL0)
            S.barrier()
            with ExitStack() as e4:
                dsa_phase(S, PS, G, e4, T, zA, zall_d, zqA, zq_d, cor[:, 0:1], cor_d, kvgi[l], ikgi[l], wuki[l], wuvi[l],
                          yx_loc, YD0)
            S.barrier()
            S.collective("AllGather", yx_loc, yA, yx_d)
            with ExitStack() as e5:
                TS = TStage(S, PS, NT, e5)
                TS.load_gains(gains[l])
                TS.load_sq_weights(ow[l], D)

                def fill_y(sq, sq_d, t0, n, yA=yA, yx_d=yx_d):
                    S.dma(sq[:, 2:4, 0:n], yc_loc[:, t0:t0 + n].rearrange("(c p) n -> p c n", p=128), w=[sq_d])
                    for kb in range(n // CH):
                        blk = t0 // CH + kb
                        cs = slice(kb * CH, (kb + 1) * CH)
                        for kk in range(2):
                            p = np.arange(128)
                            for base, kc0, nm in ((YF0, 0, "yf"), (YL0, 4, "yl")):
                                G.gather(sq[:, kc0 + kk, cs], 128, yA, (nm, blk, kk),
                                         (lambda b, j, base=base, blk=blk, kk=kk:
                                          (4 * b + 2 * kk + p // 64) * YR + base + (j * NBK + blk) * 64 + p % 64),
                                         r=[yx_d], w=[sq_d], acc=True)
                            G.gather(sq[:, 6 + kk, cs], 128, yA, ("yd", blk, kk),
                                     (lambda b, j, blk=blk, kk=kk:
                                      (4 * b + (j * NBK + blk) % 4) * YR + YD0 + ((j * NBK + blk) // 4) * 256 + kk * 128 + p),
                                     r=[yx_d], w=[sq_d], acc=True)

                TS.mixin_phase(hbuf, hbuf, NTK, 3, fill_y)
                TS.load_ffn_weights(fwin[2 * l + 1], fwout[2 * l + 1], 4)
                TS.ffn_phase(hbuf, h_out if l == NL - 1 else hbuf, NTK, 5)
            S.barrier()
        S.finish()
        print("fused ops", S.nops, "waits", S.nwaits, "gather cols", len(G.specs))
    return nc, G


_PROGS = {}


def _f32(a):
    return np.ascontiguousarray(a, dtype=np.float32)


def fused_inputs(G, T, x, meta_tokens, norm_g, ffn_w_in, ffn_w_out, w_in, w_out, fox_b_f,
                 conv_dw_w, conv_dw_b, conv_ln_g, conv_ln_b,
                 lru_conv_w, lru_conv_b, lru_w_a, lru_b_a, lru_w_i, lru_b_i, lru_lambda,
                 dsa_kv_norm_g, dsa_w_uk, dsa_w_uv, idx_k_ln_g, idx_k_ln_b):
    A = lambda a: np.asarray(a, dtype=np.float32)
    NL = norm_g.shape[0]
    B = x.shape[0]
    NTK = T // 4
    h = np.concatenate([np.broadcast_to(A(meta_tokens)[None], (B, NMETA, D)), A(x)], axis=1)
    shared = {
        "gains": _f32(np.stack([lay_gains(A(norm_g[l])) for l in range(NL)])),
        "fwin": _f32(np.stack([lay_kc(A(ffn_w_in[l, i])) for l in range(NL) for i in range(2)])),
        "fwout": _f32(np.stack([lay_kc(A(ffn_w_out[l, i])) for l in range(NL) for i in range(2)])),
        "pw": _f32(np.stack([lay_kc(A(w_in[l])) for l in range(NL)])),
        "ow": _f32(np.stack([lay_kc(A(w_out[l])) for l in range(NL)])),
        "cwi": _f32(np.stack([A(conv_dw_w[l]).T.reshape(2, 128, CW).transpose(1, 0, 2) for l in range(NL)])),
        "cvi": _f32(np.stack([np.stack([A(conv_dw_b[l]), A(conv_ln_g[l]), A(conv_ln_b[l])], -1).reshape(2, 128, 3).transpose(1, 0, 2)
                              for l in range(NL)])),
        "kvgi": _f32(A(dsa_kv_norm_g).reshape(NL, 128, 1)),
        "ikgi": _f32(np.stack([A(idx_k_ln_g), A(idx_k_ln_b)], -1)),
        "wuki": _f32(A(dsa_w_uk).transpose(0, 3, 1, 2)),
        "wuvi": _f32(A(dsa_w_uv).transpose(0, 2, 1, 3)),
    }
    lvec = np.stack([A(lru_conv_b), A(lru_b_a), A(lru_b_i), A(lru_lambda)], -1)
    maps = []
    for c in range(NCORES):
        b, j = c // 4, c % 4
        m = dict(shared)
        m["h0"] = _f32(h[b, j * NTK:(j + 1) * NTK].T)
        m["bfi"] = _f32(A(fox_b_f)[:, j].reshape(NL, 1, 1))
        m["lcwi"] = _f32(A(lru_conv_w)[:, :, 64 * j:64 * j + 64].transpose(0, 2, 1))
        m["lvi"] = _f32(lvec[:, 64 * j:64 * j + 64])
        m["lwai"] = _f32(A(lru_w_a)[:, j])
        m["lwii"] = _f32(A(lru_w_i)[:, j])
        cor = np.zeros((128, 2), np.float32)
        cor[:, 0] = CH * j
        cor[:, 1] = 1.0 if j > 0 else 0.0
        m["cori"] = cor
        m["gidx"] = G.table(b, j)
        maps.append(m)
    return maps


def kernel(x, **params):
    x = np.asarray(x, dtype=np.float32)
    B = x.shape[0]
    T = T_FULL
    NTK = T // 4
    NL = params["norm_g"].shape[0]
    if "F" not in _PROGS:
        _PROGS["F"] = build_fused(T, NL)
    nc, G = _PROGS["F"]
    maps = fused_inputs(G, T, x, **params)
    res = run_bass_kernel_spmd(nc, maps, core_ids=list(range(NCORES))).results
    out = np.stack([np.concatenate([res[4 * b + j]["h_out"].T for j in range(4)], axis=0)[NMETA:] for b in range(B)], axis=0)
    return np.ascontiguousarray(out, dtype=np.float32)
```

```python
import numpy as np
from contextlib import ExitStack
import concourse.bass as bass
import concourse.mybir as mybir
from concourse.bass_utils import run_bass_kernel_spmd

F32 = mybir.dt.float32
BF16 = mybir.dt.bfloat16
I32 = mybir.dt.int32
AF = mybir.ActivationFunctionType
ALU = mybir.AluOpType
AX = mybir.AxisListType

D = 1024
DFF = 2560
KC = D // 128
JC = DFF // 128
DIN = 2476
NCORES = 8
NMETA = 16
SEQ = 8192
T_FULL = SEQ + NMETA
CH = 114
GRP = 4 * CH


class Dep:
    __slots__ = ("w", "r")

    def __init__(self):
        self.w = []
        self.r = {}


class Sched:
    def __init__(self, nc, es, n_dma_sems=28):
        self.nc = nc
        self.es = es
        self.eng = {"pe": nc.tensor, "act": nc.scalar, "dve": nc.vector,
                    "pool": nc.gpsimd, "sp": nc.sync}
        self.sems = {}
        for k in self.eng:
            self.sems[k] = es.enter_context(nc.semaphore("c_" + k))
        self.cnt = {k: 0 for k in self.eng}
        self.seen = {k: {} for k in self.eng}
        self.dpool = []
        for i in range(n_dma_sems):
            key = "d%d" % i
            self.sems[key] = es.enter_context(nc.semaphore(key))
            self.dpool.append([key, 0])
        self.dnext = 0
        self.ccs = []
        self.nwaits = 0
        self.nops = 0
        self._uid = 0

    def sb(self, name, shape, dtype, es=None):
        self._uid += 1
        return (es or self.es).enter_context(
            self.nc.sbuf_tensor("sb%d_%s" % (self._uid, name), list(shape), dtype))

    def ps(self, name, shape, dtype=F32, es=None):
        self._uid += 1
        return (es or self.es).enter_context(
            self.nc.psum_tensor("ps%d_%s" % (self._uid, name), list(shape), dtype))

    def _wait(self, eng, ev):
        if ev is None:
            return
        key, val = ev
        if key == eng and eng in ("pe", "sp"):
            return
        if self.seen[eng].get(key, 0) >= val:
            return
        self.eng[eng].wait_ge(self.sems[key], val)
        self.seen[eng][key] = val
        self.nwaits += 1

    def _deps(self, eng, r, w, acc=False):
        for d in r:
            for ev in d.w:
                self._wait(eng, ev)
        for d in w:
            for ev in d.w:
                if acc and ev[0] not in self.eng:
                    continue
                self._wait(eng, ev)
            for e, ev in d.r.items():
                self._wait(eng, ev)

    def op(self, eng, fn, r=(), w=()):
        self._deps(eng, r, w)
        ins = fn(self.eng[eng])
        self.cnt[eng] += 1
        ins.then_inc(self.sems[eng], 1)
        ev = (eng, self.cnt[eng])
        for d in r:
            d.r[eng] = ev
        for d in w:
            d.w = [ev]
            d.r = {}
        self.nops += 1
        return ev

    def dma(self, out, in_, r=(), w=(), q="sp", acc=False, fn=None, **kw):
        self._deps(q, r, w, acc=acc)
        slot = self.dpool[self.dnext]
        self.dnext = (self.dnext + 1) % len(self.dpool)
        key, val = slot
        if val > 0:
            self._wait(q, (key, val))
        if fn is None:
            ins = self.eng[q].dma_start(out=out, in_=in_, **kw)
        else:
            ins = fn(self.eng[q])
        ins.then_inc(self.sems[key], 16)
        slot[1] = val + 16
        ev = (key, val + 16)
        for d in r:
            d.r[key] = ev
        for d in w:
            if acc:
                d.w = d.w + [ev]
            else:
                d.w = [ev]
                d.r = {}
        self.nops += 1
        return ev

    def collective(self, kind, in_ap, out_ap, w_dep):
        key = "cc%d" % len(self.ccs)
        self.sems[key] = self.es.enter_context(self.nc.semaphore(key))
        self.ccs.append(key)
        ins = self.nc.gpsimd.collective_compute(kind, ALU.bypass, replica_groups=[list(range(NCORES))],
                                                ins=[in_ap], outs=[out_ap])
        ins.then_inc(self.sems[key], 1)
        w_dep.w = [(key, 1)]
        w_dep.r = {}
        self.nops += 1

    def barrier(self):
        for e in ("pe", "act", "dve", "pool", "sp"):
            for key, val in self.dpool:
                if val > 0:
                    self._wait(e, (key, val))
            for key in self.ccs:
                self._wait(e, (key, 1))
            for k in ("pe", "act", "dve", "pool"):
                if k != e and self.cnt[k] > 0:
                    self._wait(e, (k, self.cnt[k]))
            if e not in ("pe", "sp") and self.cnt[e] > 0:
                self._wait(e, (e, self.cnt[e]))

    def finish(self):
        self.barrier()


class PsumPool:
    def __init__(self, S):
        self.f = [(S.ps("pf%d" % i, [128, 512]), Dep()) for i in range(6)]
        self.b = [(S.ps("pb%d" % i, [128, 1024], BF16), Dep()) for i in range(2)]
        self.fi = 0
        self.bi = 0

    def nf(self, lo=0, hi=6):
        p = self.f[lo + self.fi % (hi - lo)]
        self.fi += 1
        return p

    def nb(self):
        p = self.b[self.bi % 2]
        self.bi += 1
        return p


class Gather:
    def __init__(self, S, ncols):
        self.S = S
        self.ncols = ncols
        self.idx = S.sb("gidx", [128, ncols], I32)
        self.idx_d = Dep()
        self.specs = []
        self.memo = {}

    def load(self, idx_dram):
        self.S.dma(self.idx[:], idx_dram, w=[self.idx_d])

    def col(self, key, P, fn):
        if key not in self.memo:
            assert len(self.specs) < self.ncols
            self.memo[key] = len(self.specs)
            self.specs.append((P, fn))
        return self.memo[key]

    def gather(self, out_ap, P, src_ap, key, fn, r=(), w=(), acc=False):
        c = self.col(key, P, fn)
        off = bass.IndirectOffsetOnAxis(ap=self.idx[0:P, c:c + 1], axis=0)
        self.S.dma(None, None, r=list(r) + [self.idx_d], w=w, q="pool", acc=acc,
                   fn=lambda E: E.indirect_dma_start(out=out_ap, out_offset=None, in_=src_ap, in_offset=off))

    def table(self, b, j):
        t = np.zeros((128, self.ncols), np.int32)
        for c, (P, fn) in enumerate(self.specs):
            t[:P, c] = np.asarray(fn(b, j), dtype=np.int64)
        return t


O_FQ, O_FK, O_FV, O_FF, O_LX, O_LG, O_DKV, O_IK, O_CU, O_DQ, O_IQ, O_IW = (
    0, 256, 512, 768, 772, 1028, 1284, 1412, 1444, 1956, 2212, 2468)
NZG = O_CU
ZQ0 = O_DQ
ZQR = DIN - ZQ0
Z_PERM = np.concatenate([np.arange(0, 772), np.arange(1284, 1796), np.arange(2052, 2180), np.arange(2436, 2468),
                         np.arange(772, 1284), np.arange(1796, 2052), np.arange(2180, 2436), np.arange(2468, 2476)])
CW = 31
HAL = CW - 1
GW = 256
NEG = -1.0e30
TOPK = 256


class TStage:
    def __init__(self, S, PS, NT, es):
        self.S = S
        self.PS = PS
        self.NT = NT
        sb = lambda n, s, d: S.sb(n, s, d, es)
        self.ones = sb("ones", [128, 128], F32)
        self.ones_d = Dep()
        S.op("dve", lambda E: E.memset(self.ones[:], 1.0), w=[self.ones_d])
        self.eps = sb("eps", [128, 1], F32)
        self.eps_d = Dep()
        S.op("dve", lambda E: E.memset(self.eps[:], 1e-6), w=[self.eps_d])
        self.win = sb("win", [128, KC, 2 * DFF], BF16)
        self.win_d = Dep()
        self.wout = sb("wout", [128, JC, D], BF16)
        self.wout_d = Dep()
        self.stages = [(sb("stg%d" % i, [128, 1280], F32), Dep()) for i in range(3)]
        self.gt = sb("gt", [128, 6 * KC], F32)
        self.gt_d = Dep()
        self.x32 = [(sb("x32_%d" % i, [128, KC, NT], F32), Dep()) for i in range(2)]
        self.sq = (sb("sq", [128, KC, NT], F32), Dep())
        self.xn = (sb("xn", [128, KC, NT], BF16), Dep())
        self.act = (sb("actb", [128, JC, NT], BF16), Dep())
        self.sg = [(sb("sg%d" % i, [128, NT], F32), Dep()) for i in range(2)]
        self.y32 = (sb("y32", [128, KC, NT], F32), Dep())
        self.rstd = (sb("rstd", [128, NT], F32), Dep())
        self.tmp = [(sb("tmp%d" % i, [128, NT], F32), Dep()) for i in range(2)]
        self.lc = 0
        self.ddeps = {}

    def ddep(self, key, t0):
        k = (key, t0)
        if k not in self.ddeps:
            self.ddeps[k] = Dep()
        return self.ddeps[k]

    def nextps(self):
        return self.PS.nf()

    def load_cast(self, dst, dst_dep, src_dram, F, scale=None, scale_dep=None):
        S = self.S
        i = self.lc
        self.lc += 1
        st, sd = self.stages[i % len(self.stages)]
        S.dma(st[:, 0:F], src_dram, w=[sd])
        e = ("act", "dve", "pool")[i % 3]
        r = [sd] + ([scale_dep] if scale_dep is not None else [])
        if scale is None:
            if e == "act":
                S.op(e, lambda E: E.copy(out=dst, in_=st[:, 0:F]), r=r, w=[dst_dep])
            else:
                S.op(e, lambda E: E.tensor_copy(out=dst, in_=st[:, 0:F]), r=r, w=[dst_dep])
        else:
            if e == "act":
                S.op(e, lambda E: E.activation(out=dst, in_=st[:, 0:F], func=AF.Copy, scale=scale), r=r, w=[dst_dep])
            else:
                S.op(e, lambda E: E.tensor_scalar(out=dst, in0=st[:, 0:F], scalar1=scale, scalar2=None, op0=ALU.mult),
                     r=r, w=[dst_dep])

    def rms_rstd(self, src, src_d, n, out_scale=1.0):
        S = self.S
        sq, sq_d = self.sq
        S.op("act", lambda E: E.activation(out=sq[:, :, 0:n], in_=src[:, :, 0:n], func=AF.Square), r=[src_d], w=[sq_d])
        ps, ps_d = self.nextps()
        for c in range(KC):
            S.op("pe", lambda E: E.matmul(ps[:, 0:n], self.ones[:], sq[:, c, 0:n], start=(c == 0), stop=(c == KC - 1)),
                 r=[self.ones_d, sq_d], w=[ps_d])
        rs, rs_d = self.rstd
        S.op("act", lambda E: E.activation(out=rs[:, 0:n], in_=ps[:, 0:n], func=AF.Sqrt, scale=1.0 / D, bias=self.eps[:, 0:1]),
             r=[ps_d, self.eps_d], w=[rs_d])
        S.op("dve", lambda E: E.reciprocal(out=rs[:, 0:n], in_=rs[:, 0:n]), r=[rs_d], w=[rs_d])
        if out_scale != 1.0:
            S.op("dve", lambda E: E.tensor_scalar(out=rs[:, 0:n], in0=rs[:, 0:n], scalar1=float(out_scale), scalar2=None, op0=ALU.mult),
                 r=[rs_d], w=[rs_d])
        return rs, rs_d

    def load_gains(self, g_dram):
        self.S.dma(self.gt[:], g_dram, w=[self.gt_d])

    def gcol(self, gi, c):
        return self.gt[:, gi * KC + c:gi * KC + c + 1]

    def load_ffn_weights(self, win_dram, wout_dram, gi):
        for c in range(KC):
            for hh in range(4):
                self.load_cast(self.win[:, c, hh * 1280:(hh + 1) * 1280], self.win_d,
                               win_dram[:, c, hh * 1280:(hh + 1) * 1280], 1280,
                               scale=self.gcol(gi, c), scale_dep=self.gt_d)
        for j in range(JC):
            self.load_cast(self.wout[:, j, :], self.wout_d, wout_dram[:, j, :], D)

    def load_sq_weights(self, w_dram, ncols, gi=None):
        for c in range(KC):
            for f0 in range(0, ncols, 1280):
                f = min(1280, ncols - f0)
                self.load_cast(self.win[:, c, f0:f0 + f], self.win_d, w_dram[:, c, f0:f0 + f], f,
                               scale=(self.gcol(gi, c) if gi is not None else None),
                               scale_dep=(self.gt_d if gi is not None else None))

    def normalize(self, x32, x_d, n):
        S = self.S
        rs, rs_d = self.rms_rstd(x32, x_d, n)
        xn, xn_d = self.xn
        for c in range(KC):
            e = "dve" if c % 2 == 0 else "pool"
            S.op(e, lambda E: E.tensor_tensor(out=xn[:, c, 0:n], in0=x32[:, c, 0:n], in1=rs[:, 0:n], op=ALU.mult),
                 r=[x_d, rs_d], w=[xn_d])
        return xn, xn_d

    def add_normed(self, x32, x_d, n, gpost, half):
        S = self.S
        y32, y_d = self.y32
        rs2, rs2_d = self.rms_rstd(y32, y_d, n, out_scale=half)
        for c in range(KC):
            tp, tp_d = self.tmp[c % 2]
            e = "dve" if c % 2 == 0 else "pool"
            S.op("dve", lambda E: E.scalar_tensor_tensor(out=tp[:, 0:n], in0=y32[:, c, 0:n], scalar=self.gcol(gpost, c),
                                                         in1=rs2[:, 0:n], op0=ALU.mult, op1=ALU.mult),
                 r=[y_d, rs2_d, self.gt_d], w=[tp_d])
            S.op(e, lambda E: E.tensor_tensor(out=x32[:, c, 0:n], in0=tp[:, 0:n], in1=x32[:, c, 0:n], op=ALU.add),
                 r=[tp_d, x_d], w=[x_d])

    def ffn_compute(self, x32, x_d, n, gpost):
        S = self.S
        xn, xn_d = self.normalize(x32, x_d, n)
        act, act_d = self.act
        for j in range(JC):
            pg, pg_d = self.nextps()
            for c in range(KC):
                S.op("pe", lambda E: E.matmul(pg[:, 0:n], self.win[:, c, j * 128:(j + 1) * 128], xn[:, c, 0:n],
                                              start=(c == 0), stop=(c == KC - 1)),
                     r=[self.win_d, xn_d], w=[pg_d])
            pu, pu_d = self.nextps()
            for c in range(KC):
                S.op("pe", lambda E: E.matmul(pu[:, 0:n], self.win[:, c, DFF + j * 128:DFF + (j + 1) * 128], xn[:, c, 0:n],
                                              start=(c == 0), stop=(c == KC - 1)),
                     r=[self.win_d, xn_d], w=[pu_d])
            sg, sg_d = self.sg[j % 2]
            S.op("act", lambda E: E.activation(out=sg[:, 0:n], in_=pg[:, 0:n], func=AF.Silu), r=[pg_d], w=[sg_d])
            S.op("dve", lambda E: E.tensor_tensor(out=act[:, j, 0:n], in0=sg[:, 0:n], in1=pu[:, 0:n], op=ALU.mult),
                 r=[sg_d, pu_d], w=[act_d])
        y32, y_d = self.y32
        for c in range(KC):
            po, po_d = self.nextps()
            for j in range(JC):
                S.op("pe", lambda E: E.matmul(po[:, 0:n], self.wout[:, j, c * 128:(c + 1) * 128], act[:, j, 0:n],
                                              start=(j == 0), stop=(j == JC - 1)),
                     r=[self.wout_d, act_d], w=[po_d])
            S.op("act", lambda E: E.copy(out=y32[:, c, 0:n], in_=po[:, 0:n]), r=[po_d], w=[y_d])
        self.add_normed(x32, x_d, n, gpost, 0.5)

    def ffn_phase(self, h_in, h_out, ntok, gpost, kin="h", kout="h"):
        S = self.S
        it = 0
        for t0 in range(0, ntok, self.NT):
            n = min(self.NT, ntok - t0)
            x32, x_d = self.x32[it % 2]
            S.dma(x32[:, :, 0:n], h_in[:, t0:t0 + n].rearrange("(c p) n -> p c n", p=128), r=[self.ddep(kin, t0)], w=[x_d])
            self.ffn_compute(x32, x_d, n, gpost)
            S.dma(h_out[:, t0:t0 + n].rearrange("(c p) n -> p c n", p=128), x32[:, :, 0:n], r=[x_d], w=[self.ddep(kout, t0)])
            it += 1

    def proj_phase(self, h_in, z_out, zq_out, halo_out, ntok, kin="h"):
        S = self.S
        it = 0
        for t0 in range(0, ntok, self.NT):
            n = min(self.NT, ntok - t0)
            x32, x_d = self.x32[it % 2]
            S.dma(x32[:, :, 0:n], h_in[:, t0:t0 + n].rearrange("(c p) n -> p c n", p=128), r=[self.ddep(kin, t0)], w=[x_d])
            xn, xn_d = self.normalize(x32, x_d, n)
            k = 0
            for m0 in range(0, DIN, 128):
                m = min(128, DIN - m0)
                pz, pz_d = self.nextps()
                for c in range(KC):
                    S.op("pe", lambda E: E.matmul(pz[0:m, 0:n], self.win[:, c, m0:m0 + m], xn[:, c, 0:n],
                                                  start=(c == 0), stop=(c == KC - 1)),
                         r=[self.win_d, xn_d], w=[pz_d])
                zs, zs_d = self.sg[k % 2]
                if k % 2 == 0:
                    S.op("act", lambda E: E.copy(out=zs[0:m, 0:n], in_=pz[0:m, 0:n]), r=[pz_d], w=[zs_d])
                else:
                    S.op("dve", lambda E: E.tensor_copy(out=zs[0:m, 0:n], in_=pz[0:m, 0:n]), r=[pz_d], w=[zs_d])
                S.dma(z_out[m0:m0 + m, t0:t0 + n], zs[0:m, 0:n], r=[zs_d])
                lo, hi = max(m0, ZQ0), min(m0 + m, DIN)
                if lo < hi:
                    for kb in range(n // CH):
                        blk = t0 // CH + kb
                        S.dma(zq_out[blk * ZQR + lo - ZQ0:blk * ZQR + hi - ZQ0, :], zs[lo - m0:hi - m0, kb * CH:(kb + 1) * CH], r=[zs_d])
                lo, hi = max(m0, O_CU), min(m0 + m, O_CU + 2 * GW)
                if lo < hi and t0 + n == ntok:
                    S.dma(halo_out[lo - O_CU:hi - O_CU, :], zs[lo - m0:hi - m0, n - HAL:n], r=[zs_d])
                k += 1
            it += 1

    def mixin_phase(self, h_in, h_out, ntok, gpost, fill_y, kin="h", kout="h"):
        S = self.S
        it = 0
        for t0 in range(0, ntok, self.NT):
            n = min(self.NT, ntok - t0)
            x32, x_d = self.x32[it % 2]
            S.dma(x32[:, :, 0:n], h_in[:, t0:t0 + n].rearrange("(c p) n -> p c n", p=128), r=[self.ddep(kin, t0)], w=[x_d])
            sq, sq_d = self.sq
            fill_y(sq, sq_d, t0, n)
            xn, xn_d = self.xn
            S.op("dve", lambda E: E.tensor_copy(out=xn[:, :, 0:n], in_=sq[:, :, 0:n]), r=[sq_d], w=[xn_d])
            y32, y_d = self.y32
            for c in range(KC):
                po, po_d = self.nextps()
                for k in range(KC):
                    S.op("pe", lambda E: E.matmul(po[:, 0:n], self.win[:, k, c * 128:(c + 1) * 128], xn[:, k, 0:n],
                                                  start=(k == 0), stop=(k == KC - 1)),
                         r=[self.win_d, xn_d], w=[po_d])
                S.op("act", lambda E: E.copy(out=y32[:, c, 0:n], in_=po[:, 0:n]), r=[po_d], w=[y_d])
            self.add_normed(x32, x_d, n, gpost, 1.0)
            S.dma(h_out[:, t0:t0 + n].rearrange("(c p) n -> p c n", p=128), x32[:, :, 0:n], r=[x_d], w=[self.ddep(kout, t0)])
            it += 1


def lay_kc(w):
    k = w.shape[0] // 128
    return np.ascontiguousarray(w.reshape(k, 128, w.shape[1]).transpose(1, 0, 2))


def lay_gains(g):
    return np.ascontiguousarray(g.reshape(6, KC, 128).transpose(2, 0, 1).reshape(128, 6 * KC))


def fox_phase(S, PS, G, es, T, zall, zall_d, bf_dram, yx_loc):
    NTK = T // 4
    NCH = T // CH
    NQ = T // GRP
    HD = 64
    sb = lambda n, s, d: S.sb(n, s, d, es)
    R1 = sb("R1", [65, T], F32); R1_d = Dep()
    R2 = sb("R2", [65, T], F32); R2_d = Dep()
    qa = sb("qa", [65, T], BF16); qa_d = Dep(); qar_d = Dep()
    ka = sb("ka", [65, T], BF16); ka_d = Dep(); kar_d = Dep()
    vT = sb("vT", [HD, T], BF16); vT_d = Dep()
    va = sb("va", [CH, NCH, HD + 1], BF16); va_d = Dep()
    stg = [(sb("stg%d" % i, [HD, NTK], F32), Dep()) for i in range(2)]
    ffs = sb("ffs", [4, NTK], F32); ffs_d = Dep()
    sm = sb("sm", [65, 8], F32); sm_d = Dep()
    ones = sb("ones", [65, 128], F32); ones_d = Dep()
    identb = sb("identb", [HD, HD], BF16); identb_d = Dep()
    idf = sb("idf", [HD, HD], F32); idf_d = Dep()
    cc = sb("cc", [CH, NCH + NQ], F32); cc_d = Dep()
    biasT = sb("biasT", [CH, NQ, NCH], F32); biasT_d = Dep()
    mf = sb("mf", [CH, 4, GRP], F32); mf_d = Dep()
    mask = sb("mask", [CH, 4, GRP], BF16); mask_d = Dep()
    pt = [(sb("pt%d" % i, [CH, GRP], BF16), Dep()) for i in range(4)]
    osb = [(sb("osb%d" % i, [65, GRP], F32), Dep()) for i in range(2)]
    ysb = [(sb("ysb%d" % i, [HD, GRP], F32), Dep()) for i in range(2)]
    ps_s = PS.f[0:3]
    ps_o = PS.f[3:5]
    ps_m = PS.f[5]

    S.op("dve", lambda E: E.memset(ones[:], 1.0), w=[ones_d])
    S.op("pool", lambda E: E.iota(mf[:], [[-CH, 4], [1, GRP]], base=0, channel_multiplier=-1,
                                  allow_small_or_imprecise_dtypes=True), w=[mf_d])
    S.op("dve", lambda E: E.tensor_scalar(out=mask[:], in0=mf[:], scalar1=0.0, scalar2=None, op0=ALU.is_ge),
         r=[mf_d], w=[mask_d])
    S.op("pool", lambda E: E.iota(idf[:], [[1, HD]], base=0, channel_multiplier=-1, allow_small_or_imprecise_dtypes=True), w=[idf_d])
    S.op("dve", lambda E: E.tensor_scalar(out=identb[:], in0=idf[:], scalar1=0.0, scalar2=None, op0=ALU.is_equal), r=[idf_d], w=[identb_d])
    for jc in range(4):
        G.gather(ffs[:, :], 4, zall, ("ff", jc), (lambda b, j, jc=jc: (GB * b + jc) * NZG + O_FF + (j + np.arange(4)) % 4),
                 r=[zall_d], w=[ffs_d])
        S.dma(R1[64:65, jc * NTK:(jc + 1) * NTK], ffs[0:1, :], r=[ffs_d], w=[R1_d], acc=(jc > 0))
    S.dma(sm[64:65, 0:1], bf_dram, w=[sm_d])
    S.op("dve", lambda E: E.tensor_scalar(out=sm[64:65, 1:2], in0=sm[64:65, 0:1], scalar1=-1.0, scalar2=None, op0=ALU.mult),
         r=[sm_d], w=[sm_d])
    S.op("dve", lambda E: E.memset(sm[64:65, 2:3], 1.0), r=[sm_d], w=[sm_d])
    S.op("act", lambda E: E.activation(out=R1[64:65, :], in_=R1[64:65, :], func=AF.Exp, scale=-1.0, bias=sm[64:65, 1:2]),
         r=[R1_d, sm_d], w=[R1_d])
    S.op("act", lambda E: E.activation(out=R1[64:65, :], in_=R1[64:65, :], func=AF.Ln, scale=1.0, bias=sm[64:65, 2:3]),
         r=[R1_d, sm_d], w=[R1_d])
    S.op("dve", lambda E: E.tensor_scalar(out=R1[64:65, :], in0=R1[64:65, :], scalar1=-1.0, scalar2=None, op0=ALU.mult),
         r=[R1_d], w=[R1_d])
    S.op("dve", lambda E: E.tensor_tensor_scan(out=R2[64:65, :], data0=R1[64:65, :], data1=R1[64:65, :], initial=0.0,
                                               op0=ALU.add, op1=ALU.min),
         r=[R1_d], w=[R2_d])
    i = 0
    for off, dst, dd, sc in ((O_FQ, qa, qa_d, HD ** -0.5), (O_FK, ka, ka_d, 1.0), (O_FV, vT, vT_d, 1.0)):
        for jc in range(4):
            st, sd = stg[i % 2]
            G.gather(st[:, :], HD, zall, ("fqkv", off, jc),
                     (lambda b, j, jc=jc, off=off: (GB * b + jc) * NZG + off + HD * j + np.arange(HD)), r=[zall_d], w=[sd])
            sl = slice(jc * NTK, (jc + 1) * NTK)
            if i % 2 == 0:
                S.op("act", lambda E: E.mul(out=dst[0:HD, sl], in_=st[:, :], mul=float(sc)), r=[sd], w=[dd])
            else:
                S.op("dve", lambda E: E.tensor_scalar(out=dst[0:HD, sl], in0=st[:, :], scalar1=float(sc),
                                                      scalar2=None, op0=ALU.mult), r=[sd], w=[dd])
            i += 1
    S.op("pool", lambda E: E.memset(ka[64:65, :], 1.0), w=[kar_d])
    for Q in range(NQ):
        sl = slice(Q * GRP, (Q + 1) * GRP)
        S.op("dve", lambda E: E.tensor_scalar(out=qa[64:65, sl], in0=R2[64:65, sl], scalar1=R2[64:65, Q * GRP:Q * GRP + 1],
                                              scalar2=None, op0=ALU.subtract), r=[R2_d], w=[qar_d])
    pm, pm_d = ps_m
    for c in range(NCH):
        S.op("pe", lambda E: E.matmul(pm[0:CH, c:c + 1], R2[64:65, c * CH:(c + 1) * CH], ones[64:65, 0:1], start=True, stop=True),
             r=[R2_d, ones_d], w=[pm_d])
    for Q in range(NQ):
        S.op("pe", lambda E: E.matmul(pm[0:CH, NCH + Q:NCH + Q + 1], ones[64:65, 0:CH], R2[64:65, Q * GRP:Q * GRP + 1],
                                      start=True, stop=True), r=[R2_d, ones_d], w=[pm_d])
    S.op("act", lambda E: E.copy(out=cc[:, :], in_=pm[0:CH, 0:NCH + NQ]), r=[pm_d], w=[cc_d])
    for Q in range(NQ):
        S.op("dve", lambda E: E.tensor_scalar(out=biasT[:, Q, :], in0=cc[:, 0:NCH], scalar1=cc[:, NCH + Q:NCH + Q + 1],
                                              scalar2=-1.0, op0=ALU.subtract, op1=ALU.mult), r=[cc_d], w=[biasT_d])
    S.op("pool", lambda E: E.memset(va[:, :, HD:HD + 1], 1.0), w=[va_d])
    for c0 in range(0, NCH, 4):
        pT, pT_d = PS.nb()
        for k in range(4):
            S.op("pe", lambda E: E.transpose(out=pT[0:CH, k * HD:(k + 1) * HD], in_=vT[:, (c0 + k) * CH:(c0 + k + 1) * CH],
                                             identity=identb[:, :]), r=[vT_d, identb_d], w=[pT_d])
        S.op("act", lambda E: E.copy(out=va[:, c0:c0 + 4, 0:HD], in_=pT[0:CH, 0:4 * HD].rearrange("p (a b) -> p a b", a=4)),
             r=[pT_d], w=[va_d])

    si = 0
    for Q in range(NQ):
        qsl = slice(Q * GRP, (Q + 1) * GRP)
        po, po_d = ps_o[Q % 2]
        nck = 4 * Q + 4
        pend = None

        def qk(c):
            nonlocal si
            pS, pS_d = ps_s[si % 3]
            ptile, pt_d = pt[si % 4]
            si += 1
            S.op("pe", lambda E: E.matmul(pS[0:CH, 0:GRP], ka[0:65, c * CH:(c + 1) * CH], qa[0:65, qsl], start=True, stop=True),
                 r=[ka_d, kar_d, qa_d, qar_d], w=[pS_d])
            S.op("act", lambda E: E.activation(out=ptile[:, :], in_=pS[0:CH, 0:GRP], func=AF.Exp, scale=1.0,
                                               bias=biasT[:, Q, c:c + 1]), r=[pS_d, biasT_d], w=[pt_d])
            d = c - 4 * Q
            if d >= 0:
                S.op("dve", lambda E: E.tensor_tensor(out=ptile[:, :], in0=ptile[:, :], in1=mask[:, d, :], op=ALU.mult),
                     r=[pt_d, mask_d], w=[pt_d])
            return ptile, pt_d

        def pv(c, ptile, pt_d):
            S.op("pe", lambda E: E.matmul(po[0:HD + 1, 0:GRP], va[:, c, :], ptile[:, :], start=(c == 0), stop=(c == nck - 1)),
                 r=[va_d, pt_d], w=[po_d])

        for c in range(nck):
            cur = qk(c)
            if pend is not None:
                pv(c - 1, *pend)
            pend = cur
        pv(nck - 1, *pend)
        ob, ob_d = osb[Q % 2]
        S.op("act", lambda E: E.copy(out=ob[:, :], in_=po[0:HD + 1, 0:GRP]), r=[po_d], w=[ob_d])
        S.op("dve", lambda E: E.reciprocal(out=ob[64:65, :], in_=ob[64:65, :]), r=[ob_d], w=[ob_d])
        pb, pb_d = ps_m
        S.op("pe", lambda E: E.matmul(pb[0:HD, 0:GRP], ones[64:65, 0:HD], ob[64:65, :], start=True, stop=True),
             r=[ones_d, ob_d], w=[pb_d])
        yb, yb_d = ysb[Q % 2]
        S.op("dve", lambda E: E.tensor_tensor(out=yb[:, :], in0=ob[0:HD, :], in1=pb[0:HD, 0:GRP], op=ALU.mult),
             r=[ob_d, pb_d], w=[yb_d])
        S.dma(yx_loc[4 * Q * HD:(4 * Q + 4) * HD, :].rearrange("(i d) t -> d i t", d=HD),
              yb[:, :].rearrange("d (i t) -> d i t", i=4), r=[yb_d])


def conv_phase(S, PS, G, T, zloc, halo_all, halo_d, halo_on, halo_on_d, cw_dram, cv_dram, yc_loc):
    NTK = T // 4
    NT = 342 if NTK % 342 == 0 else 228
    nextps = PS.nf
    with ExitStack() as es2:
        sb = lambda n, s, d: S.sb(n, s, d, es2)
        cst = sb("cst", [128, 4], F32); cst_d = Dep()
        S.op("dve", lambda E: E.memset(cst[:, 0:1], 1e-5), w=[cst_d])
        S.op("dve", lambda E: E.memset(cst[:, 1:2], 1.0), w=[cst_d])
        o256 = sb("o256", [128, 128], F32); o256_d = Dep()
        S.op("dve", lambda E: E.memset(o256[:], 1.0 / GW), w=[o256_d])
        A = sb("A", [128, 2, HAL + NTK], F32); A_d = [Dep(), Dep()]
        Gt = sb("G", [128, 2, HAL + NTK], F32); G_d = [Dep(), Dep()]
        acc = sb("acc", [128, 2, NTK], F32); acc_d = [Dep(), Dep()]
        cw = sb("cw", [128, 2, CW], F32); cw_d = Dep()
        cv = sb("cv", [128, 2, 3], F32); cv_d = Dep()
        yc = [(sb("yc%d" % i, [128, NT], F32), Dep()) for i in range(2)]
        sq = [(sb("sqc%d" % i, [128, NT], F32), Dep()) for i in range(2)]
        rs = (sb("rsc", [128, NT], F32), Dep())
        yo = [(sb("yo%d" % i, [128, NT], F32), Dep()) for i in range(2)]
        S.dma(cw[:], cw_dram, w=[cw_d])
        S.dma(cv[:], cv_dram, w=[cv_d])
        for cc in range(2):
            G.gather(A[:, cc, 0:HAL], 128, halo_all, ("halo", cc),
                     (lambda b, j, cc=cc: (GB * b + max(j - 1, 0)) * 2 * GW + cc * 128 + np.arange(128)), r=[halo_d], w=[A_d[cc]])
            G.gather(Gt[:, cc, 0:HAL], 128, halo_all, ("halo", 2 + cc),
                     (lambda b, j, cc=cc: (GB * b + max(j - 1, 0)) * 2 * GW + GW + cc * 128 + np.arange(128)), r=[halo_d], w=[G_d[cc]])
            S.dma(A[:, cc, HAL:], zloc[O_CU + cc * 128:O_CU + (cc + 1) * 128, :], w=[A_d[cc]], acc=True)
            S.dma(Gt[:, cc, HAL:], zloc[O_CU + GW + cc * 128:O_CU + GW + (cc + 1) * 128, :], w=[G_d[cc]], acc=True)
            S.op("dve", lambda E: E.tensor_scalar(out=A[:, cc, 0:HAL], in0=A[:, cc, 0:HAL], scalar1=halo_on[:, 0:1], scalar2=None,
                                                  op0=ALU.mult), r=[A_d[cc], halo_on_d], w=[A_d[cc]])
        for cc in range(2):
            e = "dve" if cc == 0 else "pool"
            S.op("act", lambda E: E.activation(out=Gt[:, cc, :], in_=Gt[:, cc, :], func=AF.Sigmoid), r=[G_d[cc]], w=[G_d[cc]])
            S.op(e, lambda E: E.tensor_tensor(out=A[:, cc, :], in0=A[:, cc, :], in1=Gt[:, cc, :], op=ALU.mult),
                 r=[A_d[cc], G_d[cc]], w=[A_d[cc]])
        for k in range(CW):
            for cc in range(2):
                if k == 0:
                    S.op("dve", lambda E: E.tensor_scalar(out=acc[:, cc, :], in0=A[:, cc, 0:NTK], scalar1=cw[:, cc, 0:1],
                                                          scalar2=cv[:, cc, 0:1], op0=ALU.mult, op1=ALU.add),
                         r=[A_d[cc], cw_d, cv_d], w=[acc_d[cc]])
                else:
                    S.op("dve", lambda E: E.scalar_tensor_tensor(out=acc[:, cc, :], in0=A[:, cc, k:k + NTK], scalar=cw[:, cc, k:k + 1],
                                                                 in1=acc[:, cc, :], op0=ALU.mult, op1=ALU.add),
                         r=[A_d[cc], cw_d, acc_d[cc]], w=[acc_d[cc]])
        for t0 in range(0, NTK, NT):
            n = min(NT, NTK - t0)
            pm, pm_d = nextps()
            for cc in range(2):
                S.op("pe", lambda E: E.matmul(pm[:, 0:n], o256[:], acc[:, cc, t0:t0 + n], start=(cc == 0), stop=(cc == 1)),
                     r=[o256_d, acc_d[cc]], w=[pm_d])
            pv, pv_d = nextps()
            for cc in range(2):
                y_, y_d = yc[cc]
                s_, s_d = sq[cc]
                S.op("dve", lambda E: E.tensor_tensor(out=y_[:, 0:n], in0=acc[:, cc, t0:t0 + n], in1=pm[:, 0:n], op=ALU.subtract),
                     r=[acc_d[cc], pm_d], w=[y_d])
                S.op("act", lambda E: E.activation(out=s_[:, 0:n], in_=y_[:, 0:n], func=AF.Square), r=[y_d], w=[s_d])
                S.op("pe", lambda E: E.matmul(pv[:, 0:n], o256[:], s_[:, 0:n], start=(cc == 0), stop=(cc == 1)),
                     r=[o256_d, s_d], w=[pv_d])
            r_, r_d = rs
            S.op("act", lambda E: E.activation(out=r_[:, 0:n], in_=pv[:, 0:n], func=AF.Sqrt, scale=1.0, bias=cst[:, 0:1]),
                 r=[pv_d, cst_d], w=[r_d])
            S.op("dve", lambda E: E.reciprocal(out=r_[:, 0:n], in_=r_[:, 0:n]), r=[r_d], w=[r_d])
            for cc in range(2):
                y_, y_d = yc[cc]
                o_, o_d = yo[cc]
                e = "dve" if cc == 0 else "pool"
                S.op(e, lambda E: E.tensor_tensor(out=y_[:, 0:n], in0=y_[:, 0:n], in1=r_[:, 0:n], op=ALU.mult),
                     r=[y_d, r_d], w=[y_d])
                S.op("act", lambda E: E.activation(out=o_[:, 0:n], in_=y_[:, 0:n], func=AF.Silu, scale=cv[:, cc, 1:2],
                                                   bias=cv[:, cc, 2:3]), r=[y_d, cv_d], w=[o_d])
                S.dma(yc_loc[cc * 128:(cc + 1) * 128, t0:t0 + n], o_[:, 0:n], r=[o_d])


def lru_phase(S, PS, G, T, zall, zall_d, lcw_dram, lv_dram, lwa_dram, lwi_dram, yx_loc, YL0):
    NTK = T // 4
    SEG = NTK
    NBK = NTK // CH
    SUB = 342 if SEG % 342 == 0 else 228
    nextps = PS.nf
    with ExitStack() as es3:
        sb = lambda n, s, d: S.sb(n, s, d, es3)
        cst = sb("cst2", [128, 4], F32); cst_d = Dep()
        S.op("dve", lambda E: E.memset(cst[:, 1:2], 1.0), w=[cst_d])
        X = sb("X", [64, 3 + SEG], F32); X_d = Dep()
        Gt = sb("Gl", [64, SEG], F32); Gt_d = Dep()
        xc = sb("xc", [64, SEG], F32); xc_d = Dep()
        rt = sb("rt", [64, SEG], F32); rt_d = Dep()
        itl = sb("itl", [64, SEG], F32); it_d = Dep()
        at = sb("at", [64, SEG], F32); at_d = Dep()
        ut = sb("ut", [64, SEG], F32); ut_d = Dep()
        hs = sb("hs", [64, SEG], F32); hs_d = Dep()
        g1 = sb("g1", [64, SEG], F32); g1_d = Dep()
        lcw = sb("lcw", [64, 4], F32); lcw_d = Dep()
        lv = sb("lv", [64, 8], F32); lv_d = Dep()
        lwa = sb("lwa", [64, 64], F32); lwa_d = Dep()
        lwi = sb("lwi", [64, 64], F32); lwi_d = Dep()
        carry = sb("carry", [64, 1], F32); carry_d = Dep()
        S.dma(lcw[:], lcw_dram, w=[lcw_d])
        S.dma(lv[:, 0:4], lv_dram, w=[lv_d])
        S.dma(lwa[:], lwa_dram, w=[lwa_d])
        S.dma(lwi[:], lwi_dram, w=[lwi_d])
        S.op("act", lambda E: E.activation(out=lv[:, 4:5], in_=lv[:, 3:4], func=AF.Exp, scale=-1.0), r=[lv_d], w=[lv_d])
        S.op("act", lambda E: E.activation(out=lv[:, 4:5], in_=lv[:, 4:5], func=AF.Ln, scale=1.0, bias=cst[0:64, 1:2]),
             r=[lv_d, cst_d], w=[lv_d])
        S.op("dve", lambda E: E.tensor_scalar(out=lv[:, 5:6], in0=lv[:, 4:5], scalar1=-16.0, scalar2=None, op0=ALU.mult),
             r=[lv_d], w=[lv_d])
        S.op("dve", lambda E: E.tensor_scalar(out=lv[:, 4:5], in0=lv[:, 4:5], scalar1=-8.0, scalar2=None, op0=ALU.mult),
             r=[lv_d], w=[lv_d])
        S.op("dve", lambda E: E.memset(carry[:], 0.0), w=[carry_d])
        S.op("dve", lambda E: E.memset(X[:, 0:3], 0.0), w=[X_d])
        for s in range(4):
            t0 = s * SEG
            if s > 0:
                S.op("dve", lambda E: E.tensor_copy(out=X[:, 0:3], in_=X[:, SEG:SEG + 3]), r=[X_d], w=[X_d])
            G.gather(X[:, 3:3 + SEG], 64, zall, ("lx", s), (lambda b, j, s=s: (GB * b + s) * NZG + O_LX + 64 * j + np.arange(64)),
                     r=[zall_d], w=[X_d], acc=True)
            G.gather(Gt[:, :], 64, zall, ("lg", s), (lambda b, j, s=s: (GB * b + s) * NZG + O_LG + 64 * j + np.arange(64)),
                     r=[zall_d], w=[Gt_d])
            S.op("dve", lambda E: E.tensor_scalar(out=xc[:, :], in0=X[:, 0:SEG], scalar1=lcw[:, 0:1], scalar2=lv[:, 0:1],
                                                  op0=ALU.mult, op1=ALU.add), r=[X_d, lcw_d, lv_d], w=[xc_d])
            for k in range(1, 4):
                S.op("dve", lambda E: E.scalar_tensor_tensor(out=xc[:, :], in0=X[:, k:k + SEG], scalar=lcw[:, k:k + 1], in1=xc[:, :],
                                                             op0=ALU.mult, op1=ALU.add), r=[X_d, lcw_d, xc_d], w=[xc_d])
            for u0 in range(0, SEG, SUB):
                pa, pa_d = nextps()
                S.op("pe", lambda E: E.matmul(pa[0:64, 0:SUB], lwa[:, :], xc[:, u0:u0 + SUB], start=True, stop=True),
                     r=[lwa_d, xc_d], w=[pa_d])
                S.op("act", lambda E: E.activation(out=rt[:, u0:u0 + SUB], in_=pa[0:64, 0:SUB], func=AF.Sigmoid, scale=1.0,
                                                   bias=lv[:, 1:2]), r=[pa_d, lv_d], w=[rt_d])
                pb, pb_d = nextps()
                S.op("pe", lambda E: E.matmul(pb[0:64, 0:SUB], lwi[:, :], xc[:, u0:u0 + SUB], start=True, stop=True),
                     r=[lwi_d, xc_d], w=[pb_d])
                S.op("act", lambda E: E.activation(out=itl[:, u0:u0 + SUB], in_=pb[0:64, 0:SUB], func=AF.Sigmoid, scale=1.0,
                                                   bias=lv[:, 2:3]), r=[pb_d, lv_d], w=[it_d])
            S.op("act", lambda E: E.activation(out=at[:, :], in_=rt[:, :], func=AF.Exp, scale=lv[:, 4:5]), r=[rt_d, lv_d], w=[at_d])
            S.op("act", lambda E: E.activation(out=ut[:, :], in_=rt[:, :], func=AF.Exp, scale=lv[:, 5:6]), r=[rt_d, lv_d], w=[ut_d])
            S.op("pool", lambda E: E.tensor_scalar(out=ut[:, :], in0=ut[:, :], scalar1=-1.0, scalar2=1.0, op0=ALU.mult, op1=ALU.add),
                 r=[ut_d], w=[ut_d])
            S.op("act", lambda E: E.activation(out=ut[:, :], in_=ut[:, :], func=AF.Sqrt), r=[ut_d], w=[ut_d])
            S.op("pool", lambda E: E.tensor_tensor(out=itl[:, :], in0=itl[:, :], in1=xc[:, :], op=ALU.mult), r=[it_d, xc_d], w=[it_d])
            S.op("pool", lambda E: E.tensor_tensor(out=ut[:, :], in0=ut[:, :], in1=itl[:, :], op=ALU.mult), r=[ut_d, it_d], w=[ut_d])
            S.op("dve", lambda E: E.tensor_tensor_scan(out=hs[:, :], data0=at[:, :], data1=ut[:, :], initial=carry[:, 0:1],
                                                       op0=ALU.mult, op1=ALU.add), r=[at_d, ut_d, carry_d], w=[hs_d])
            S.op("dve", lambda E: E.tensor_copy(out=carry[:, :], in_=hs[:, SEG - 1:SEG]), r=[hs_d], w=[carry_d])
            S.op("act", lambda E: E.activation(out=g1[:, :], in_=Gt[:, :], func=AF.Square), r=[Gt_d], w=[g1_d])
            S.op("pool", lambda E: E.tensor_scalar(out=g1[:, :], in0=g1[:, :], scalar1=0.044715, scalar2=1.0, op0=ALU.mult, op1=ALU.add),
                 r=[g1_d], w=[g1_d])
            S.op("pool", lambda E: E.tensor_tensor(out=g1[:, :], in0=g1[:, :], in1=Gt[:, :], op=ALU.mult), r=[g1_d, Gt_d], w=[g1_d])
            S.op("act", lambda E: E.activation(out=g1[:, :], in_=g1[:, :], func=AF.Sigmoid, scale=1.5957691216057308), r=[g1_d], w=[g1_d])
            S.op("pool", lambda E: E.tensor_tensor(out=g1[:, :], in0=g1[:, :], in1=Gt[:, :], op=ALU.mult), r=[g1_d, Gt_d], w=[g1_d])
            S.op("dve", lambda E: E.tensor_tensor(out=hs[:, :], in0=hs[:, :], in1=g1[:, :], op=ALU.mult), r=[hs_d, g1_d], w=[hs_d])
            S.dma(yx_loc[YL0 + s * NBK * 64:YL0 + (s + 1) * NBK * 64, :].rearrange("(i d) t -> d i t", d=64),
                  hs[:, :].rearrange("d (i t) -> d i t", t=CH), r=[hs_d])


def dsa_phase(S, PS, G, es, T, zall, zall_d, zq_all, zq_d, joff, joff_d, kvg_dram, ikg_dram, wuk_dram, wuv_dram,
              yx_loc, YD0, NR=22):
    NTK = T // 4
    NCH = T // CH
    NB = NCH // 4
    NBK = NTK // CH
    LAT = 128
    sb = lambda n, s, d: S.sb(n, s, d, es)
    psA = PS.f[0:3]
    pso = PS.f[3]
    psd = PS.f[4]
    ai = [0]

    def nextA():
        p = psA[ai[0] % 3]
        ai[0] += 1
        return p

    nextT = PS.nb
    cst = sb("cst", [128, 4], F32); cst_d = Dep()
    S.op("dve", lambda E: E.memset(cst[:, 0:1], 1e-6), w=[cst_d])
    S.op("dve", lambda E: E.memset(cst[:, 1:2], 1e-5), w=[cst_d])
    idf = sb("idf", [128, 128], F32); idf_d = Dep()
    ident = sb("ident", [128, 128], BF16); ident_d = Dep()
    identf = sb("identf", [128, 128], F32); identf_d = Dep()
    S.op("pool", lambda E: E.iota(idf[:], [[1, 128]], base=0, channel_multiplier=-1, allow_small_or_imprecise_dtypes=True), w=[idf_d])
    S.op("dve", lambda E: E.tensor_scalar(out=ident[:], in0=idf[:], scalar1=0.0, scalar2=None, op0=ALU.is_equal), r=[idf_d], w=[ident_d])
    S.op("dve", lambda E: E.tensor_scalar(out=identf[:], in0=idf[:], scalar1=0.0, scalar2=None, op0=ALU.is_equal), r=[idf_d], w=[identf_d])
    onesb = sb("onesb", [128, 128], BF16); onesb_d = Dep()
    S.op("dve", lambda E: E.memset(onesb[:], 1.0), w=[onesb_d])
    onesL = sb("onesL", [128, 128], F32); onesL_d = Dep()
    S.op("dve", lambda E: E.memset(onesL[:], 1.0 / LAT), w=[onesL_d])
    ones32 = sb("ones32", [32, 32], F32); ones32_d = Dep()
    S.op("dve", lambda E: E.memset(ones32[:], 1.0 / 32), w=[ones32_d])
    negm = sb("negm", [CH, GRP], F32); negm_d = Dep()
    S.op("pool", lambda E: E.iota(negm[:], [[1, GRP]], base=0, channel_multiplier=-1, allow_small_or_imprecise_dtypes=True), w=[negm_d])
    S.op("dve", lambda E: E.tensor_scalar(out=negm[:], in0=negm[:], scalar1=joff[0:CH, 0:1], scalar2=NEG, op0=ALU.is_gt, op1=ALU.mult),
         r=[negm_d, joff_d], w=[negm_d])
    ctok = sb("ctok", [CH, NCH, LAT], BF16); ctok_d = Dep()
    cT = sb("cT", [LAT, T], BF16); cT_d = Dep()
    kiT = sb("kiT", [32, T], F32); kiT_d = Dep()
    wuk = sb("wuk", [64, 4, LAT], F32); wuk_d = Dep()
    wuv = sb("wuv", [LAT, 4, 64], BF16); wuv_d = Dep()
    kvg = sb("kvg", [LAT, 1], F32); kvg_d = Dep()
    ikg = sb("ikg", [32, 2], F32); ikg_d = Dep()
    S.dma(kvg[:], kvg_dram, w=[kvg_d])
    S.dma(ikg[:], ikg_dram, w=[ikg_d])
    S.dma(wuk[:], wuk_dram, w=[wuk_d])

    with ExitStack() as es2:
        sb2 = lambda n, s, d: S.sb(n, s, d, es2)
        dkvT = sb2("dkvT", [LAT, T], F32); dkvT_d = Dep()
        ikT = sb2("ikT", [32, T], F32); ikT_d = Dep()
        sq = [(sb2("sqp%d" % i, [LAT, GRP], F32), Dep()) for i in range(2)]
        rs = [(sb2("rsp%d" % i, [LAT, GRP], F32), Dep()) for i in range(2)]
        wst = sb2("wst", [128, 256], F32); wst_d = Dep()
        S.dma(wst[:, :], wuv_dram.rearrange("p a b -> p (a b)"), w=[wst_d])
        S.op("dve", lambda E: E.tensor_copy(out=wuv[:].rearrange("p a b -> p (a b)"), in_=wst[:, :]), r=[wst_d], w=[wuv_d])
        for s in range(4):
            G.gather(dkvT[:, s * NTK:(s + 1) * NTK], LAT, zall, ("dkv", s),
                     (lambda b, j, s=s: (GB * b + s) * NZG + O_DKV + np.arange(LAT)), r=[zall_d], w=[dkvT_d], acc=(s > 0))
            G.gather(ikT[:, s * NTK:(s + 1) * NTK], 32, zall, ("ik", s),
                     (lambda b, j, s=s: (GB * b + s) * NZG + O_IK + np.arange(32)), r=[zall_d], w=[ikT_d], acc=(s > 0))
        for g in range(T // GRP):
            gs = slice(g * GRP, (g + 1) * GRP)
            s_, s_d = sq[g % 2]
            r_, r_d = rs[g % 2]
            S.op("act", lambda E: E.activation(out=s_[:, :], in_=dkvT[:, gs], func=AF.Square), r=[dkvT_d], w=[s_d])
            pm, pm_d = nextA()
            S.op("pe", lambda E: E.matmul(pm[:, 0:GRP], onesL[:, :], s_[:, :], start=True, stop=True), r=[onesL_d, s_d], w=[pm_d])
            S.op("act", lambda E: E.activation(out=r_[:, :], in_=pm[:, 0:GRP], func=AF.Sqrt, scale=1.0, bias=cst[:, 0:1]),
                 r=[pm_d, cst_d], w=[r_d])
            S.op("dve", lambda E: E.reciprocal(out=r_[:, :], in_=r_[:, :]), r=[r_d], w=[r_d])
            S.op("dve", lambda E: E.scalar_tensor_tensor(out=cT[:, gs], in0=dkvT[:, gs], scalar=kvg[:, 0:1], in1=r_[:, :],
                                                         op0=ALU.mult, op1=ALU.mult), r=[dkvT_d, kvg_d, r_d], w=[cT_d])
            pmu, pmu_d = nextA()
            S.op("pe", lambda E: E.matmul(pmu[0:32, 0:GRP], ones32[:, :], ikT[:, gs], start=True, stop=True), r=[ones32_d, ikT_d], w=[pmu_d])
            S.op("dve", lambda E: E.tensor_tensor(out=ikT[:, gs], in0=ikT[:, gs], in1=pmu[0:32, 0:GRP], op=ALU.subtract),
                 r=[ikT_d, pmu_d], w=[ikT_d])
            s2_, s2_d = sq[(g + 1) % 2]
            S.op("act", lambda E: E.activation(out=s2_[0:32, :], in_=ikT[:, gs], func=AF.Square), r=[ikT_d], w=[s2_d])
            pvr, pvr_d = nextA()
            S.op("pe", lambda E: E.matmul(pvr[0:32, 0:GRP], ones32[:, :], s2_[0:32, :], start=True, stop=True), r=[ones32_d, s2_d], w=[pvr_d])
            r2_, r2_d = rs[(g + 1) % 2]
            S.op("act", lambda E: E.activation(out=r2_[0:32, :], in_=pvr[0:32, 0:GRP], func=AF.Sqrt, scale=1.0, bias=cst[0:32, 1:2]),
                 r=[pvr_d, cst_d], w=[r2_d])
            S.op("dve", lambda E: E.reciprocal(out=r2_[0:32, :], in_=r2_[0:32, :]), r=[r2_d], w=[r2_d])
            S.op("dve", lambda E: E.tensor_tensor(out=ikT[:, gs], in0=ikT[:, gs], in1=r2_[0:32, :], op=ALU.mult), r=[ikT_d, r2_d], w=[ikT_d])
            S.op("dve", lambda E: E.tensor_scalar(out=kiT[:, gs], in0=ikT[:, gs], scalar1=ikg[:, 0:1], scalar2=ikg[:, 1:2],
                                                  op0=ALU.mult, op1=ALU.add), r=[ikT_d, ikg_d], w=[kiT_d])
            pT, pT_d = nextT()
            for k in range(4):
                c = 4 * g + k
                S.op("pe", lambda E: E.transpose(out=pT[0:CH, k * LAT:(k + 1) * LAT], in_=cT[:, c * CH:(c + 1) * CH], identity=ident[:, :]),
                     r=[cT_d, ident_d], w=[pT_d])
            S.op("act", lambda E: E.copy(out=ctok[:, 4 * g:4 * g + 4, :], in_=pT[0:CH, 0:4 * LAT].rearrange("p (a b) -> p a b", a=4)),
                 r=[pT_d], w=[ctok_d])
    S.barrier()

    Ib = [sb("I%d" % i, [CH, T], F32) for i in range(2)]
    Igd = [[Dep() for _ in range(NB)] for _ in range(2)]
    mask = sb("mask", [CH, T], BF16); mask_d = Dep()
    junk = sb("junk", [CH, T], BF16); junk_d = Dep()
    rt = [(sb("rt%d" % i, [CH, GRP], F32), Dep()) for i in range(3)]
    Et = [(sb("Et%d" % i, [CH, GRP], BF16), Dep()) for i in range(2)]
    Pt = [(sb("Pt%d" % i, [CH, 4, CH], BF16), Dep()) for i in range(2)]
    qlb = [(sb("qlat%d" % i, [LAT, GRP], BF16), Dep()) for i in range(2)]
    svb = [(sb("sv%d" % i, [CH, 8], F32), Dep()) for i in range(2)]
    t12_d = Dep()
    sa = sb("sa", [CH, 2], F32); sa_d = Dep()
    NR3 = 10
    c3 = sb("c3", [CH, 2 * NR3], F32); c3_d = Dep()
    st3b = [(sb("st3_%d" % i, [CH, 2 * NR3], F32), Dep()) for i in range(2)]
    for n in range(NR3):
        S.op("dve", lambda E: E.memset(c3[:, 2 * n:2 * n + 1], float(3.0 ** -(n + 1))), w=[c3_d])
        S.op("dve", lambda E: E.memset(c3[:, 2 * n + 1:2 * n + 2], float(2.0 * 3.0 ** -(n + 1))), w=[c3_d])
    rden = sb("rden", [LAT, GRP], F32); rden_d = Dep()
    ob = sb("ob", [LAT, GRP], BF16); ob_d = Dep()
    ysb = sb("ysb", [64, GRP], F32); ysb_d = Dep()
    dqb = [(sb("dqb%d" % i, [64, 4, CH], F32), Dep()) for i in range(2)]
    iqb = [(sb("iqb%d" % i, [32, 8, CH], F32), Dep()) for i in range(2)]
    iwb = [(sb("iwb%d" % i, [8, CH], F32), Dep()) for i in range(2)]
    w16 = [(sb("w16_%d" % i, [CH, 8], F32), Dep()) for i in range(2)]
    cnt = {"ri": 0, "ei": 0}

    def qrow(m, f0):
        return lambda b, j, m=m, f0=f0: ((GB * b + (4 * m + j) // NBK) * NBK + (4 * m + j) % NBK) * ZQR + f0

    def units_A(m):
        ng = m + 1
        nk = ng * GRP
        I = Ib[m % 2]
        Id = Igd[m % 2]
        dqT, dqT_d = dqb[m % 2]
        iqT, iqT_d = iqb[m % 2]
        iwT, iwT_d = iwb[m % 2]
        w_, w_d = w16[m % 2]
        ql, ql_d = qlb[m % 2]
        sv, sv_d = svb[m % 2]
        st3, st3_d = st3b[m % 2]
        us = []

        def load():
            for h in range(4):
                G.gather(dqT[:, h, :], 64, zq_all, ("dq", m, h),
                         (lambda b, j, m=m, h=h: qrow(m, O_DQ - ZQ0 + 64 * h)(b, j) + np.arange(64)), r=[zq_d], w=[dqT_d], acc=(h > 0))
            for h in range(8):
                G.gather(iqT[:, h, :], 32, zq_all, ("iq", m, h),
                         (lambda b, j, m=m, h=h: qrow(m, O_IQ - ZQ0 + 32 * h)(b, j) + np.arange(32)), r=[zq_d], w=[iqT_d], acc=(h > 0))
            G.gather(iwT[:, :], 8, zq_all, ("iw", m), (lambda b, j, m=m: qrow(m, O_IW - ZQ0)(b, j) + np.arange(8)), r=[zq_d], w=[iwT_d])
            pw_, pw_d = nextA()
            S.op("pe", lambda E: E.transpose(out=pw_[0:CH, 0:8], in_=iwT[:, :], identity=identf[0:8, 0:8]), r=[iwT_d, identf_d], w=[pw_d])
            S.op("act", lambda E: E.mul(out=w_[:, :], in_=pw_[0:CH, 0:8], mul=1.0 / 16), r=[pw_d], w=[w_d])
            pq, pq_d = nextA()
            for h in range(4):
                S.op("pe", lambda E: E.matmul(pq[:, h * CH:(h + 1) * CH], wuk[:, h, :], dqT[:, h, :], start=True, stop=True),
                     r=[wuk_d, dqT_d], w=[pq_d])
            S.op("act", lambda E: E.mul(out=ql[:, :], in_=pq[:, 0:GRP], mul=0.125), r=[pq_d], w=[ql_d])
        us.append(load)

        def score(h, g):
            ks = slice(g * GRP, (g + 1) * GRP)
            px, px_d = nextA()
            S.op("pe", lambda E: E.matmul(px[0:CH, 0:GRP], iqT[:, h, :], kiT[:, ks], start=True, stop=True),
                 r=[iqT_d, kiT_d], w=[px_d])
            r_, r_d = rt[cnt["ri"] % 3]
            cnt["ri"] += 1
            S.op("act", lambda E: E.activation(out=r_[:, :], in_=px[0:CH, 0:GRP], func=AF.Relu), r=[px_d], w=[r_d])
            if h == 0:
                S.op("dve", lambda E: E.tensor_scalar(out=I[:, ks], in0=r_[:, :], scalar1=w_[:, 0:1], scalar2=None, op0=ALU.mult),
                     r=[r_d, w_d], w=[Id[g]])
            else:
                S.op("dve", lambda E: E.scalar_tensor_tensor(out=I[:, ks], in0=r_[:, :], scalar=w_[:, h:h + 1], in1=I[:, ks],
                                                             op0=ALU.mult, op1=ALU.add), r=[r_d, w_d, Id[g]], w=[Id[g]])
        for h in range(8):
            for g in range(ng):
                us.append(lambda h=h, g=g: score(h, g))

        def fin():
            S.op("dve", lambda E: E.tensor_reduce(out=sv[:, 0:1], in_=I[:, 0:nk], axis=AX.X, op=ALU.min), r=Id[0:ng], w=[sv_d])
            S.op("dve", lambda E: E.tensor_reduce(out=sv[:, 5:6], in_=I[:, 0:nk], axis=AX.X, op=ALU.max), r=Id[0:ng], w=[sv_d])
            S.op("dve", lambda E: E.tensor_tensor(out=sv[:, 1:2], in0=sv[:, 5:6], in1=sv[:, 0:1], op=ALU.subtract), r=[sv_d], w=[sv_d])
            S.op("dve", lambda E: E.tensor_scalar(out=sv[:, 1:2], in0=sv[:, 1:2], scalar1=1.0001, scalar2=1e-20, op0=ALU.mult, op1=ALU.add),
                 r=[sv_d], w=[sv_d])
            S.op("dve", lambda E: E.tensor_tensor(out=I[:, m * GRP:nk], in0=I[:, m * GRP:nk], in1=negm[:, :], op=ALU.add),
                 r=[Id[m], negm_d], w=[Id[m]])
            S.op("dve", lambda E: E.tensor_scalar(out=st3[:, :], in0=c3[:, :], scalar1=sv[:, 1:2], scalar2=None, op0=ALU.mult),
                 r=[sv_d, c3_d], w=[st3_d])
        us.append(fin)
        return us

    def units_B(m):
        ng = m + 1
        nk = ng * GRP
        I = Ib[m % 2]
        Id = Igd[m % 2]
        sv, sv_d = svb[m % 2]
        st3, st3_d = st3b[m % 2]
        us = []

        def rnd(n):
            S.op("dve", lambda E: E.tensor_scalar(out=sv[:, 2:4], in0=st3[:, 2 * n:2 * n + 2], scalar1=sv[:, 0:1], scalar2=None, op0=ALU.add),
                 r=[sv_d, st3_d], w=[sv_d, t12_d])
            S.op("act", lambda E: E.activation(out=junk[:, 0:nk], in_=I[:, 0:nk], func=AF.Sign, scale=-1.0, bias=sv[:, 3:4],
                                               accum_out=sa[:, 0:1]), r=Id[0:ng] + [t12_d], w=[junk_d, sa_d])
            S.op("dve", lambda E: E.tensor_scalar(out=mask[:, 0:nk], in0=I[:, 0:nk], scalar1=sv[:, 2:3], scalar2=None,
                                                  op0=ALU.is_ge, op1=ALU.add, accum_out=sv[:, 4:5]), r=Id[0:ng] + [sv_d], w=[mask_d, sv_d])
            S.op("dve", lambda E: E.scalar_tensor_tensor(out=sv[:, 6:7], in0=sv[:, 4:5], scalar=float(TOPK), in1=st3[:, 2 * n:2 * n + 1],
                                                         op0=ALU.is_ge, op1=ALU.mult), r=[sv_d, st3_d], w=[sv_d])
            S.op("dve", lambda E: E.scalar_tensor_tensor(out=sv[:, 7:8], in0=sa[:, 0:1], scalar=float(nk - 2 * TOPK), in1=st3[:, 2 * n:2 * n + 1],
                                                         op0=ALU.is_le, op1=ALU.mult), r=[sv_d, sa_d, st3_d], w=[sv_d])
            S.op("dve", lambda E: E.scalar_tensor_tensor(out=sv[:, 0:1], in0=sv[:, 6:7], scalar=sv[:, 7:8], in1=sv[:, 0:1],
                                                         op0=ALU.add, op1=ALU.add), r=[sv_d], w=[sv_d])
        for n in range(NR3):
            us.append(lambda n=n: rnd(n))

        def fin():
            S.op("dve", lambda E: E.tensor_scalar(out=mask[:, 0:nk], in0=I[:, 0:nk], scalar1=sv[:, 0:1], scalar2=None, op0=ALU.is_ge),
                 r=Id[0:ng] + [sv_d], w=[mask_d])
        us.append(fin)
        return us

    def run_C(m):
        ng = m + 1
        ql, ql_d = qlb[m % 2]
        po, po_d = pso
        pd, pd_d = psd
        nck = 4 * ng

        def front(c):
            cs = slice(c * CH, (c + 1) * CH)
            pT, pT_d = nextT()
            S.op("pe", lambda E: E.transpose(out=pT[0:CH, 0:CH], in_=mask[:, cs], identity=ident[0:CH, 0:CH]),
                 r=[mask_d, ident_d], w=[pT_d])
            pS, pS_d = nextA()
            S.op("pe", lambda E: E.matmul(pS[0:CH, 0:GRP], cT[:, cs], ql[:, :], start=True, stop=True),
                 r=[cT_d, ql_d], w=[pS_d])
            e_, e_d = Et[cnt["ei"] % 2]
            p_, p_d = Pt[cnt["ei"] % 2]
            cnt["ei"] += 1
            S.op("act", lambda E: E.activation(out=e_[:, :], in_=pS[0:CH, 0:GRP], func=AF.Exp), r=[pS_d], w=[e_d])
            S.op("dve", lambda E: E.tensor_tensor(out=p_[:, :, :], in0=e_[:, :].rearrange("p (h q) -> p h q", h=4),
                                                  in1=pT[0:CH, 0:CH].unsqueeze(1).to_broadcast([CH, 4, CH]), op=ALU.mult),
                 r=[e_d, pT_d], w=[p_d])
            return p_, p_d

        def back(c, p_, p_d):
            pf = p_[:, :, :].rearrange("p h q -> p (h q)")
            S.op("pe", lambda E: E.matmul(po[:, 0:GRP], ctok[:, c, :], pf, start=(c == 0), stop=(c == nck - 1)),
                 r=[ctok_d, p_d], w=[po_d])
            S.op("pe", lambda E: E.matmul(pd[:, 0:GRP], onesb[0:CH, :], pf, start=(c == 0), stop=(c == nck - 1)),
                 r=[onesb_d, p_d], w=[pd_d])

        pend = None
        for c in range(nck):
            cur = front(c)
            if pend is not None:
                back(c - 1, *pend)
            pend = cur
        back(nck - 1, *pend)
        S.op("dve", lambda E: E.reciprocal(out=rden[:, :], in_=pd[:, 0:GRP]), r=[pd_d], w=[rden_d])
        S.op("dve", lambda E: E.tensor_tensor(out=ob[:, :], in0=po[:, 0:GRP], in1=rden[:, :], op=ALU.mult), r=[po_d, rden_d], w=[ob_d])
        py, py_d = nextA()
        for h in range(4):
            S.op("pe", lambda E: E.matmul(py[0:64, h * CH:(h + 1) * CH], wuv[:, h, :], ob[:, h * CH:(h + 1) * CH], start=True, stop=True),
                 r=[wuv_d, ob_d], w=[py_d])
        S.op("act", lambda E: E.copy(out=ysb[:, :], in_=py[0:64, 0:GRP]), r=[py_d], w=[ysb_d])
        S.dma(yx_loc[YD0 + m * 256:YD0 + (m + 1) * 256, :].rearrange("(h d) t -> d h t", d=64),
              ysb[:, :].rearrange("d (h t) -> d h t", h=4), r=[ysb_d])

    def interleave(ua, ub):
        na, nb = len(ua), len(ub)
        ia = ib = 0
        while ia < na or ib < nb:
            if ib >= nb or (ia < na and ia * nb <= ib * na):
                ua[ia]()
                ia += 1
            else:
                ub[ib]()
                ib += 1

    for u in units_A(0):
        u()
    for m in range(NB):
        interleave(units_A(m + 1) if m + 1 < NB else [], units_B(m))
        run_C(m)


NGCOLS = 448
PREFETCH = False
NRANK = 8
GB = 4


def build_fused(T, NL):
    NTK = T // 4
    NCH = T // CH
    NB = NCH // 4
    NBK = NTK // CH
    NT = 342 if NTK % 342 == 0 else 228
    YF0 = 0
    YL0 = NCH * 64
    YD0 = 2 * NCH * 64
    YR = YD0 + NB * 256
    nc = bass.Bass("TRN2", target_bir_lowering=False)
    dt = lambda name, shape, kind, dtype=F32: nc.dram_tensor(name, list(shape), dtype, kind=kind).ap()
    h0 = dt("h0", [D, NTK], "ExternalInput")
    gains = dt("gains", [NL, 128, 6 * KC], "ExternalInput")
    fwin = dt("fwin", [NL * 2, 128, KC, 2 * DFF], "ExternalInput")
    fwout = dt("fwout", [NL * 2, 128, JC, D], "ExternalInput")
    pw = dt("pw", [NL, 128, KC, DIN], "ExternalInput")
    ow = dt("ow", [NL, 128, KC, D], "ExternalInput")
    bfi = dt("bfi", [NL, 1, 1], "ExternalInput")
    cwi = dt("cwi", [NL, 128, 2, CW], "ExternalInput")
    cvi = dt("cvi", [NL, 128, 2, 3], "ExternalInput")
    lcwi = dt("lcwi", [NL, 64, 4], "ExternalInput")
    lvi = dt("lvi", [NL, 64, 4], "ExternalInput")
    lwai = dt("lwai", [NL, 64, 64], "ExternalInput")
    lwii = dt("lwii", [NL, 64, 64], "ExternalInput")
    kvgi = dt("kvgi", [NL, 128, 1], "ExternalInput")
    ikgi = dt("ikgi", [NL, 32, 2], "ExternalInput")
    wuki = dt("wuki", [NL, 64, 4, 128], "ExternalInput")
    wuvi = dt("wuvi", [NL, 128, 4, 64], "ExternalInput")
    cori = dt("cori", [128, 2], "ExternalInput")
    gidx = dt("gidx", [128, NGCOLS], "ExternalInput", I32)
    h_out = dt("h_out", [D, NTK], "ExternalOutput")
    hbuf = dt("hbuf", [D, NTK], "Internal")
    zloc = dt("zloc", [DIN, NTK], "Internal")
    zq_loc = dt("zq_loc", [NBK * ZQR, CH], "Internal")
    halo_loc = dt("halo_loc", [2 * GW, HAL], "Internal")
    yx_loc = dt("yx_loc", [YR, CH], "Internal")
    yc_loc = dt("yc_loc", [GW, NTK], "Internal")
    zall = [dt("zall%d" % i, [NRANK * NZG, NTK], "Internal") for i in range(2)]
    zq_all = [dt("zq_all%d" % i, [NRANK * NBK * ZQR, CH], "Internal") for i in range(2)]
    halo_all = [dt("halo_all%d" % i, [NRANK * 2 * GW, HAL], "Internal") for i in range(2)]
    yx_all = [dt("yx_all%d" % i, [NRANK * YR, CH], "Internal") for i in range(2)]
    es = ExitStack()
    with es:
        S = Sched(nc, es)
        PS = PsumPool(S)
        G = Gather(S, NGCOLS)
        G.load(gidx)
        cor = S.sb("cor", [128, 2], F32); cor_d = Dep()
        S.dma(cor[:], cori, w=[cor_d])
        for l in range(NL):
            zA, zqA, hA, yA = zall[l % 2], zq_all[l % 2], halo_all[l % 2], yx_all[l % 2]
            zall_d, zq_d, halo_d, yx_d = Dep(), Dep(), Dep(), Dep()
            with ExitStack() as e1:
                TS = TStage(S, PS, NT, e1)
                TS.load_gains(gains[l])
                TS.load_ffn_weights(fwin[2 * l], fwout[2 * l], 0)
                TS.ffn_phase(h0 if l == 0 else hbuf, hbuf, NTK, 1)
                TS.load_sq_weights(pw[l], DIN, gi=2)
                TS.proj_phase(hbuf, zloc, zq_loc, halo_loc, NTK)
            S.barrier()
            S.collective("AllGather", halo_loc, hA, halo_d)
            S.collective("AllGather", zloc[0:NZG, :], zA, zall_d)
            S.collective("AllGather", zq_loc, zqA, zq_d)
            S.barrier()
            conv_phase(S, PS, G, T, zloc, hA, halo_d, cor[:, 1:2], cor_d, cwi[l], cvi[l], yc_loc)
            S.barrier()
            with ExitStack() as e2:
                fox_phase(S, PS, G, e2, T, zA, zall_d, bfi[l], yx_loc)
            S.barrier()
            lru_phase(S, PS, G, T, zA, zall_d, lcwi[l], lvi[l], lwai[l], lwii[l], yx_loc, YL0)
            S.barrier()
            with ExitStack() as e4:
                dsa_phase(S, PS, G, e4, T, zA, zall_d, zqA, zq_d, cor[:, 0:1], cor_d, kvgi[l], ikgi[l], wuki[l], wuvi[l],
                          yx_loc, YD0)
            S.barrier()
            S.collective("AllGather", yx_loc, yA, yx_d)
            with ExitStack() as e5:
                TS = TStage(S, PS, NT, e5)
                TS.load_gains(gains[l])
                TS.load_sq_weights(ow[l], D)

                def fill_y(sq, sq_d, t0, n, yA=yA, yx_d=yx_d):
                    S.dma(sq[:, 2:4, 0:n], yc_loc[:, t0:t0 + n].rearrange("(c p) n -> p c n", p=128), w=[sq_d])
                    for kb in range(n // CH):
                        blk = t0 // CH + kb
                        cs = slice(kb * CH, (kb + 1) * CH)
                        for kk in range(2):
                            p = np.arange(128)
                            for base, kc0, nm in ((YF0, 0, "yf"), (YL0, 4, "yl")):
                                G.gather(sq[:, kc0 + kk, cs], 128, yA, (nm, blk, kk),
                                         (lambda b, j, base=base, blk=blk, kk=kk:
                                          (GB * b + 2 * kk + p // 64) * YR + base + (j * NBK + blk) * 64 + p % 64),
                                         r=[yx_d], w=[sq_d], acc=True)
                            G.gather(sq[:, 6 + kk, cs], 128, yA, ("yd", blk, kk),
                                     (lambda b, j, blk=blk, kk=kk:
                                      (GB * b + (j * NBK + blk) % 4) * YR + YD0 + ((j * NBK + blk) // 4) * 256 + kk * 128 + p),
                                     r=[yx_d], w=[sq_d], acc=True)

                TS.mixin_phase(hbuf, hbuf, NTK, 3, fill_y)
                TS.load_ffn_weights(fwin[2 * l + 1], fwout[2 * l + 1], 4)
                TS.ffn_phase(hbuf, h_out if l == NL - 1 else hbuf, NTK, 5)
            S.barrier()
        S.finish()
        print("fused ops", S.nops, "waits", S.nwaits, "gather cols", len(G.specs))
    return nc, G


_PROGS = {}


def _f32(a):
    return np.ascontiguousarray(a, dtype=np.float32)


def fused_inputs(G, T, x, meta_tokens, norm_g, ffn_w_in, ffn_w_out, w_in, w_out, fox_b_f,
                 conv_dw_w, conv_dw_b, conv_ln_g, conv_ln_b,
                 lru_conv_w, lru_conv_b, lru_w_a, lru_b_a, lru_w_i, lru_b_i, lru_lambda,
                 dsa_kv_norm_g, dsa_w_uk, dsa_w_uv, idx_k_ln_g, idx_k_ln_b):
    A = lambda a: np.asarray(a, dtype=np.float32)
    NL = norm_g.shape[0]
    B = x.shape[0]
    NTK = T // 4
    h = np.concatenate([np.broadcast_to(A(meta_tokens)[None], (B, NMETA, D)), A(x)], axis=1)
    shared = {
        "gains": _f32(np.stack([lay_gains(A(norm_g[l])) for l in range(NL)])),
        "fwin": _f32(np.stack([lay_kc(A(ffn_w_in[l, i])) for l in range(NL) for i in range(2)])),
        "fwout": _f32(np.stack([lay_kc(A(ffn_w_out[l, i])) for l in range(NL) for i in range(2)])),
        "pw": _f32(np.stack([lay_kc(A(w_in[l])[:, Z_PERM]) for l in range(NL)])),
        "ow": _f32(np.stack([lay_kc(A(w_out[l])) for l in range(NL)])),
        "cwi": _f32(np.stack([A(conv_dw_w[l]).T.reshape(2, 128, CW).transpose(1, 0, 2) for l in range(NL)])),
        "cvi": _f32(np.stack([np.stack([A(conv_dw_b[l]), A(conv_ln_g[l]), A(conv_ln_b[l])], -1).reshape(2, 128, 3).transpose(1, 0, 2)
                              for l in range(NL)])),
        "kvgi": _f32(A(dsa_kv_norm_g).reshape(NL, 128, 1)),
        "ikgi": _f32(np.stack([A(idx_k_ln_g), A(idx_k_ln_b)], -1)),
        "wuki": _f32(A(dsa_w_uk).transpose(0, 3, 1, 2)),
        "wuvi": _f32(A(dsa_w_uv).transpose(0, 2, 1, 3)),
    }
    lvec = np.stack([A(lru_conv_b), A(lru_b_a), A(lru_b_i), A(lru_lambda)], -1)
    maps = []
    for c in range(NCORES):
        b, j = c // 4, c % 4
        m = dict(shared)
        m["h0"] = _f32(h[b, j * NTK:(j + 1) * NTK].T)
        m["bfi"] = _f32(A(fox_b_f)[:, j].reshape(NL, 1, 1))
        m["lcwi"] = _f32(A(lru_conv_w)[:, :, 64 * j:64 * j + 64].transpose(0, 2, 1))
        m["lvi"] = _f32(lvec[:, 64 * j:64 * j + 64])
        m["lwai"] = _f32(A(lru_w_a)[:, j])
        m["lwii"] = _f32(A(lru_w_i)[:, j])
        cor = np.zeros((128, 2), np.float32)
        cor[:, 0] = CH * j
        cor[:, 1] = 1.0 if j > 0 else 0.0
        m["cori"] = cor
        m["gidx"] = G.table(b, j)
        maps.append(m)
    return maps


def kernel(x, **params):
    x = np.asarray(x, dtype=np.float32)
    B = x.shape[0]
    T = T_FULL
    NTK = T // 4
    NL = params["norm_g"].shape[0]
    if "F" not in _PROGS:
        _PROGS["F"] = build_fused(T, NL)
    nc, G = _PROGS["F"]
    maps = fused_inputs(G, T, x, **params)
    res = run_bass_kernel_spmd(nc, maps, core_ids=list(range(NCORES))).results
    out = np.stack([np.concatenate([res[4 * b + j]["h_out"].T for j in range(4)], axis=0)[NMETA:] for b in range(B)], axis=0)
    return np.ascontiguousarray(out, dtype=np.float32)
```

```python
import numpy as np
from contextlib import ExitStack
import concourse.bass as bass
import concourse.mybir as mybir
from concourse.bass_utils import run_bass_kernel_spmd

F32 = mybir.dt.float32
BF16 = mybir.dt.bfloat16
I32 = mybir.dt.int32
AF = mybir.ActivationFunctionType
ALU = mybir.AluOpType
AX = mybir.AxisListType

D = 1024
DFF = 2560
KC = D // 128
JC = DFF // 128
DIN = 2476
NCORES = 8
NMETA = 16
SEQ = 8192
T_FULL = SEQ + NMETA
CH = 114
GRP = 4 * CH


class Dep:
    __slots__ = ("w", "r")

    def __init__(self):
        self.w = []
        self.r = {}


class Sched:
    def __init__(self, nc, es, n_dma_sems=28):
        self.nc = nc
        self.es = es
        self.eng = {"pe": nc.tensor, "act": nc.scalar, "dve": nc.vector,
                    "pool": nc.gpsimd, "sp": nc.sync}
        self.sems = {}
        for k in self.eng:
            self.sems[k] = es.enter_context(nc.semaphore("c_" + k))
        self.cnt = {k: 0 for k in self.eng}
        self.seen = {k: {} for k in self.eng}
        self.dpool = []
        for i in range(n_dma_sems):
            key = "d%d" % i
            self.sems[key] = es.enter_context(nc.semaphore(key))
            self.dpool.append([key, 0])
        self.dnext = 0
        self.ccs = []
        self.nwaits = 0
        self.nops = 0
        self._uid = 0

    def sb(self, name, shape, dtype, es=None):
        self._uid += 1
        return (es or self.es).enter_context(
            self.nc.sbuf_tensor("sb%d_%s" % (self._uid, name), list(shape), dtype))

    def ps(self, name, shape, dtype=F32, es=None):
        self._uid += 1
        return (es or self.es).enter_context(
            self.nc.psum_tensor("ps%d_%s" % (self._uid, name), list(shape), dtype))

    def _wait(self, eng, ev):
        if ev is None:
            return
        key, val = ev
        if key == eng and eng in ("pe", "sp"):
            return
        if self.seen[eng].get(key, 0) >= val:
            return
        self.eng[eng].wait_ge(self.sems[key], val)
        self.seen[eng][key] = val
        self.nwaits += 1

    def _deps(self, eng, r, w, acc=False):
        for d in r:
            for ev in d.w:
                self._wait(eng, ev)
        for d in w:
            for ev in d.w:
                if acc and ev[0] not in self.eng:
                    continue
                self._wait(eng, ev)
            for e, ev in d.r.items():
                self._wait(eng, ev)

    def op(self, eng, fn, r=(), w=()):
        self._deps(eng, r, w)
        ins = fn(self.eng[eng])
        self.cnt[eng] += 1
        ins.then_inc(self.sems[eng], 1)
        ev = (eng, self.cnt[eng])
        for d in r:
            d.r[eng] = ev
        for d in w:
            d.w = [ev]
            d.r = {}
        self.nops += 1
        return ev

    def dma(self, out, in_, r=(), w=(), q="sp", acc=False, fn=None, **kw):
        self._deps(q, r, w, acc=acc)
        slot = self.dpool[self.dnext]
        self.dnext = (self.dnext + 1) % len(self.dpool)
        key, val = slot
        if val > 0:
            self._wait(q, (key, val))
        if fn is None:
            ins = self.eng[q].dma_start(out=out, in_=in_, **kw)
        else:
            ins = fn(self.eng[q])
        ins.then_inc(self.sems[key], 16)
        slot[1] = val + 16
        ev = (key, val + 16)
        for d in r:
            d.r[key] = ev
        for d in w:
            if acc:
                d.w = d.w + [ev]
            else:
                d.w = [ev]
                d.r = {}
        self.nops += 1
        return ev

    def collective(self, kind, in_ap, out_ap, w_dep):
        key = "cc%d" % len(self.ccs)
        self.sems[key] = self.es.enter_context(self.nc.semaphore(key))
        self.ccs.append(key)
        ins = self.nc.gpsimd.collective_compute(kind, ALU.bypass, replica_groups=[list(range(NCORES))],
                                                ins=[in_ap], outs=[out_ap])
        ins.then_inc(self.sems[key], 1)
        w_dep.w = [(key, 1)]
        w_dep.r = {}
        self.nops += 1

    def barrier(self):
        for e in ("pe", "act", "dve", "pool", "sp"):
            for key, val in self.dpool:
                if val > 0:
                    self._wait(e, (key, val))
            for key in self.ccs:
                self._wait(e, (key, 1))
            for k in ("pe", "act", "dve", "pool"):
                if k != e and self.cnt[k] > 0:
                    self._wait(e, (k, self.cnt[k]))
            if e not in ("pe", "sp") and self.cnt[e] > 0:
                self._wait(e, (e, self.cnt[e]))

    def finish(self):
        self.barrier()


class PsumPool:
    def __init__(self, S):
        self.f = [(S.ps("pf%d" % i, [128, 512]), Dep()) for i in range(6)]
        self.b = [(S.ps("pb%d" % i, [128, 1024], BF16), Dep()) for i in range(2)]
        self.fi = 0
        self.bi = 0

    def nf(self, lo=0, hi=6):
        p = self.f[lo + self.fi % (hi - lo)]
        self.fi += 1
        return p

    def nb(self):
        p = self.b[self.bi % 2]
        self.bi += 1
        return p


class Gather:
    def __init__(self, S, ncols):
        self.S = S
        self.ncols = ncols
        self.idx = S.sb("gidx", [128, ncols], I32)
        self.idx_d = Dep()
        self.specs = []
        self.memo = {}

    def load(self, idx_dram):
        self.S.dma(self.idx[:], idx_dram, w=[self.idx_d])

    def col(self, key, P, fn):
        if key not in self.memo:
            assert len(self.specs) < self.ncols
            self.memo[key] = len(self.specs)
            self.specs.append((P, fn))
        return self.memo[key]

    def gather(self, out_ap, P, src_ap, key, fn, r=(), w=(), acc=False):
        c = self.col(key, P, fn)
        off = bass.IndirectOffsetOnAxis(ap=self.idx[0:P, c:c + 1], axis=0)
        self.S.dma(None, None, r=list(r) + [self.idx_d], w=w, q="pool", acc=acc,
                   fn=lambda E: E.indirect_dma_start(out=out_ap, out_offset=None, in_=src_ap, in_offset=off))

    def table(self, b, j):
        t = np.zeros((128, self.ncols), np.int32)
        for c, (P, fn) in enumerate(self.specs):
            t[:P, c] = np.asarray(fn(b, j), dtype=np.int64)
        return t


O_FQ, O_FK, O_FV, O_FF, O_LX, O_LG, O_DKV, O_IK, O_CU, O_DQ, O_IQ, O_IW = (
    0, 256, 512, 768, 772, 1028, 1284, 1412, 1444, 1956, 2212, 2468)
NZG = O_CU
ZQ0 = O_DQ
ZQR = DIN - ZQ0
Z_PERM = np.concatenate([np.arange(0, 772), np.arange(1284, 1796), np.arange(2052, 2180), np.arange(2436, 2468),
                         np.arange(772, 1284), np.arange(1796, 2052), np.arange(2180, 2436), np.arange(2468, 2476)])
CW = 31
HAL = CW - 1
GW = 256
NEG = -1.0e30
TOPK = 256


class TStage:
    def __init__(self, S, PS, NT, es):
        self.S = S
        self.PS = PS
        self.NT = NT
        sb = lambda n, s, d: S.sb(n, s, d, es)
        self.ones = sb("ones", [128, 128], F32)
        self.ones_d = Dep()
        S.op("dve", lambda E: E.memset(self.ones[:], 1.0), w=[self.ones_d])
        self.eps = sb("eps", [128, 1], F32)
        self.eps_d = Dep()
        S.op("dve", lambda E: E.memset(self.eps[:], 1e-6), w=[self.eps_d])
        self.win = sb("win", [128, KC, 2 * DFF], BF16)
        self.win_d = Dep()
        self.wout = sb("wout", [128, JC, D], BF16)
        self.wout_d = Dep()
        self.stages = [(sb("stg%d" % i, [128, 1280], F32), Dep()) for i in range(3)]
        self.gt = sb("gt", [128, 6 * KC], F32)
        self.gt_d = Dep()
        self.x32 = [(sb("x32_%d" % i, [128, KC, NT], F32), Dep()) for i in range(2)]
        self.sq = (sb("sq", [128, KC, NT], F32), Dep())
        self.xn = (sb("xn", [128, KC, NT], BF16), Dep())
        self.act = (sb("actb", [128, JC, NT], BF16), Dep())
        self.sg = [(sb("sg%d" % i, [128, NT], F32), Dep()) for i in range(2)]
        self.y32 = (sb("y32", [128, KC, NT], F32), Dep())
        self.rstd = (sb("rstd", [128, NT], F32), Dep())
        self.tmp = [(sb("tmp%d" % i, [128, NT], F32), Dep()) for i in range(2)]
        self.lc = 0
        self.ddeps = {}

    def ddep(self, key, t0):
        k = (key, t0)
        if k not in self.ddeps:
            self.ddeps[k] = Dep()
        return self.ddeps[k]

    def nextps(self):
        return self.PS.nf()

    def load_cast(self, dst, dst_dep, src_dram, F, scale=None, scale_dep=None):
        S = self.S
        i = self.lc
        self.lc += 1
        st, sd = self.stages[i % len(self.stages)]
        S.dma(st[:, 0:F], src_dram, w=[sd])
        e = ("act", "dve", "pool")[i % 3]
        r = [sd] + ([scale_dep] if scale_dep is not None else [])
        if scale is None:
            if e == "act":
                S.op(e, lambda E: E.copy(out=dst, in_=st[:, 0:F]), r=r, w=[dst_dep])
            else:
                S.op(e, lambda E: E.tensor_copy(out=dst, in_=st[:, 0:F]), r=r, w=[dst_dep])
        else:
            if e == "act":
                S.op(e, lambda E: E.activation(out=dst, in_=st[:, 0:F], func=AF.Copy, scale=scale), r=r, w=[dst_dep])
            else:
                S.op(e, lambda E: E.tensor_scalar(out=dst, in0=st[:, 0:F], scalar1=scale, scalar2=None, op0=ALU.mult),
                     r=r, w=[dst_dep])

    def rms_rstd(self, src, src_d, n, out_scale=1.0):
        S = self.S
        sq, sq_d = self.sq
        S.op("act", lambda E: E.activation(out=sq[:, :, 0:n], in_=src[:, :, 0:n], func=AF.Square), r=[src_d], w=[sq_d])
        ps, ps_d = self.nextps()
        for c in range(KC):
            S.op("pe", lambda E: E.matmul(ps[:, 0:n], self.ones[:], sq[:, c, 0:n], start=(c == 0), stop=(c == KC - 1)),
                 r=[self.ones_d, sq_d], w=[ps_d])
        rs, rs_d = self.rstd
        S.op("act", lambda E: E.activation(out=rs[:, 0:n], in_=ps[:, 0:n], func=AF.Sqrt, scale=1.0 / D, bias=self.eps[:, 0:1]),
             r=[ps_d, self.eps_d], w=[rs_d])
        S.op("dve", lambda E: E.reciprocal(out=rs[:, 0:n], in_=rs[:, 0:n]), r=[rs_d], w=[rs_d])
        if out_scale != 1.0:
            S.op("dve", lambda E: E.tensor_scalar(out=rs[:, 0:n], in0=rs[:, 0:n], scalar1=float(out_scale), scalar2=None, op0=ALU.mult),
                 r=[rs_d], w=[rs_d])
        return rs, rs_d

    def load_gains(self, g_dram):
        self.S.dma(self.gt[:], g_dram, w=[self.gt_d])

    def gcol(self, gi, c):
        return self.gt[:, gi * KC + c:gi * KC + c + 1]

    def load_ffn_weights(self, win_dram, wout_dram, gi):
        for c in range(KC):
            for hh in range(4):
                self.load_cast(self.win[:, c, hh * 1280:(hh + 1) * 1280], self.win_d,
                               win_dram[:, c, hh * 1280:(hh + 1) * 1280], 1280,
                               scale=self.gcol(gi, c), scale_dep=self.gt_d)
        for j in range(JC):
            self.load_cast(self.wout[:, j, :], self.wout_d, wout_dram[:, j, :], D)

    def load_sq_weights(self, w_dram, ncols, gi=None):
        for c in range(KC):
            for f0 in range(0, ncols, 1280):
                f = min(1280, ncols - f0)
                self.load_cast(self.win[:, c, f0:f0 + f], self.win_d, w_dram[:, c, f0:f0 + f], f,
                               scale=(self.gcol(gi, c) if gi is not None else None),
                               scale_dep=(self.gt_d if gi is not None else None))

    def normalize(self, x32, x_d, n):
        S = self.S
        rs, rs_d = self.rms_rstd(x32, x_d, n)
        xn, xn_d = self.xn
        for c in range(KC):
            e = "dve" if c % 2 == 0 else "pool"
            S.op(e, lambda E: E.tensor_tensor(out=xn[:, c, 0:n], in0=x32[:, c, 0:n], in1=rs[:, 0:n], op=ALU.mult),
                 r=[x_d, rs_d], w=[xn_d])
        return xn, xn_d

    def add_normed(self, x32, x_d, n, gpost, half):
        S = self.S
        y32, y_d = self.y32
        rs2, rs2_d = self.rms_rstd(y32, y_d, n, out_scale=half)
        for c in range(KC):
            tp, tp_d = self.tmp[c % 2]
            e = "dve" if c % 2 == 0 else "pool"
            S.op("dve", lambda E: E.scalar_tensor_tensor(out=tp[:, 0:n], in0=y32[:, c, 0:n], scalar=self.gcol(gpost, c),
                                                         in1=rs2[:, 0:n], op0=ALU.mult, op1=ALU.mult),
                 r=[y_d, rs2_d, self.gt_d], w=[tp_d])
            S.op(e, lambda E: E.tensor_tensor(out=x32[:, c, 0:n], in0=tp[:, 0:n], in1=x32[:, c, 0:n], op=ALU.add),
                 r=[tp_d, x_d], w=[x_d])

    def ffn_compute(self, x32, x_d, n, gpost):
        S = self.S
        xn, xn_d = self.normalize(x32, x_d, n)
        act, act_d = self.act
        for j in range(JC):
            pg, pg_d = self.nextps()
            for c in range(KC):
                S.op("pe", lambda E: E.matmul(pg[:, 0:n], self.win[:, c, j * 128:(j + 1) * 128], xn[:, c, 0:n],
                                              start=(c == 0), stop=(c == KC - 1)),
                     r=[self.win_d, xn_d], w=[pg_d])
            pu, pu_d = self.nextps()
            for c in range(KC):
                S.op("pe", lambda E: E.matmul(pu[:, 0:n], self.win[:, c, DFF + j * 128:DFF + (j + 1) * 128], xn[:, c, 0:n],
                                              start=(c == 0), stop=(c == KC - 1)),
                     r=[self.win_d, xn_d], w=[pu_d])
            sg, sg_d = self.sg[j % 2]
            S.op("act", lambda E: E.activation(out=sg[:, 0:n], in_=pg[:, 0:n], func=AF.Silu), r=[pg_d], w=[sg_d])
            S.op("dve", lambda E: E.tensor_tensor(out=act[:, j, 0:n], in0=sg[:, 0:n], in1=pu[:, 0:n], op=ALU.mult),
                 r=[sg_d, pu_d], w=[act_d])
        y32, y_d = self.y32
        for c in range(KC):
            po, po_d = self.nextps()
            for j in range(JC):
                S.op("pe", lambda E: E.matmul(po[:, 0:n], self.wout[:, j, c * 128:(c + 1) * 128], act[:, j, 0:n],
                                              start=(j == 0), stop=(j == JC - 1)),
                     r=[self.wout_d, act_d], w=[po_d])
            S.op("act", lambda E: E.copy(out=y32[:, c, 0:n], in_=po[:, 0:n]), r=[po_d], w=[y_d])
        self.add_normed(x32, x_d, n, gpost, 0.5)

    def ffn_phase(self, h_in, h_out, ntok, gpost, kin="h", kout="h"):
        S = self.S
        tl = [(t0, min(self.NT, ntok - t0)) for t0 in range(0, ntok, self.NT)]

        def load(i):
            xt, xt_d = self.x32[i % 2]
            a, m = tl[i]
            S.dma(xt[:, :, 0:m], h_in[:, a:a + m].rearrange("(c p) n -> p c n", p=128), r=[self.ddep(kin, a)], w=[xt_d])
        load(0)
        for it, (t0, n) in enumerate(tl):
            if it + 1 < len(tl):
                load(it + 1)
            x32, x_d = self.x32[it % 2]
            self.ffn_compute(x32, x_d, n, gpost)
            S.dma(h_out[:, t0:t0 + n].rearrange("(c p) n -> p c n", p=128), x32[:, :, 0:n], r=[x_d], w=[self.ddep(kout, t0)])

    def proj_phase(self, h_in, z_out, zq_out, halo_out, ntok, kin="h"):
        S = self.S
        tl = [(t0, min(self.NT, ntok - t0)) for t0 in range(0, ntok, self.NT)]

        def load(i):
            xt, xt_d = self.x32[i % 2]
            a, m = tl[i]
            S.dma(xt[:, :, 0:m], h_in[:, a:a + m].rearrange("(c p) n -> p c n", p=128), r=[self.ddep(kin, a)], w=[xt_d])
        load(0)
        for it, (t0, n) in enumerate(tl):
            if it + 1 < len(tl):
                load(it + 1)
            x32, x_d = self.x32[it % 2]
            xn, xn_d = self.normalize(x32, x_d, n)
            k = 0
            for m0 in range(0, DIN, 128):
                m = min(128, DIN - m0)
                pz, pz_d = self.nextps()
                for c in range(KC):
                    S.op("pe", lambda E: E.matmul(pz[0:m, 0:n], self.win[:, c, m0:m0 + m], xn[:, c, 0:n],
                                                  start=(c == 0), stop=(c == KC - 1)),
                         r=[self.win_d, xn_d], w=[pz_d])
                zs, zs_d = self.sg[k % 2]
                if k % 2 == 0:
                    S.op("act", lambda E: E.copy(out=zs[0:m, 0:n], in_=pz[0:m, 0:n]), r=[pz_d], w=[zs_d])
                else:
                    S.op("dve", lambda E: E.tensor_copy(out=zs[0:m, 0:n], in_=pz[0:m, 0:n]), r=[pz_d], w=[zs_d])
                S.dma(z_out[m0:m0 + m, t0:t0 + n], zs[0:m, 0:n], r=[zs_d])
                lo, hi = max(m0, ZQ0), min(m0 + m, DIN)
                if lo < hi:
                    for kb in range(n // CH):
                        blk = t0 // CH + kb
                        S.dma(zq_out[blk * ZQR + lo - ZQ0:blk * ZQR + hi - ZQ0, :], zs[lo - m0:hi - m0, kb * CH:(kb + 1) * CH], r=[zs_d])
                lo, hi = max(m0, O_CU), min(m0 + m, O_CU + 2 * GW)
                if lo < hi and t0 + n == ntok:
                    S.dma(halo_out[lo - O_CU:hi - O_CU, :], zs[lo - m0:hi - m0, n - HAL:n], r=[zs_d])
                k += 1

    def mixin_phase(self, h_in, h_out, ntok, gpost, fill_y, kin="h", kout="h"):
        S = self.S
        tl = [(t0, min(self.NT, ntok - t0)) for t0 in range(0, ntok, self.NT)]

        def load(i):
            xt, xt_d = self.x32[i % 2]
            a, m = tl[i]
            S.dma(xt[:, :, 0:m], h_in[:, a:a + m].rearrange("(c p) n -> p c n", p=128), r=[self.ddep(kin, a)], w=[xt_d])
        load(0)
        for it, (t0, n) in enumerate(tl):
            if it + 1 < len(tl):
                load(it + 1)
            x32, x_d = self.x32[it % 2]
            sq, sq_d = self.sq
            fill_y(sq, sq_d, t0, n)
            xn, xn_d = self.xn
            S.op("dve", lambda E: E.tensor_copy(out=xn[:, :, 0:n], in_=sq[:, :, 0:n]), r=[sq_d], w=[xn_d])
            y32, y_d = self.y32
            for c in range(KC):
                po, po_d = self.nextps()
                for k in range(KC):
                    S.op("pe", lambda E: E.matmul(po[:, 0:n], self.win[:, k, c * 128:(c + 1) * 128], xn[:, k, 0:n],
                                                  start=(k == 0), stop=(k == KC - 1)),
                         r=[self.win_d, xn_d], w=[po_d])
                S.op("act", lambda E: E.copy(out=y32[:, c, 0:n], in_=po[:, 0:n]), r=[po_d], w=[y_d])
            self.add_normed(x32, x_d, n, gpost, 1.0)
            S.dma(h_out[:, t0:t0 + n].rearrange("(c p) n -> p c n", p=128), x32[:, :, 0:n], r=[x_d], w=[self.ddep(kout, t0)])


def lay_kc(w):
    k = w.shape[0] // 128
    return np.ascontiguousarray(w.reshape(k, 128, w.shape[1]).transpose(1, 0, 2))


def lay_gains(g):
    return np.ascontiguousarray(g.reshape(6, KC, 128).transpose(2, 0, 1).reshape(128, 6 * KC))


def fox_phase(S, PS, G, es, T, zall, zall_d, bf_dram, yx_loc):
    NTK = T // 4
    NCH = T // CH
    NQ = T // GRP
    HD = 64
    sb = lambda n, s, d: S.sb(n, s, d, es)
    R1 = sb("R1", [65, T], F32); R1_d = Dep()
    R2 = sb("R2", [65, T], F32); R2_d = Dep()
    qa = sb("qa", [65, T], BF16); qa_d = Dep(); qar_d = Dep()
    ka = sb("ka", [65, T], BF16); ka_d = Dep(); kar_d = Dep()
    vT = sb("vT", [HD, T], BF16); vT_d = Dep()
    va = sb("va", [CH, NCH, HD + 1], BF16); va_d = Dep()
    stg = [(sb("stg%d" % i, [HD, NTK], F32), Dep()) for i in range(2)]
    ffs = sb("ffs", [4, NTK], F32); ffs_d = Dep()
    sm = sb("sm", [65, 8], F32); sm_d = Dep()
    ones = sb("ones", [65, 128], F32); ones_d = Dep()
    identb = sb("identb", [HD, HD], BF16); identb_d = Dep()
    idf = sb("idf", [HD, HD], F32); idf_d = Dep()
    cc = sb("cc", [CH, NCH + NQ], F32); cc_d = Dep()
    biasT = sb("biasT", [CH, NQ, NCH], F32); biasT_d = Dep()
    mf = sb("mf", [CH, 4, GRP], F32); mf_d = Dep()
    mask = sb("mask", [CH, 4, GRP], BF16); mask_d = Dep()
    pt = [(sb("pt%d" % i, [CH, GRP], BF16), Dep()) for i in range(4)]
    osb = [(sb("osb%d" % i, [65, GRP], F32), Dep()) for i in range(2)]
    ysb = [(sb("ysb%d" % i, [HD, GRP], F32), Dep()) for i in range(2)]
    ps_s = PS.f[0:3]
    ps_o = PS.f[3:5]
    ps_m = PS.f[5]

    S.op("dve", lambda E: E.memset(ones[:], 1.0), w=[ones_d])
    S.op("pool", lambda E: E.iota(mf[:], [[-CH, 4], [1, GRP]], base=0, channel_multiplier=-1,
                                  allow_small_or_imprecise_dtypes=True), w=[mf_d])
    S.op("dve", lambda E: E.tensor_scalar(out=mask[:], in0=mf[:], scalar1=0.0, scalar2=None, op0=ALU.is_ge),
         r=[mf_d], w=[mask_d])
    S.op("pool", lambda E: E.iota(idf[:], [[1, HD]], base=0, channel_multiplier=-1, allow_small_or_imprecise_dtypes=True), w=[idf_d])
    S.op("dve", lambda E: E.tensor_scalar(out=identb[:], in0=idf[:], scalar1=0.0, scalar2=None, op0=ALU.is_equal), r=[idf_d], w=[identb_d])
    for jc in range(4):
        G.gather(ffs[:, :], 4, zall, ("ff", jc), (lambda b, j, jc=jc: (GB * b + jc) * NZG + O_FF + (j + np.arange(4)) % 4),
                 r=[zall_d], w=[ffs_d])
        S.dma(R1[64:65, jc * NTK:(jc + 1) * NTK], ffs[0:1, :], r=[ffs_d], w=[R1_d], acc=(jc > 0))
    S.dma(sm[64:65, 0:1], bf_dram, w=[sm_d])
    S.op("dve", lambda E: E.tensor_scalar(out=sm[64:65, 1:2], in0=sm[64:65, 0:1], scalar1=-1.0, scalar2=None, op0=ALU.mult),
         r=[sm_d], w=[sm_d])
    S.op("dve", lambda E: E.memset(sm[64:65, 2:3], 1.0), r=[sm_d], w=[sm_d])
    S.op("act", lambda E: E.activation(out=R1[64:65, :], in_=R1[64:65, :], func=AF.Exp, scale=-1.0, bias=sm[64:65, 1:2]),
         r=[R1_d, sm_d], w=[R1_d])
    S.op("act", lambda E: E.activation(out=R1[64:65, :], in_=R1[64:65, :], func=AF.Ln, scale=1.0, bias=sm[64:65, 2:3]),
         r=[R1_d, sm_d], w=[R1_d])
    S.op("dve", lambda E: E.tensor_scalar(out=R1[64:65, :], in0=R1[64:65, :], scalar1=-1.0, scalar2=None, op0=ALU.mult),
         r=[R1_d], w=[R1_d])
    S.op("dve", lambda E: E.tensor_tensor_scan(out=R2[64:65, :], data0=R1[64:65, :], data1=R1[64:65, :], initial=0.0,
                                               op0=ALU.add, op1=ALU.min),
         r=[R1_d], w=[R2_d])
    i = 0
    for off, dst, dd, sc in ((O_FQ, qa, qa_d, HD ** -0.5), (O_FK, ka, ka_d, 1.0), (O_FV, vT, vT_d, 1.0)):
        for jc in range(4):
            st, sd = stg[i % 2]
            G.gather(st[:, :], HD, zall, ("fqkv", off, jc),
                     (lambda b, j, jc=jc, off=off: (GB * b + jc) * NZG + off + HD * j + np.arange(HD)), r=[zall_d], w=[sd])
            sl = slice(jc * NTK, (jc + 1) * NTK)
            if i % 2 == 0:
                S.op("act", lambda E: E.mul(out=dst[0:HD, sl], in_=st[:, :], mul=float(sc)), r=[sd], w=[dd])
            else:
                S.op("dve", lambda E: E.tensor_scalar(out=dst[0:HD, sl], in0=st[:, :], scalar1=float(sc),
                                                      scalar2=None, op0=ALU.mult), r=[sd], w=[dd])
            i += 1
    S.op("pool", lambda E: E.memset(ka[64:65, :], 1.0), w=[kar_d])
    for Q in range(NQ):
        sl = slice(Q * GRP, (Q + 1) * GRP)
        S.op("dve", lambda E: E.tensor_scalar(out=qa[64:65, sl], in0=R2[64:65, sl], scalar1=R2[64:65, Q * GRP:Q * GRP + 1],
                                              scalar2=None, op0=ALU.subtract), r=[R2_d], w=[qar_d])
    pm, pm_d = ps_m
    for c in range(NCH):
        S.op("pe", lambda E: E.matmul(pm[0:CH, c:c + 1], R2[64:65, c * CH:(c + 1) * CH], ones[64:65, 0:1], start=True, stop=True),
             r=[R2_d, ones_d], w=[pm_d])
    for Q in range(NQ):
        S.op("pe", lambda E: E.matmul(pm[0:CH, NCH + Q:NCH + Q + 1], ones[64:65, 0:CH], R2[64:65, Q * GRP:Q * GRP + 1],
                                      start=True, stop=True), r=[R2_d, ones_d], w=[pm_d])
    S.op("act", lambda E: E.copy(out=cc[:, :], in_=pm[0:CH, 0:NCH + NQ]), r=[pm_d], w=[cc_d])
    for Q in range(NQ):
        S.op("dve", lambda E: E.tensor_scalar(out=biasT[:, Q, :], in0=cc[:, 0:NCH], scalar1=cc[:, NCH + Q:NCH + Q + 1],
                                              scalar2=-1.0, op0=ALU.subtract, op1=ALU.mult), r=[cc_d], w=[biasT_d])
    S.op("pool", lambda E: E.memset(va[:, :, HD:HD + 1], 1.0), w=[va_d])
    for c0 in range(0, NCH, 4):
        pT, pT_d = PS.nb()
        for k in range(4):
            S.op("pe", lambda E: E.transpose(out=pT[0:CH, k * HD:(k + 1) * HD], in_=vT[:, (c0 + k) * CH:(c0 + k + 1) * CH],
                                             identity=identb[:, :]), r=[vT_d, identb_d], w=[pT_d])
        S.op("act", lambda E: E.copy(out=va[:, c0:c0 + 4, 0:HD], in_=pT[0:CH, 0:4 * HD].rearrange("p (a b) -> p a b", a=4)),
             r=[pT_d], w=[va_d])

    si = 0
    for Q in range(NQ):
        qsl = slice(Q * GRP, (Q + 1) * GRP)
        po, po_d = ps_o[Q % 2]
        nck = 4 * Q + 4
        pend = None

        def qk(c):
            nonlocal si
            pS, pS_d = ps_s[si % 3]
            ptile, pt_d = pt[si % 4]
            si += 1
            S.op("pe", lambda E: E.matmul(pS[0:CH, 0:GRP], ka[0:65, c * CH:(c + 1) * CH], qa[0:65, qsl], start=True, stop=True),
                 r=[ka_d, kar_d, qa_d, qar_d], w=[pS_d])
            S.op("act", lambda E: E.activation(out=ptile[:, :], in_=pS[0:CH, 0:GRP], func=AF.Exp, scale=1.0,
                                               bias=biasT[:, Q, c:c + 1]), r=[pS_d, biasT_d], w=[pt_d])
            d = c - 4 * Q
            if d >= 0:
                S.op("dve", lambda E: E.tensor_tensor(out=ptile[:, :], in0=ptile[:, :], in1=mask[:, d, :], op=ALU.mult),
                     r=[pt_d, mask_d], w=[pt_d])
            return ptile, pt_d

        def pv(c, ptile, pt_d):
            S.op("pe", lambda E: E.matmul(po[0:HD + 1, 0:GRP], va[:, c, :], ptile[:, :], start=(c == 0), stop=(c == nck - 1)),
                 r=[va_d, pt_d], w=[po_d])

        for c in range(nck):
            cur = qk(c)
            if pend is not None:
                pv(c - 1, *pend)
            pend = cur
        pv(nck - 1, *pend)
        ob, ob_d = osb[Q % 2]
        S.op("act", lambda E: E.copy(out=ob[:, :], in_=po[0:HD + 1, 0:GRP]), r=[po_d], w=[ob_d])
        S.op("dve", lambda E: E.reciprocal(out=ob[64:65, :], in_=ob[64:65, :]), r=[ob_d], w=[ob_d])
        pb, pb_d = ps_m
        S.op("pe", lambda E: E.matmul(pb[0:HD, 0:GRP], ones[64:65, 0:HD], ob[64:65, :], start=True, stop=True),
             r=[ones_d, ob_d], w=[pb_d])
        yb, yb_d = ysb[Q % 2]
        S.op("dve", lambda E: E.tensor_tensor(out=yb[:, :], in0=ob[0:HD, :], in1=pb[0:HD, 0:GRP], op=ALU.mult),
             r=[ob_d, pb_d], w=[yb_d])
        S.dma(yx_loc[4 * Q * HD:(4 * Q + 4) * HD, :].rearrange("(i d) t -> d i t", d=HD),
              yb[:, :].rearrange("d (i t) -> d i t", i=4), r=[yb_d])


def conv_phase(S, PS, G, T, zloc, halo_all, halo_d, halo_on, halo_on_d, cw_dram, cv_dram, yc_loc):
    NTK = T // 4
    NT = 342 if NTK % 342 == 0 else 228
    nextps = PS.nf
    with ExitStack() as es2:
        sb = lambda n, s, d: S.sb(n, s, d, es2)
        cst = sb("cst", [128, 4], F32); cst_d = Dep()
        S.op("dve", lambda E: E.memset(cst[:, 0:1], 1e-5), w=[cst_d])
        S.op("dve", lambda E: E.memset(cst[:, 1:2], 1.0), w=[cst_d])
        o256 = sb("o256", [128, 128], F32); o256_d = Dep()
        S.op("dve", lambda E: E.memset(o256[:], 1.0 / GW), w=[o256_d])
        A = sb("A", [128, 2, HAL + NTK], F32); A_d = [Dep(), Dep()]
        Gt = sb("G", [128, 2, HAL + NTK], F32); G_d = [Dep(), Dep()]
        acc = sb("acc", [128, 2, NTK], F32); acc_d = [Dep(), Dep()]
        cw = sb("cw", [128, 2, CW], F32); cw_d = Dep()
        cv = sb("cv", [128, 2, 3], F32); cv_d = Dep()
        yc = [(sb("yc%d" % i, [128, NT], F32), Dep()) for i in range(2)]
        sq = [(sb("sqc%d" % i, [128, NT], F32), Dep()) for i in range(2)]
        rs = (sb("rsc", [128, NT], F32), Dep())
        yo = [(sb("yo%d" % i, [128, NT], F32), Dep()) for i in range(2)]
        S.dma(cw[:], cw_dram, w=[cw_d])
        S.dma(cv[:], cv_dram, w=[cv_d])
        for cc in range(2):
            G.gather(A[:, cc, 0:HAL], 128, halo_all, ("halo", cc),
                     (lambda b, j, cc=cc: (GB * b + max(j - 1, 0)) * 2 * GW + cc * 128 + np.arange(128)), r=[halo_d], w=[A_d[cc]])
            G.gather(Gt[:, cc, 0:HAL], 128, halo_all, ("halo", 2 + cc),
                     (lambda b, j, cc=cc: (GB * b + max(j - 1, 0)) * 2 * GW + GW + cc * 128 + np.arange(128)), r=[halo_d], w=[G_d[cc]])
            S.dma(A[:, cc, HAL:], zloc[O_CU + cc * 128:O_CU + (cc + 1) * 128, :], w=[A_d[cc]], acc=True)
            S.dma(Gt[:, cc, HAL:], zloc[O_CU + GW + cc * 128:O_CU + GW + (cc + 1) * 128, :], w=[G_d[cc]], acc=True)
            S.op("dve", lambda E: E.tensor_scalar(out=A[:, cc, 0:HAL], in0=A[:, cc, 0:HAL], scalar1=halo_on[:, 0:1], scalar2=None,
                                                  op0=ALU.mult), r=[A_d[cc], halo_on_d], w=[A_d[cc]])
        for cc in range(2):
            e = "dve" if cc == 0 else "pool"
            S.op("act", lambda E: E.activation(out=Gt[:, cc, :], in_=Gt[:, cc, :], func=AF.Sigmoid), r=[G_d[cc]], w=[G_d[cc]])
            S.op(e, lambda E: E.tensor_tensor(out=A[:, cc, :], in0=A[:, cc, :], in1=Gt[:, cc, :], op=ALU.mult),
                 r=[A_d[cc], G_d[cc]], w=[A_d[cc]])
        for k in range(CW):
            for cc in range(2):
                if k == 0:
                    S.op("dve", lambda E: E.tensor_scalar(out=acc[:, cc, :], in0=A[:, cc, 0:NTK], scalar1=cw[:, cc, 0:1],
                                                          scalar2=cv[:, cc, 0:1], op0=ALU.mult, op1=ALU.add),
                         r=[A_d[cc], cw_d, cv_d], w=[acc_d[cc]])
                else:
                    S.op("dve", lambda E: E.scalar_tensor_tensor(out=acc[:, cc, :], in0=A[:, cc, k:k + NTK], scalar=cw[:, cc, k:k + 1],
                                                                 in1=acc[:, cc, :], op0=ALU.mult, op1=ALU.add),
                         r=[A_d[cc], cw_d, acc_d[cc]], w=[acc_d[cc]])
        for t0 in range(0, NTK, NT):
            n = min(NT, NTK - t0)
            pm, pm_d = nextps()
            for cc in range(2):
                S.op("pe", lambda E: E.matmul(pm[:, 0:n], o256[:], acc[:, cc, t0:t0 + n], start=(cc == 0), stop=(cc == 1)),
                     r=[o256_d, acc_d[cc]], w=[pm_d])
            pv, pv_d = nextps()
            for cc in range(2):
                y_, y_d = yc[cc]
                s_, s_d = sq[cc]
                S.op("dve", lambda E: E.tensor_tensor(out=y_[:, 0:n], in0=acc[:, cc, t0:t0 + n], in1=pm[:, 0:n], op=ALU.subtract),
                     r=[acc_d[cc], pm_d], w=[y_d])
                S.op("act", lambda E: E.activation(out=s_[:, 0:n], in_=y_[:, 0:n], func=AF.Square), r=[y_d], w=[s_d])
                S.op("pe", lambda E: E.matmul(pv[:, 0:n], o256[:], s_[:, 0:n], start=(cc == 0), stop=(cc == 1)),
                     r=[o256_d, s_d], w=[pv_d])
            r_, r_d = rs
            S.op("act", lambda E: E.activation(out=r_[:, 0:n], in_=pv[:, 0:n], func=AF.Sqrt, scale=1.0, bias=cst[:, 0:1]),
                 r=[pv_d, cst_d], w=[r_d])
            S.op("dve", lambda E: E.reciprocal(out=r_[:, 0:n], in_=r_[:, 0:n]), r=[r_d], w=[r_d])
            for cc in range(2):
                y_, y_d = yc[cc]
                o_, o_d = yo[cc]
                e = "dve" if cc == 0 else "pool"
                S.op(e, lambda E: E.tensor_tensor(out=y_[:, 0:n], in0=y_[:, 0:n], in1=r_[:, 0:n], op=ALU.mult),
                     r=[y_d, r_d], w=[y_d])
                S.op("act", lambda E: E.activation(out=o_[:, 0:n], in_=y_[:, 0:n], func=AF.Silu, scale=cv[:, cc, 1:2],
                                                   bias=cv[:, cc, 2:3]), r=[y_d, cv_d], w=[o_d])
                S.dma(yc_loc[cc * 128:(cc + 1) * 128, t0:t0 + n], o_[:, 0:n], r=[o_d])


def lru_phase(S, PS, G, T, zall, zall_d, lcw_dram, lv_dram, lwa_dram, lwi_dram, yx_loc, YL0):
    NTK = T // 4
    SEG = NTK
    NBK = NTK // CH
    SUB = 342 if SEG % 342 == 0 else 228
    nextps = PS.nf
    with ExitStack() as es3:
        sb = lambda n, s, d: S.sb(n, s, d, es3)
        cst = sb("cst2", [128, 4], F32); cst_d = Dep()
        S.op("dve", lambda E: E.memset(cst[:, 1:2], 1.0), w=[cst_d])
        X = sb("X", [64, 3 + SEG], F32); X_d = Dep()
        Gt = sb("Gl", [64, SEG], F32); Gt_d = Dep()
        xc = sb("xc", [64, SEG], F32); xc_d = Dep()
        rt = sb("rt", [64, SEG], F32); rt_d = Dep()
        itl = sb("itl", [64, SEG], F32); it_d = Dep()
        at = sb("at", [64, SEG], F32); at_d = Dep()
        ut = sb("ut", [64, SEG], F32); ut_d = Dep()
        hs = sb("hs", [64, SEG], F32); hs_d = Dep()
        g1 = sb("g1", [64, SEG], F32); g1_d = Dep()
        lcw = sb("lcw", [64, 4], F32); lcw_d = Dep()
        lv = sb("lv", [64, 8], F32); lv_d = Dep()
        lwa = sb("lwa", [64, 64], F32); lwa_d = Dep()
        lwi = sb("lwi", [64, 64], F32); lwi_d = Dep()
        carry = sb("carry", [64, 1], F32); carry_d = Dep()
        S.dma(lcw[:], lcw_dram, w=[lcw_d])
        S.dma(lv[:, 0:4], lv_dram, w=[lv_d])
        S.dma(lwa[:], lwa_dram, w=[lwa_d])
        S.dma(lwi[:], lwi_dram, w=[lwi_d])
        S.op("act", lambda E: E.activation(out=lv[:, 4:5], in_=lv[:, 3:4], func=AF.Exp, scale=-1.0), r=[lv_d], w=[lv_d])
        S.op("act", lambda E: E.activation(out=lv[:, 4:5], in_=lv[:, 4:5], func=AF.Ln, scale=1.0, bias=cst[0:64, 1:2]),
             r=[lv_d, cst_d], w=[lv_d])
        S.op("dve", lambda E: E.tensor_scalar(out=lv[:, 5:6], in0=lv[:, 4:5], scalar1=-16.0, scalar2=None, op0=ALU.mult),
             r=[lv_d], w=[lv_d])
        S.op("dve", lambda E: E.tensor_scalar(out=lv[:, 4:5], in0=lv[:, 4:5], scalar1=-8.0, scalar2=None, op0=ALU.mult),
             r=[lv_d], w=[lv_d])
        S.op("dve", lambda E: E.memset(carry[:], 0.0), w=[carry_d])
        S.op("dve", lambda E: E.memset(X[:, 0:3], 0.0), w=[X_d])
        for s in range(4):
            t0 = s * SEG
            if s > 0:
                S.op("dve", lambda E: E.tensor_copy(out=X[:, 0:3], in_=X[:, SEG:SEG + 3]), r=[X_d], w=[X_d])
            G.gather(X[:, 3:3 + SEG], 64, zall, ("lx", s), (lambda b, j, s=s: (GB * b + s) * NZG + O_LX + 64 * j + np.arange(64)),
                     r=[zall_d], w=[X_d], acc=True)
            G.gather(Gt[:, :], 64, zall, ("lg", s), (lambda b, j, s=s: (GB * b + s) * NZG + O_LG + 64 * j + np.arange(64)),
                     r=[zall_d], w=[Gt_d])
            S.op("dve", lambda E: E.tensor_scalar(out=xc[:, :], in0=X[:, 0:SEG], scalar1=lcw[:, 0:1], scalar2=lv[:, 0:1],
                                                  op0=ALU.mult, op1=ALU.add), r=[X_d, lcw_d, lv_d], w=[xc_d])
            for k in range(1, 4):
                S.op("dve", lambda E: E.scalar_tensor_tensor(out=xc[:, :], in0=X[:, k:k + SEG], scalar=lcw[:, k:k + 1], in1=xc[:, :],
                                                             op0=ALU.mult, op1=ALU.add), r=[X_d, lcw_d, xc_d], w=[xc_d])
            for u0 in range(0, SEG, SUB):
                pa, pa_d = nextps()
                S.op("pe", lambda E: E.matmul(pa[0:64, 0:SUB], lwa[:, :], xc[:, u0:u0 + SUB], start=True, stop=True),
                     r=[lwa_d, xc_d], w=[pa_d])
                S.op("act", lambda E: E.activation(out=rt[:, u0:u0 + SUB], in_=pa[0:64, 0:SUB], func=AF.Sigmoid, scale=1.0,
                                                   bias=lv[:, 1:2]), r=[pa_d, lv_d], w=[rt_d])
                pb, pb_d = nextps()
                S.op("pe", lambda E: E.matmul(pb[0:64, 0:SUB], lwi[:, :], xc[:, u0:u0 + SUB], start=True, stop=True),
                     r=[lwi_d, xc_d], w=[pb_d])
                S.op("act", lambda E: E.activation(out=itl[:, u0:u0 + SUB], in_=pb[0:64, 0:SUB], func=AF.Sigmoid, scale=1.0,
                                                   bias=lv[:, 2:3]), r=[pb_d, lv_d], w=[it_d])
            S.op("act", lambda E: E.activation(out=at[:, :], in_=rt[:, :], func=AF.Exp, scale=lv[:, 4:5]), r=[rt_d, lv_d], w=[at_d])
            S.op("act", lambda E: E.activation(out=ut[:, :], in_=rt[:, :], func=AF.Exp, scale=lv[:, 5:6]), r=[rt_d, lv_d], w=[ut_d])
            S.op("pool", lambda E: E.tensor_scalar(out=ut[:, :], in0=ut[:, :], scalar1=-1.0, scalar2=1.0, op0=ALU.mult, op1=ALU.add),
                 r=[ut_d], w=[ut_d])
            S.op("act", lambda E: E.activation(out=ut[:, :], in_=ut[:, :], func=AF.Sqrt), r=[ut_d], w=[ut_d])
            S.op("pool", lambda E: E.tensor_tensor(out=itl[:, :], in0=itl[:, :], in1=xc[:, :], op=ALU.mult), r=[it_d, xc_d], w=[it_d])
            S.op("pool", lambda E: E.tensor_tensor(out=ut[:, :], in0=ut[:, :], in1=itl[:, :], op=ALU.mult), r=[ut_d, it_d], w=[ut_d])
            S.op("dve", lambda E: E.tensor_tensor_scan(out=hs[:, :], data0=at[:, :], data1=ut[:, :], initial=carry[:, 0:1],
                                                       op0=ALU.mult, op1=ALU.add), r=[at_d, ut_d, carry_d], w=[hs_d])
            S.op("dve", lambda E: E.tensor_copy(out=carry[:, :], in_=hs[:, SEG - 1:SEG]), r=[hs_d], w=[carry_d])
            S.op("act", lambda E: E.activation(out=g1[:, :], in_=Gt[:, :], func=AF.Square), r=[Gt_d], w=[g1_d])
            S.op("pool", lambda E: E.tensor_scalar(out=g1[:, :], in0=g1[:, :], scalar1=0.044715, scalar2=1.0, op0=ALU.mult, op1=ALU.add),
                 r=[g1_d], w=[g1_d])
            S.op("pool", lambda E: E.tensor_tensor(out=g1[:, :], in0=g1[:, :], in1=Gt[:, :], op=ALU.mult), r=[g1_d, Gt_d], w=[g1_d])
            S.op("act", lambda E: E.activation(out=g1[:, :], in_=g1[:, :], func=AF.Sigmoid, scale=1.5957691216057308), r=[g1_d], w=[g1_d])
            S.op("pool", lambda E: E.tensor_tensor(out=g1[:, :], in0=g1[:, :], in1=Gt[:, :], op=ALU.mult), r=[g1_d, Gt_d], w=[g1_d])
            S.op("dve", lambda E: E.tensor_tensor(out=hs[:, :], in0=hs[:, :], in1=g1[:, :], op=ALU.mult), r=[hs_d, g1_d], w=[hs_d])
            S.dma(yx_loc[YL0 + s * NBK * 64:YL0 + (s + 1) * NBK * 64, :].rearrange("(i d) t -> d i t", d=64),
                  hs[:, :].rearrange("d (i t) -> d i t", t=CH), r=[hs_d])


def dsa_phase(S, PS, G, es, T, zall, zall_d, zq_all, zq_d, joff, joff_d, kvg_dram, ikg_dram, wuk_dram, wuv_dram,
              yx_loc, YD0, NR=22):
    NTK = T // 4
    NCH = T // CH
    NB = NCH // 4
    NBK = NTK // CH
    LAT = 128
    sb = lambda n, s, d: S.sb(n, s, d, es)
    psA = PS.f[0:3]
    pso = PS.f[3]
    psd = PS.f[4]
    ai = [0]

    def nextA():
        p = psA[ai[0] % 3]
        ai[0] += 1
        return p

    nextT = PS.nb
    cst = sb("cst", [128, 4], F32); cst_d = Dep()
    S.op("dve", lambda E: E.memset(cst[:, 0:1], 1e-6), w=[cst_d])
    S.op("dve", lambda E: E.memset(cst[:, 1:2], 1e-5), w=[cst_d])
    idf = sb("idf", [128, 128], F32); idf_d = Dep()
    ident = sb("ident", [128, 128], BF16); ident_d = Dep()
    identf = sb("identf", [128, 128], F32); identf_d = Dep()
    S.op("pool", lambda E: E.iota(idf[:], [[1, 128]], base=0, channel_multiplier=-1, allow_small_or_imprecise_dtypes=True), w=[idf_d])
    S.op("dve", lambda E: E.tensor_scalar(out=ident[:], in0=idf[:], scalar1=0.0, scalar2=None, op0=ALU.is_equal), r=[idf_d], w=[ident_d])
    S.op("dve", lambda E: E.tensor_scalar(out=identf[:], in0=idf[:], scalar1=0.0, scalar2=None, op0=ALU.is_equal), r=[idf_d], w=[identf_d])
    onesb = sb("onesb", [128, 128], BF16); onesb_d = Dep()
    S.op("dve", lambda E: E.memset(onesb[:], 1.0), w=[onesb_d])
    onesL = sb("onesL", [128, 128], F32); onesL_d = Dep()
    S.op("dve", lambda E: E.memset(onesL[:], 1.0 / LAT), w=[onesL_d])
    ones32 = sb("ones32", [32, 32], F32); ones32_d = Dep()
    S.op("dve", lambda E: E.memset(ones32[:], 1.0 / 32), w=[ones32_d])
    negm = sb("negm", [CH, GRP], F32); negm_d = Dep()
    S.op("pool", lambda E: E.iota(negm[:], [[1, GRP]], base=0, channel_multiplier=-1, allow_small_or_imprecise_dtypes=True), w=[negm_d])
    S.op("dve", lambda E: E.tensor_scalar(out=negm[:], in0=negm[:], scalar1=joff[0:CH, 0:1], scalar2=NEG, op0=ALU.is_gt, op1=ALU.mult),
         r=[negm_d, joff_d], w=[negm_d])
    ctok = sb("ctok", [CH, NCH, LAT], BF16); ctok_d = Dep()
    cT = sb("cT", [LAT, T], BF16); cT_d = Dep()
    kiT = sb("kiT", [32, T], F32); kiT_d = Dep()
    wuk = sb("wuk", [64, 4, LAT], F32); wuk_d = Dep()
    wuv = sb("wuv", [LAT, 4, 64], BF16); wuv_d = Dep()
    kvg = sb("kvg", [LAT, 1], F32); kvg_d = Dep()
    ikg = sb("ikg", [32, 2], F32); ikg_d = Dep()
    S.dma(kvg[:], kvg_dram, w=[kvg_d])
    S.dma(ikg[:], ikg_dram, w=[ikg_d])
    S.dma(wuk[:], wuk_dram, w=[wuk_d])

    with ExitStack() as es2:
        sb2 = lambda n, s, d: S.sb(n, s, d, es2)
        dkvT = sb2("dkvT", [LAT, T], F32); dkvT_d = Dep()
        ikT = sb2("ikT", [32, T], F32); ikT_d = Dep()
        sq = [(sb2("sqp%d" % i, [LAT, GRP], F32), Dep()) for i in range(2)]
        rs = [(sb2("rsp%d" % i, [LAT, GRP], F32), Dep()) for i in range(2)]
        wst = sb2("wst", [128, 256], F32); wst_d = Dep()
        S.dma(wst[:, :], wuv_dram.rearrange("p a b -> p (a b)"), w=[wst_d])
        S.op("dve", lambda E: E.tensor_copy(out=wuv[:].rearrange("p a b -> p (a b)"), in_=wst[:, :]), r=[wst_d], w=[wuv_d])
        for s in range(4):
            G.gather(dkvT[:, s * NTK:(s + 1) * NTK], LAT, zall, ("dkv", s),
                     (lambda b, j, s=s: (GB * b + s) * NZG + O_DKV + np.arange(LAT)), r=[zall_d], w=[dkvT_d], acc=(s > 0))
            G.gather(ikT[:, s * NTK:(s + 1) * NTK], 32, zall, ("ik", s),
                     (lambda b, j, s=s: (GB * b + s) * NZG + O_IK + np.arange(32)), r=[zall_d], w=[ikT_d], acc=(s > 0))
        for g in range(T // GRP):
            gs = slice(g * GRP, (g + 1) * GRP)
            s_, s_d = sq[g % 2]
            r_, r_d = rs[g % 2]
            S.op("act", lambda E: E.activation(out=s_[:, :], in_=dkvT[:, gs], func=AF.Square), r=[dkvT_d], w=[s_d])
            pm, pm_d = nextA()
            S.op("pe", lambda E: E.matmul(pm[:, 0:GRP], onesL[:, :], s_[:, :], start=True, stop=True), r=[onesL_d, s_d], w=[pm_d])
            S.op("act", lambda E: E.activation(out=r_[:, :], in_=pm[:, 0:GRP], func=AF.Sqrt, scale=1.0, bias=cst[:, 0:1]),
                 r=[pm_d, cst_d], w=[r_d])
            S.op("dve", lambda E: E.reciprocal(out=r_[:, :], in_=r_[:, :]), r=[r_d], w=[r_d])
            S.op("dve", lambda E: E.scalar_tensor_tensor(out=cT[:, gs], in0=dkvT[:, gs], scalar=kvg[:, 0:1], in1=r_[:, :],
                                                         op0=ALU.mult, op1=ALU.mult), r=[dkvT_d, kvg_d, r_d], w=[cT_d])
            pmu, pmu_d = nextA()
            S.op("pe", lambda E: E.matmul(pmu[0:32, 0:GRP], ones32[:, :], ikT[:, gs], start=True, stop=True), r=[ones32_d, ikT_d], w=[pmu_d])
            S.op("dve", lambda E: E.tensor_tensor(out=ikT[:, gs], in0=ikT[:, gs], in1=pmu[0:32, 0:GRP], op=ALU.subtract),
                 r=[ikT_d, pmu_d], w=[ikT_d])
            s2_, s2_d = sq[(g + 1) % 2]
            S.op("act", lambda E: E.activation(out=s2_[0:32, :], in_=ikT[:, gs], func=AF.Square), r=[ikT_d], w=[s2_d])
            pvr, pvr_d = nextA()
            S.op("pe", lambda E: E.matmul(pvr[0:32, 0:GRP], ones32[:, :], s2_[0:32, :], start=True, stop=True), r=[ones32_d, s2_d], w=[pvr_d])
            r2_, r2_d = rs[(g + 1) % 2]
            S.op("act", lambda E: E.activation(out=r2_[0:32, :], in_=pvr[0:32, 0:GRP], func=AF.Sqrt, scale=1.0, bias=cst[0:32, 1:2]),
                 r=[pvr_d, cst_d], w=[r2_d])
            S.op("dve", lambda E: E.reciprocal(out=r2_[0:32, :], in_=r2_[0:32, :]), r=[r2_d], w=[r2_d])
            S.op("dve", lambda E: E.tensor_tensor(out=ikT[:, gs], in0=ikT[:, gs], in1=r2_[0:32, :], op=ALU.mult), r=[ikT_d, r2_d], w=[ikT_d])
            S.op("dve", lambda E: E.tensor_scalar(out=kiT[:, gs], in0=ikT[:, gs], scalar1=ikg[:, 0:1], scalar2=ikg[:, 1:2],
                                                  op0=ALU.mult, op1=ALU.add), r=[ikT_d, ikg_d], w=[kiT_d])
            pT, pT_d = nextT()
            for k in range(4):
                c = 4 * g + k
                S.op("pe", lambda E: E.transpose(out=pT[0:CH, k * LAT:(k + 1) * LAT], in_=cT[:, c * CH:(c + 1) * CH], identity=ident[:, :]),
                     r=[cT_d, ident_d], w=[pT_d])
            S.op("act", lambda E: E.copy(out=ctok[:, 4 * g:4 * g + 4, :], in_=pT[0:CH, 0:4 * LAT].rearrange("p (a b) -> p a b", a=4)),
                 r=[pT_d], w=[ctok_d])
    S.barrier()

    Ib = [sb("I%d" % i, [CH, T], F32) for i in range(2)]
    Igd = [[Dep() for _ in range(NB)] for _ in range(2)]
    mask = sb("mask", [CH, T], BF16); mask_d = Dep()
    junk = sb("junk", [CH, T], BF16); junk_d = Dep()
    rt = [(sb("rt%d" % i, [CH, GRP], F32), Dep()) for i in range(3)]
    Et = [(sb("Et%d" % i, [CH, GRP], BF16), Dep()) for i in range(2)]
    Pt = [(sb("Pt%d" % i, [CH, 4, CH], BF16), Dep()) for i in range(2)]
    qlb = [(sb("qlat%d" % i, [LAT, GRP], BF16), Dep()) for i in range(2)]
    svb = [(sb("sv%d" % i, [CH, 8], F32), Dep()) for i in range(2)]
    t12_d = Dep()
    sa = sb("sa", [CH, 2], F32); sa_d = Dep()
    NR3 = 10
    c3 = sb("c3", [CH, 2 * NR3], F32); c3_d = Dep()
    st3b = [(sb("st3_%d" % i, [CH, 2 * NR3], F32), Dep()) for i in range(2)]
    for n in range(NR3):
        S.op("dve", lambda E: E.memset(c3[:, 2 * n:2 * n + 1], float(3.0 ** -(n + 1))), w=[c3_d])
        S.op("dve", lambda E: E.memset(c3[:, 2 * n + 1:2 * n + 2], float(2.0 * 3.0 ** -(n + 1))), w=[c3_d])
    rden = sb("rden", [LAT, GRP], F32); rden_d = Dep()
    ob = sb("ob", [LAT, GRP], BF16); ob_d = Dep()
    ysb = sb("ysb", [64, GRP], F32); ysb_d = Dep()
    dqb = [(sb("dqb%d" % i, [64, 4, CH], F32), Dep()) for i in range(2)]
    iqb = [(sb("iqb%d" % i, [32, 8, CH], F32), Dep()) for i in range(2)]
    iwb = [(sb("iwb%d" % i, [8, CH], F32), Dep()) for i in range(2)]
    w16 = [(sb("w16_%d" % i, [CH, 8], F32), Dep()) for i in range(2)]
    cnt = {"ri": 0, "ei": 0}

    def qrow(m, f0):
        return lambda b, j, m=m, f0=f0: ((GB * b + (4 * m + j) // NBK) * NBK + (4 * m + j) % NBK) * ZQR + f0

    def units_A(m):
        ng = m + 1
        nk = ng * GRP
        I = Ib[m % 2]
        Id = Igd[m % 2]
        dqT, dqT_d = dqb[m % 2]
        iqT, iqT_d = iqb[m % 2]
        iwT, iwT_d = iwb[m % 2]
        w_, w_d = w16[m % 2]
        ql, ql_d = qlb[m % 2]
        sv, sv_d = svb[m % 2]
        st3, st3_d = st3b[m % 2]
        us = []

        def load():
            for h in range(4):
                G.gather(dqT[:, h, :], 64, zq_all, ("dq", m, h),
                         (lambda b, j, m=m, h=h: qrow(m, O_DQ - ZQ0 + 64 * h)(b, j) + np.arange(64)), r=[zq_d], w=[dqT_d], acc=(h > 0))
            for h in range(8):
                G.gather(iqT[:, h, :], 32, zq_all, ("iq", m, h),
                         (lambda b, j, m=m, h=h: qrow(m, O_IQ - ZQ0 + 32 * h)(b, j) + np.arange(32)), r=[zq_d], w=[iqT_d], acc=(h > 0))
            G.gather(iwT[:, :], 8, zq_all, ("iw", m), (lambda b, j, m=m: qrow(m, O_IW - ZQ0)(b, j) + np.arange(8)), r=[zq_d], w=[iwT_d])
            pw_, pw_d = nextA()
            S.op("pe", lambda E: E.transpose(out=pw_[0:CH, 0:8], in_=iwT[:, :], identity=identf[0:8, 0:8]), r=[iwT_d, identf_d], w=[pw_d])
            S.op("act", lambda E: E.mul(out=w_[:, :], in_=pw_[0:CH, 0:8], mul=1.0 / 16), r=[pw_d], w=[w_d])
            pq, pq_d = nextA()
            for h in range(4):
                S.op("pe", lambda E: E.matmul(pq[:, h * CH:(h + 1) * CH], wuk[:, h, :], dqT[:, h, :], start=True, stop=True),
                     r=[wuk_d, dqT_d], w=[pq_d])
            S.op("act", lambda E: E.mul(out=ql[:, :], in_=pq[:, 0:GRP], mul=0.125), r=[pq_d], w=[ql_d])
        us.append(load)

        def score(h, g):
            ks = slice(g * GRP, (g + 1) * GRP)
            px, px_d = nextA()
            S.op("pe", lambda E: E.matmul(px[0:CH, 0:GRP], iqT[:, h, :], kiT[:, ks], start=True, stop=True),
                 r=[iqT_d, kiT_d], w=[px_d])
            r_, r_d = rt[cnt["ri"] % 3]
            cnt["ri"] += 1
            S.op("act", lambda E: E.activation(out=r_[:, :], in_=px[0:CH, 0:GRP], func=AF.Relu), r=[px_d], w=[r_d])
            if h == 0:
                S.op("dve", lambda E: E.tensor_scalar(out=I[:, ks], in0=r_[:, :], scalar1=w_[:, 0:1], scalar2=None, op0=ALU.mult),
                     r=[r_d, w_d], w=[Id[g]])
            else:
                S.op("dve", lambda E: E.scalar_tensor_tensor(out=I[:, ks], in0=r_[:, :], scalar=w_[:, h:h + 1], in1=I[:, ks],
                                                             op0=ALU.mult, op1=ALU.add), r=[r_d, w_d, Id[g]], w=[Id[g]])
        for h in range(8):
            for g in range(ng):
                us.append(lambda h=h, g=g: score(h, g))

        def fin():
            S.op("dve", lambda E: E.tensor_reduce(out=sv[:, 0:1], in_=I[:, 0:nk], axis=AX.X, op=ALU.min), r=Id[0:ng], w=[sv_d])
            S.op("dve", lambda E: E.tensor_reduce(out=sv[:, 5:6], in_=I[:, 0:nk], axis=AX.X, op=ALU.max), r=Id[0:ng], w=[sv_d])
            S.op("dve", lambda E: E.tensor_tensor(out=sv[:, 1:2], in0=sv[:, 5:6], in1=sv[:, 0:1], op=ALU.subtract), r=[sv_d], w=[sv_d])
            S.op("dve", lambda E: E.tensor_scalar(out=sv[:, 1:2], in0=sv[:, 1:2], scalar1=1.0001, scalar2=1e-20, op0=ALU.mult, op1=ALU.add),
                 r=[sv_d], w=[sv_d])
            S.op("dve", lambda E: E.tensor_tensor(out=I[:, m * GRP:nk], in0=I[:, m * GRP:nk], in1=negm[:, :], op=ALU.add),
                 r=[Id[m], negm_d], w=[Id[m]])
            S.op("dve", lambda E: E.tensor_scalar(out=st3[:, :], in0=c3[:, :], scalar1=sv[:, 1:2], scalar2=None, op0=ALU.mult),
                 r=[sv_d, c3_d], w=[st3_d])
        us.append(fin)
        return us

    def units_B(m):
        ng = m + 1
        nk = ng * GRP
        I = Ib[m % 2]
        Id = Igd[m % 2]
        sv, sv_d = svb[m % 2]
        st3, st3_d = st3b[m % 2]
        us = []

        def rnd(n):
            S.op("dve", lambda E: E.tensor_scalar(out=sv[:, 2:4], in0=st3[:, 2 * n:2 * n + 2], scalar1=sv[:, 0:1], scalar2=None, op0=ALU.add),
                 r=[sv_d, st3_d], w=[sv_d, t12_d])
            S.op("act", lambda E: E.activation(out=junk[:, 0:nk], in_=I[:, 0:nk], func=AF.Sign, scale=-1.0, bias=sv[:, 3:4],
                                               accum_out=sa[:, 0:1]), r=Id[0:ng] + [t12_d], w=[junk_d, sa_d])
            S.op("dve", lambda E: E.tensor_scalar(out=mask[:, 0:nk], in0=I[:, 0:nk], scalar1=sv[:, 2:3], scalar2=None,
                                                  op0=ALU.is_ge, op1=ALU.add, accum_out=sv[:, 4:5]), r=Id[0:ng] + [sv_d], w=[mask_d, sv_d])
            S.op("dve", lambda E: E.scalar_tensor_tensor(out=sv[:, 6:7], in0=sv[:, 4:5], scalar=float(TOPK), in1=st3[:, 2 * n:2 * n + 1],
                                                         op0=ALU.is_ge, op1=ALU.mult), r=[sv_d, st3_d], w=[sv_d])
            S.op("dve", lambda E: E.scalar_tensor_tensor(out=sv[:, 7:8], in0=sa[:, 0:1], scalar=float(nk - 2 * TOPK), in1=st3[:, 2 * n:2 * n + 1],
                                                         op0=ALU.is_le, op1=ALU.mult), r=[sv_d, sa_d, st3_d], w=[sv_d])
            S.op("dve", lambda E: E.scalar_tensor_tensor(out=sv[:, 0:1], in0=sv[:, 6:7], scalar=sv[:, 7:8], in1=sv[:, 0:1],
                                                         op0=ALU.add, op1=ALU.add), r=[sv_d], w=[sv_d])
        for n in range(NR3):
            us.append(lambda n=n: rnd(n))

        def fin():
            S.op("dve", lambda E: E.tensor_scalar(out=mask[:, 0:nk], in0=I[:, 0:nk], scalar1=sv[:, 0:1], scalar2=None, op0=ALU.is_ge),
                 r=Id[0:ng] + [sv_d], w=[mask_d])
        us.append(fin)
        return us

    def run_C(m):
        ng = m + 1
        ql, ql_d = qlb[m % 2]
        po, po_d = pso
        pd, pd_d = psd
        nck = 4 * ng

        def front(c):
            cs = slice(c * CH, (c + 1) * CH)
            pT, pT_d = nextT()
            S.op("pe", lambda E: E.transpose(out=pT[0:CH, 0:CH], in_=mask[:, cs], identity=ident[0:CH, 0:CH]),
                 r=[mask_d, ident_d], w=[pT_d])
            pS, pS_d = nextA()
            S.op("pe", lambda E: E.matmul(pS[0:CH, 0:GRP], cT[:, cs], ql[:, :], start=True, stop=True),
                 r=[cT_d, ql_d], w=[pS_d])
            e_, e_d = Et[cnt["ei"] % 2]
            p_, p_d = Pt[cnt["ei"] % 2]
            cnt["ei"] += 1
            S.op("act", lambda E: E.activation(out=e_[:, :], in_=pS[0:CH, 0:GRP], func=AF.Exp), r=[pS_d], w=[e_d])
            S.op("dve", lambda E: E.tensor_tensor(out=p_[:, :, :], in0=e_[:, :].rearrange("p (h q) -> p h q", h=4),
                                                  in1=pT[0:CH, 0:CH].unsqueeze(1).to_broadcast([CH, 4, CH]), op=ALU.mult),
                 r=[e_d, pT_d], w=[p_d])
            return p_, p_d

        def back(c, p_, p_d):
            pf = p_[:, :, :].rearrange("p h q -> p (h q)")
            S.op("pe", lambda E: E.matmul(po[:, 0:GRP], ctok[:, c, :], pf, start=(c == 0), stop=(c == nck - 1)),
                 r=[ctok_d, p_d], w=[po_d])
            S.op("pe", lambda E: E.matmul(pd[:, 0:GRP], onesb[0:CH, :], pf, start=(c == 0), stop=(c == nck - 1)),
                 r=[onesb_d, p_d], w=[pd_d])

        pend = None
        for c in range(nck):
            cur = front(c)
            if pend is not None:
                back(c - 1, *pend)
            pend = cur
        back(nck - 1, *pend)
        S.op("dve", lambda E: E.reciprocal(out=rden[:, :], in_=pd[:, 0:GRP]), r=[pd_d], w=[rden_d])
        S.op("dve", lambda E: E.tensor_tensor(out=ob[:, :], in0=po[:, 0:GRP], in1=rden[:, :], op=ALU.mult), r=[po_d, rden_d], w=[ob_d])
        py, py_d = nextA()
        for h in range(4):
            S.op("pe", lambda E: E.matmul(py[0:64, h * CH:(h + 1) * CH], wuv[:, h, :], ob[:, h * CH:(h + 1) * CH], start=True, stop=True),
                 r=[wuv_d, ob_d], w=[py_d])
        S.op("act", lambda E: E.copy(out=ysb[:, :], in_=py[0:64, 0:GRP]), r=[py_d], w=[ysb_d])
        S.dma(yx_loc[YD0 + m * 256:YD0 + (m + 1) * 256, :].rearrange("(h d) t -> d h t", d=64),
              ysb[:, :].rearrange("d (h t) -> d h t", h=4), r=[ysb_d])

    def interleave(ua, ub):
        na, nb = len(ua), len(ub)
        ia = ib = 0
        while ia < na or ib < nb:
            if ib >= nb or (ia < na and ia * nb <= ib * na):
                ua[ia]()
                ia += 1
            else:
                ub[ib]()
                ib += 1

    for u in units_A(0):
        u()
    for m in range(NB):
        interleave(units_A(m + 1) if m + 1 < NB else [], units_B(m))
        run_C(m)


NGCOLS = 448
PREFETCH = False
NRANK = 8
GB = 4


def build_fused(T, NL):
    NTK = T // 4
    NCH = T // CH
    NB = NCH // 4
    NBK = NTK // CH
    NT = 342 if NTK % 342 == 0 else 228
    YF0 = 0
    YL0 = NCH * 64
    YD0 = 2 * NCH * 64
    YR = YD0 + NB * 256
    nc = bass.Bass("TRN2", target_bir_lowering=False)
    dt = lambda name, shape, kind, dtype=F32: nc.dram_tensor(name, list(shape), dtype, kind=kind).ap()
    h0 = dt("h0", [D, NTK], "ExternalInput")
    gains = dt("gains", [NL, 128, 6 * KC], "ExternalInput")
    fwin = dt("fwin", [NL * 2, 128, KC, 2 * DFF], "ExternalInput")
    fwout = dt("fwout", [NL * 2, 128, JC, D], "ExternalInput")
    pw = dt("pw", [NL, 128, KC, DIN], "ExternalInput")
    ow = dt("ow", [NL, 128, KC, D], "ExternalInput")
    bfi = dt("bfi", [NL, 1, 1], "ExternalInput")
    cwi = dt("cwi", [NL, 128, 2, CW], "ExternalInput")
    cvi = dt("cvi", [NL, 128, 2, 3], "ExternalInput")
    lcwi = dt("lcwi", [NL, 64, 4], "ExternalInput")
    lvi = dt("lvi", [NL, 64, 4], "ExternalInput")
    lwai = dt("lwai", [NL, 64, 64], "ExternalInput")
    lwii = dt("lwii", [NL, 64, 64], "ExternalInput")
    kvgi = dt("kvgi", [NL, 128, 1], "ExternalInput")
    ikgi = dt("ikgi", [NL, 32, 2], "ExternalInput")
    wuki = dt("wuki", [NL, 64, 4, 128], "ExternalInput")
    wuvi = dt("wuvi", [NL, 128, 4, 64], "ExternalInput")
    cori = dt("cori", [128, 2], "ExternalInput")
    gidx = dt("gidx", [128, NGCOLS], "ExternalInput", I32)
    h_out = dt("h_out", [D, NTK], "ExternalOutput")
    hbuf = dt("hbuf", [D, NTK], "Internal")
    zloc = dt("zloc", [DIN, NTK], "Internal")
    zq_loc = dt("zq_loc", [NBK * ZQR, CH], "Internal")
    halo_loc = dt("halo_loc", [2 * GW, HAL], "Internal")
    yx_loc = dt("yx_loc", [YR, CH], "Internal")
    yc_loc = dt("yc_loc", [GW, NTK], "Internal")
    zall = [dt("zall%d" % i, [NRANK * NZG, NTK], "Internal") for i in range(2)]
    zq_all = [dt("zq_all%d" % i, [NRANK * NBK * ZQR, CH], "Internal") for i in range(2)]
    halo_all = [dt("halo_all%d" % i, [NRANK * 2 * GW, HAL], "Internal") for i in range(2)]
    yx_all = [dt("yx_all%d" % i, [NRANK * YR, CH], "Internal") for i in range(2)]
    es = ExitStack()
    with es:
        S = Sched(nc, es)
        PS = PsumPool(S)
        G = Gather(S, NGCOLS)
        G.load(gidx)
        cor = S.sb("cor", [128, 2], F32); cor_d = Dep()
        S.dma(cor[:], cori, w=[cor_d])
        for l in range(NL):
            zA, zqA, hA, yA = zall[l % 2], zq_all[l % 2], halo_all[l % 2], yx_all[l % 2]
            zall_d, zq_d, halo_d, yx_d = Dep(), Dep(), Dep(), Dep()
            with ExitStack() as e1:
                TS = TStage(S, PS, NT, e1)
                TS.load_gains(gains[l])
                TS.load_ffn_weights(fwin[2 * l], fwout[2 * l], 0)
                TS.ffn_phase(h0 if l == 0 else hbuf, hbuf, NTK, 1)
                TS.load_sq_weights(pw[l], DIN, gi=2)
                TS.proj_phase(hbuf, zloc, zq_loc, halo_loc, NTK)
            S.barrier()
            S.collective("AllGather", halo_loc, hA, halo_d)
            S.collective("AllGather", zloc[0:NZG, :], zA, zall_d)
            S.collective("AllGather", zq_loc, zqA, zq_d)
            S.barrier()
            conv_phase(S, PS, G, T, zloc, hA, halo_d, cor[:, 1:2], cor_d, cwi[l], cvi[l], yc_loc)
            S.barrier()
            with ExitStack() as e2:
                fox_phase(S, PS, G, e2, T, zA, zall_d, bfi[l], yx_loc)
            S.barrier()
            lru_phase(S, PS, G, T, zA, zall_d, lcwi[l], lvi[l], lwai[l], lwii[l], yx_loc, YL0)
            S.barrier()
            with ExitStack() as e4:
                dsa_phase(S, PS, G, e4, T, zA, zall_d, zqA, zq_d, cor[:, 0:1], cor_d, kvgi[l], ikgi[l], wuki[l], wuvi[l],
                          yx_loc, YD0)
            S.barrier()
            S.collective("AllGather", yx_loc, yA, yx_d)
            with ExitStack() as e5:
                TS = TStage(S, PS, NT, e5)
                TS.load_gains(gains[l])
                TS.load_sq_weights(ow[l], D)

                def fill_y(sq, sq_d, t0, n, yA=yA, yx_d=yx_d):
                    S.dma(sq[:, 2:4, 0:n], yc_loc[:, t0:t0 + n].rearrange("(c p) n -> p c n", p=128), w=[sq_d])
                    for kb in range(n // CH):
                        blk = t0 // CH + kb
                        cs = slice(kb * CH, (kb + 1) * CH)
                        for kk in range(2):
                            p = np.arange(128)
                            for base, kc0, nm in ((YF0, 0, "yf"), (YL0, 4, "yl")):
                                G.gather(sq[:, kc0 + kk, cs], 128, yA, (nm, blk, kk),
                                         (lambda b, j, base=base, blk=blk, kk=kk:
                                          (GB * b + 2 * kk + p // 64) * YR + base + (j * NBK + blk) * 64 + p % 64),
                                         r=[yx_d], w=[sq_d], acc=True)
                            G.gather(sq[:, 6 + kk, cs], 128, yA, ("yd", blk, kk),
                                     (lambda b, j, blk=blk, kk=kk:
                                      (GB * b + (j * NBK + blk) % 4) * YR + YD0 + ((j * NBK + blk) // 4) * 256 + kk * 128 + p),
                                     r=[yx_d], w=[sq_d], acc=True)

                TS.mixin_phase(hbuf, hbuf, NTK, 3, fill_y)
                TS.load_ffn_weights(fwin[2 * l + 1], fwout[2 * l + 1], 4)
                TS.ffn_phase(hbuf, h_out if l == NL - 1 else hbuf, NTK, 5)
            S.barrier()
        S.finish()
        print("fused ops", S.nops, "waits", S.nwaits, "gather cols", len(G.specs))
    return nc, G


_PROGS = {}


def _f32(a):
    return np.ascontiguousarray(a, dtype=np.float32)


def fused_inputs(G, T, x, meta_tokens, norm_g, ffn_w_in, ffn_w_out, w_in, w_out, fox_b_f,
                 conv_dw_w, conv_dw_b, conv_ln_g, conv_ln_b,
                 lru_conv_w, lru_conv_b, lru_w_a, lru_b_a, lru_w_i, lru_b_i, lru_lambda,
                 dsa_kv_norm_g, dsa_w_uk, dsa_w_uv, idx_k_ln_g, idx_k_ln_b):
    A = lambda a: np.asarray(a, dtype=np.float32)
    NL = norm_g.shape[0]
    B = x.shape[0]
    NTK = T // 4
    h = np.concatenate([np.broadcast_to(A(meta_tokens)[None], (B, NMETA, D)), A(x)], axis=1)
    shared = {
        "gains": _f32(np.stack([lay_gains(A(norm_g[l])) for l in range(NL)])),
        "fwin": _f32(np.stack([lay_kc(A(ffn_w_in[l, i])) for l in range(NL) for i in range(2)])),
        "fwout": _f32(np.stack([lay_kc(A(ffn_w_out[l, i])) for l in range(NL) for i in range(2)])),
        "pw": _f32(np.stack([lay_kc(A(w_in[l])[:, Z_PERM]) for l in range(NL)])),
        "ow": _f32(np.stack([lay_kc(A(w_out[l])) for l in range(NL)])),
        "cwi": _f32(np.stack([A(conv_dw_w[l]).T.reshape(2, 128, CW).transpose(1, 0, 2) for l in range(NL)])),
        "cvi": _f32(np.stack([np.stack([A(conv_dw_b[l]), A(conv_ln_g[l]), A(conv_ln_b[l])], -1).reshape(2, 128, 3).transpose(1, 0, 2)
                              for l in range(NL)])),
        "kvgi": _f32(A(dsa_kv_norm_g).reshape(NL, 128, 1)),
        "ikgi": _f32(np.stack([A(idx_k_ln_g), A(idx_k_ln_b)], -1)),
        "wuki": _f32(A(dsa_w_uk).transpose(0, 3, 1, 2)),
        "wuvi": _f32(A(dsa_w_uv).transpose(0, 2, 1, 3)),
    }
    lvec = np.stack([A(lru_conv_b), A(lru_b_a), A(lru_b_i), A(lru_lambda)], -1)
    maps = []
    for c in range(NCORES):
        b, j = c // 4, c % 4
        m = dict(shared)
        m["h0"] = _f32(h[b, j * NTK:(j + 1) * NTK].T)
        m["bfi"] = _f32(A(fox_b_f)[:, j].reshape(NL, 1, 1))
        m["lcwi"] = _f32(A(lru_conv_w)[:, :, 64 * j:64 * j + 64].transpose(0, 2, 1))
        m["lvi"] = _f32(lvec[:, 64 * j:64 * j + 64])
        m["lwai"] = _f32(A(lru_w_a)[:, j])
        m["lwii"] = _f32(A(lru_w_i)[:, j])
        cor = np.zeros((128, 2), np.float32)
        cor[:, 0] = CH * j
        cor[:, 1] = 1.0 if j > 0 else 0.0
        m["cori"] = cor
        m["gidx"] = G.table(b, j)
        maps.append(m)
    return maps


def kernel(x, **params):
    x = np.asarray(x, dtype=np.float32)
    B = x.shape[0]
    T = T_FULL
    NTK = T // 4
    NL = params["norm_g"].shape[0]
    if "F" not in _PROGS:
        _PROGS["F"] = build_fused(T, NL)
    nc, G = _PROGS["F"]
    maps = fused_inputs(G, T, x, **params)
    res = run_bass_kernel_spmd(nc, maps, core_ids=list(range(NCORES))).results
    out = np.stack([np.concatenate([res[4 * b + j]["h_out"].T for j in range(4)], axis=0)[NMETA:] for b in range(B)], axis=0)
    return np.ascontiguousarray(out, dtype=np.float32)
```
